# Optimizing a Trainium2 kernel written in Bass

```python
import jax, jax.numpy as jnp
from jax import lax
import numpy as np

D_MODEL = 1024
BATCH = 16
SEQ = 2048
DEPTH = 4

HEAD_DIM = 64
POOL_WIDTH = D_MODEL // 2
POOL_WINDOWS = (2, 4, 8, 16)
POOL_GROUPS = len(POOL_WINDOWS)
POOL_GROUP_DIM = POOL_WIDTH // POOL_GROUPS
CONV_WIDTH = D_MODEL // 2
CONV_K = 3
EVEN_IN = POOL_WIDTH + 3 * CONV_WIDTH
EVEN_CAT = POOL_WIDTH + CONV_WIDTH
C_HEADS = (D_MODEL // 2) // HEAD_DIM
C_PATTERNS = ((128, 1), (512, 4), (2048, 16))
D_HEADS = (D_MODEL // 2) // HEAD_DIM
D_KV_HEADS = 2
D_GROUP = D_HEADS // D_KV_HEADS
D_RADIUS = 128
C_WIDTH = C_HEADS * HEAD_DIM
D_Q_WIDTH = D_HEADS * HEAD_DIM
D_KV_WIDTH = D_KV_HEADS * HEAD_DIM
ODD_IN = 3 * C_WIDTH + D_Q_WIDTH + 2 * D_KV_WIDTH
ODD_CAT = C_WIDTH + D_Q_WIDTH
N_EXPERTS = 32
TOP_K = 4
D_EXPERT = D_MODEL
SWIGLU_LIMIT = 7.0
SWIGLU_ALPHA = 1.702
MOE_BLOCK = 256
N_EVEN = (DEPTH + 1) // 2
N_ODD = DEPTH // 2
DEEPNORM_ALPHA = (2 * DEPTH) ** 0.25
DEEPNORM_BETA = (8 * DEPTH) ** -0.25
LN_EPS = 1e-5
NEG_INF = -1e30

kernel_name = 'hybrid_pool_conv_dilated_swa_moe_encoder'


def split_cols(h, sizes):
    return jnp.split(h, [int(i) for i in np.cumsum(sizes)[:-1]], axis=-1)


def layer_norm(x, g, b):
    xf = x.astype(jnp.float32)
    mu = jnp.mean(xf, axis=-1, keepdims=True)
    xc = xf - mu
    var = jnp.mean(xc * xc, axis=-1, keepdims=True)
    y = xc * lax.rsqrt(var + LN_EPS) * g.astype(jnp.float32) + b.astype(jnp.float32)
    return y.astype(x.dtype)


def alibi_slopes(n):
    return jnp.asarray([2.0 ** (-8.0 * (i + 1) / n) for i in range(n)], jnp.float32)


def pool_mixer(u, pool_w, pool_scale):
    b_, s_, _ = u.shape
    uf = u.astype(jnp.float32)
    cs = jnp.concatenate([jnp.zeros_like(uf[:, :1]), jnp.cumsum(uf, axis=1)], axis=1)
    t = jnp.arange(s_)
    outs = []
    for g, w in enumerate(POOL_WINDOWS):
        sl = slice(g * POOL_GROUP_DIM, (g + 1) * POOL_GROUP_DIM)
        lo = jnp.clip(t - w // 2, 0, s_)
        hi = jnp.clip(t + w - w // 2, 0, s_)
        csg = cs[..., sl]
        win_sum = jnp.take(csg, hi, axis=1) - jnp.take(csg, lo, axis=1)
        cnt = (hi - lo).astype(jnp.float32)[None, :, None]
        outs.append(win_sum / cnt - uf[..., sl])
    pooled = jnp.stack(outs, axis=2).astype(u.dtype)
    mixed = jnp.einsum('bsgc,gcd->bsgd', pooled, pool_w).reshape(b_, s_, POOL_WIDTH)
    return mixed * pool_scale


def short_conv_mixer(b_gate, c_gate, v, conv_w):
    u = c_gate * v
    s_ = u.shape[1]
    half = CONV_K // 2
    up = jnp.pad(u, ((0, 0), (half, CONV_K - 1 - half), (0, 0)))
    conv = up[:, 0:s_] * conv_w[0]
    for k in range(1, CONV_K):
        conv = conv + up[:, k:k + s_] * conv_w[k]
    return b_gate * conv


def even_mixer(x, w_in, pool_w, pool_scale, conv_w, w_out):
    h = x @ w_in
    a_in, b_gate, c_gate, v = split_cols(h, (POOL_WIDTH, CONV_WIDTH, CONV_WIDTH, CONV_WIDTH))
    ya = pool_mixer(a_in, pool_w, pool_scale)
    yb = short_conv_mixer(b_gate, c_gate, v, conv_w)
    return jnp.concatenate([ya, yb], axis=-1) @ w_out


def banded_attention(q, k, v, radius, slopes, dist_unit):
    n_, l_, hkv, g_, hd = q.shape
    nb = -(-l_ // radius)
    lp = nb * radius
    pad = lp - l_
    qb = jnp.pad(q, ((0, 0), (0, pad), (0, 0), (0, 0), (0, 0))).reshape(n_, nb, radius, hkv, g_, hd)

    def key_span(t):
        tp = jnp.pad(t, ((0, 0), (radius, radius + pad), (0, 0), (0, 0)))
        tb = tp.reshape(n_, nb + 2, radius, hkv, hd)
        return jnp.concatenate([tb[:, :-2], tb[:, 1:-1], tb[:, 2:]], axis=2)

    kb, vb = key_span(k), key_span(v)
    q_pos = jnp.arange(lp).reshape(nb, radius)
    k_pos = jnp.arange(nb)[:, None] * radius - radius + jnp.arange(3 * radius)[None, :]
    dist = jnp.abs(k_pos[:, None, :] - q_pos[:, :, None])
    valid = (dist <= radius) & (k_pos >= 0)[:, None, :] & (k_pos < l_)[:, None, :]
    s = jnp.einsum('nbqhgd,nbkhd->nbhgqk', qb, kb).astype(jnp.float32) * (hd ** -0.5)
    s = s - slopes[None, None, :, :, None, None] * (dist * dist_unit).astype(jnp.float32)[None, :, None, None]
    s = jnp.where(valid[None, :, None, None], s, NEG_INF)
    lse = jax.nn.logsumexp(s, axis=-1)
    p = jnp.exp(s - lse[..., None]).astype(v.dtype)
    o = jnp.einsum('nbhgqk,nbkhd->nbqhgd', p, vb).reshape(n_, lp, hkv, g_, hd)[:, :l_]
    lse = lse.transpose(0, 1, 4, 2, 3).reshape(n_, lp, hkv, g_)[:, :l_]
    return o, lse


def dilated_attention(q, k, v):
    b_, s_, h_, hd = q.shape
    slopes = alibi_slopes(h_)[:, None]
    outs, lses = [], []
    for window, dil in C_PATTERNS:
        radius = window // 2 // dil
        l_ = s_ // dil

        def to_sub(t):
            return t.reshape(b_, l_, dil, h_, hd).transpose(0, 2, 1, 3, 4).reshape(b_ * dil, l_, h_, hd)

        o, lse = banded_attention(to_sub(q)[:, :, :, None], to_sub(k), to_sub(v), radius, slopes, dil)
        outs.append(o.reshape(b_, dil, l_, h_, hd).transpose(0, 2, 1, 3, 4).reshape(b_, s_, h_, hd))
        lses.append(lse.reshape(b_, dil, l_, h_).transpose(0, 2, 1, 3).reshape(b_, s_, h_))
    w = jax.nn.softmax(jnp.stack(lses, axis=0), axis=0).astype(q.dtype)
    return jnp.sum(w[..., None] * jnp.stack(outs, axis=0), axis=0)


def odd_mixer(x, w_in, sink, w_out):
    b_, s_, _ = x.shape
    h = x @ w_in
    qc, kc, vc, qd, kd, vd = split_cols(h, (C_WIDTH, C_WIDTH, C_WIDTH, D_Q_WIDTH, D_KV_WIDTH, D_KV_WIDTH))
    heads = lambda t, n: t.reshape(b_, s_, n, HEAD_DIM)
    yc = dilated_attention(heads(qc, C_HEADS), heads(kc, C_HEADS), heads(vc, C_HEADS)).reshape(b_, s_, C_WIDTH)
    od, lse = banded_attention(qd.reshape(b_, s_, D_KV_HEADS, D_GROUP, HEAD_DIM),
                               heads(kd, D_KV_HEADS), heads(vd, D_KV_HEADS), D_RADIUS,
                               alibi_slopes(D_HEADS).reshape(D_KV_HEADS, D_GROUP), 1)
    sink_gate = jax.nn.sigmoid(lse - sink.astype(jnp.float32).reshape(D_KV_HEADS, D_GROUP))
    yd = (od * sink_gate[..., None].astype(od.dtype)).reshape(b_, s_, D_Q_WIDTH)
    return jnp.concatenate([yc, yd], axis=-1) @ w_out


def moe_ffn(x, router_w, router_b, w_gu, b_gu, w_down, b_down):
    b_, s_, d_ = x.shape
    t_ = b_ * s_
    xt = x.reshape(t_, d_)
    logits = (xt @ router_w).astype(jnp.float32) + router_b.astype(jnp.float32)
    top_logits, top_idx = lax.top_k(logits, TOP_K)
    gates = jax.nn.softmax(top_logits, axis=-1)
    a_ = t_ * TOP_K
    flat_e = top_idx.reshape(a_)
    order = jnp.argsort(flat_e)
    e_sorted = flat_e[order]
    counts = jnp.bincount(flat_e, length=N_EXPERTS)
    padded = (counts + MOE_BLOCK - 1) // MOE_BLOCK * MOE_BLOCK
    pad_end = jnp.cumsum(padded)
    grp_start = jnp.cumsum(counts) - counts
    dest = (pad_end - padded)[e_sorted] + jnp.arange(a_) - grp_start[e_sorted]
    n_blocks = -(-a_ // MOE_BLOCK) + N_EXPERTS
    rows = n_blocks * MOE_BLOCK
    slot_tok = jnp.full((rows,), t_, jnp.int32).at[dest].set((order // TOP_K).astype(jnp.int32))
    slot_gate = jnp.zeros((rows,), jnp.float32).at[dest].set(gates.reshape(a_)[order])
    block_e = jnp.minimum(jnp.searchsorted(pad_end, jnp.arange(n_blocks) * MOE_BLOCK, side='right'), N_EXPERTS - 1)
    x_rows = jnp.concatenate([xt, jnp.zeros((1, d_), xt.dtype)], axis=0)[slot_tok].reshape(n_blocks, MOE_BLOCK, d_)

    def expert_block(args):
        xb, e = args
        h = xb @ w_gu[e] + b_gu[e]
        glu = jnp.minimum(h[:, 0::2], SWIGLU_LIMIT)
        lin = jnp.clip(h[:, 1::2], -SWIGLU_LIMIT, SWIGLU_LIMIT)
        act = glu * jax.nn.sigmoid(SWIGLU_ALPHA * glu) * (lin + 1.0)
        return act @ w_down[e] + b_down[e]

    y = lax.map(expert_block, (x_rows, block_e)).reshape(rows, d_)
    y = y * slot_gate[:, None].astype(y.dtype)
    out = jnp.zeros((t_ + 1, d_), y.dtype).at[slot_tok].add(y)[:t_]
    return out.reshape(b_, s_, d_)


def setup_inputs(seed: int = 0) -> dict:
    key = jax.random.key(seed)
    ks = jax.random.split(key, 17)

    def normal(k, shape, scale):
        return jax.random.normal(k, shape, jnp.float32) * scale

    return {
        'x': normal(ks[0], (BATCH, SEQ, D_MODEL), 1.0),
        'ev_w_in': normal(ks[1], (N_EVEN, D_MODEL, EVEN_IN), D_MODEL ** -0.5),
        'ev_pool_w': normal(ks[2], (N_EVEN, POOL_GROUPS, POOL_GROUP_DIM, POOL_GROUP_DIM), POOL_GROUP_DIM ** -0.5),
        'ev_pool_scale': 1.0 + normal(ks[3], (N_EVEN, POOL_WIDTH), 0.1),
        'ev_conv_w': normal(ks[4], (N_EVEN, CONV_K, CONV_WIDTH), CONV_K ** -0.5),
        'ev_w_out': normal(ks[5], (N_EVEN, EVEN_CAT, D_MODEL), DEEPNORM_BETA * EVEN_CAT ** -0.5),
        'od_w_in': normal(ks[6], (N_ODD, D_MODEL, ODD_IN), D_MODEL ** -0.5),
        'od_sink': normal(ks[7], (N_ODD, D_HEADS), 1.0),
        'od_w_out': normal(ks[8], (N_ODD, ODD_CAT, D_MODEL), DEEPNORM_BETA * ODD_CAT ** -0.5),
        'router_w': normal(ks[9], (DEPTH, D_MODEL, N_EXPERTS), D_MODEL ** -0.5),
        'router_b': normal(ks[10], (DEPTH, N_EXPERTS), 0.01),
        'exp_w_gu': normal(ks[11], (DEPTH, N_EXPERTS, D_MODEL, 2 * D_EXPERT), D_MODEL ** -0.5),
        'exp_b_gu': normal(ks[12], (DEPTH, N_EXPERTS, 2 * D_EXPERT), 0.02),
        'exp_w_down': normal(ks[13], (DEPTH, N_EXPERTS, D_EXPERT, D_MODEL), DEEPNORM_BETA * D_EXPERT ** -0.5),
        'exp_b_down': normal(ks[14], (DEPTH, N_EXPERTS, D_MODEL), 0.02),
        'ln_g': 1.0 + normal(ks[15], (DEPTH, 2, D_MODEL), 0.02),
        'ln_b': normal(ks[16], (DEPTH, 2, D_MODEL), 0.02),
    }


def reference(x, ev_w_in, ev_pool_w, ev_pool_scale, ev_conv_w, ev_w_out, od_w_in, od_sink, od_w_out,
              router_w, router_b, exp_w_gu, exp_b_gu, exp_w_down, exp_b_down, ln_g, ln_b):
    for layer in range(DEPTH):
        i = layer // 2
        if layer % 2 == 0:
            mix = even_mixer(x, ev_w_in[i], ev_pool_w[i], ev_pool_scale[i], ev_conv_w[i], ev_w_out[i])
        else:
            mix = odd_mixer(x, od_w_in[i], od_sink[i], od_w_out[i])
        x = layer_norm(DEEPNORM_ALPHA * x + mix, ln_g[layer, 0], ln_b[layer, 0])
        ffn = moe_ffn(x, router_w[layer], router_b[layer], exp_w_gu[layer], exp_b_gu[layer],
                      exp_w_down[layer], exp_b_down[layer])
        x = layer_norm(DEEPNORM_ALPHA * x + ffn, ln_g[layer, 1], ln_b[layer, 1])
    return x
```

```python
import numpy as np
from contextlib import ExitStack
import concourse.bass as bass
import concourse.mybir as mybir
from concourse.bass_utils import run_bass_kernel_spmd

F32 = mybir.dt.float32
BF16 = mybir.dt.bfloat16
I32 = mybir.dt.int32
U32 = mybir.dt.uint32
AF = mybir.ActivationFunctionType
ALU = mybir.AluOpType
AX = mybir.AxisListType

NCORES = 8
D = 1024
S = 2048
NSEQ = 2
T = NSEQ * S
NT = T // 128
DEPTH = 4
NE = 32
TOPK = 4
CAP = 768
NJ = CAP // 128
NSLOT = NE * CAP
ALPHA = float((2 * DEPTH) ** 0.25)
LN_EPS = 1e-5
PAD = 8
SW_LIMIT = 7.0
SW_ALPHA = 1.702
POOL_WINDOWS = (2, 4, 8, 16)
C_PATTERNS = ((128, 1), (512, 4), (2048, 16))


class Buf:
    __slots__ = ("name", "w", "r")

    def __init__(self, name):
        self.name = name
        self.w = None
        self.r = {}


class Sched:
    ENGS = ("pe", "act", "dve", "pool", "sp")

    def __init__(self, nc, es, n_lanes=40):
        self.nc = nc
        self.h = {"pe": nc.tensor, "act": nc.scalar, "dve": nc.vector, "pool": nc.gpsimd, "sp": nc.sync}
        self.sem = {e: es.enter_context(nc.semaphore("s_" + e)) for e in self.ENGS}
        self.cnt = {e: 0 for e in self.ENGS}
        self.known = {e: {} for e in self.ENGS}
        self.n_lanes = n_lanes
        self.lsem = [es.enter_context(nc.semaphore("l%d" % i)) for i in range(n_lanes)]
        self.lcnt = [0] * n_lanes
        self.next_lane = 0
        self.next_sw = 0
        self.n_hw = 24
        self.nwait = 0

    def _semof(self, key):
        return self.lsem[key[1]] if isinstance(key, tuple) else self.sem[key]

    def _need(self, e, ev, waits):
        if ev is None:
            return
        key, val = ev
        if key == e and e == "pe":
            return
        if self.known[e].get(key, 0) >= val:
            return
        if waits.get(key, 0) < val:
            waits[key] = val

    @staticmethod
    def _flat(bs):
        out = []
        for b in bs:
            if isinstance(b, (list, tuple)):
                out.extend(Sched._flat(b))
            else:
                out.append(b)
        return out

    def _deps(self, e, reads, writes):
        waits = {}
        for b in reads:
            self._need(e, b.w, waits)
        for b in writes:
            self._need(e, b.w, waits)
            for k, v in b.r.items():
                self._need(e, (k, v), waits)
        return waits

    def _emit_waits(self, e, waits):
        h = self.h[e]
        for k, v in waits.items():
            h.wait_ge(self._semof(k), v)
            self.known[e][k] = v
            self.nwait += 1

    def op(self, e, fn, reads=(), writes=()):
        reads = self._flat(reads); writes = self._flat(writes)
        waits = self._deps(e, reads, writes)
        self._emit_waits(e, waits)
        ins = fn(self.h[e])
        ins.then_inc(self.sem[e], 1)
        self.cnt[e] += 1
        v = self.cnt[e]
        for b in reads:
            b.r[e] = v
        for b in writes:
            b.w = (e, v)
            b.r = {}

    def dma(self, q, fn, reads=(), writes=()):
        if q == "pool":
            lane = self.n_hw + self.next_sw
            self.next_sw = (self.next_sw + 1) % (self.n_lanes - self.n_hw)
        else:
            lane = self.next_lane
            self.next_lane = (lane + 1) % self.n_hw
        key = ("L", lane)
        reads = self._flat(reads); writes = self._flat(writes)
        waits = self._deps(q, reads, writes)
        self._need(q, (key, self.lcnt[lane]), waits)
        self._emit_waits(q, waits)
        ins = fn(self.h[q])
        ins.then_inc(self.lsem[lane], 16)
        self.lcnt[lane] += 16
        v = self.lcnt[lane]
        for b in reads:
            b.r[key] = v
        for b in writes:
            b.w = (key, v)
            b.r = {}

    def barrier(self):
        for e in self.ENGS:
            waits = {}
            for o in self.ENGS:
                if o != e:
                    self._need(e, (o, self.cnt[o]), waits)
            for i in range(self.n_lanes):
                self._need(e, (("L", i), self.lcnt[i]), waits)
            self._emit_waits(e, waits)


def _consts():
    c = {}
    c["ident"] = np.eye(128, dtype=np.float32)
    c["ustrict"] = np.triu(np.ones((128, 128), np.float32), 1)
    c["ones"] = np.ones((128, 128), np.float32)
    rc = np.ones((4, 16), np.float32)
    for g, w in enumerate(POOL_WINDOWS):
        for t in range(8):
            lo = max(t - w // 2, 0)
            hi = min(t + w - w // 2, S)
            rc[g, t] = 1.0 / (hi - lo)
            tt = S - 8 + t
            lo = max(tt - w // 2, 0)
            hi = min(tt + w - w // 2, S)
            rc[g, 8 + t] = 1.0 / (hi - lo)
    c["poolrc"] = np.broadcast_to(rc.reshape(1, 64), (128, 64)).copy()
    ecap = (np.arange(NE, dtype=np.float32) * CAP)
    c["ecap"] = np.broadcast_to(np.tile(ecap, NT).reshape(1, NT * NE), (128, NT * NE)).copy()
    c["tokidx"] = (np.arange(NT, dtype=np.int32)[None, :] * 128 + np.arange(128, dtype=np.int32)[:, None]).astype(np.int32)
    c["dump"] = np.broadcast_to((NSLOT + np.arange(128, dtype=np.float32))[:, None], (128, NT * NE)).copy()
    init = np.zeros((128, (NSLOT + 128) // 128, 2), np.int32)
    init[:, :, 0] = T
    c["slotinit"] = init
    slopes = np.array([2.0 ** (-8.0 * (i + 1) / 8) for i in range(8)], np.float64)
    kk = np.arange(128)[:, None]
    cc = np.arange(384)[None, :]
    dist = np.abs(cc - 128 - kk)
    ed = np.zeros((128, 8, 384), np.float32)
    for h in range(8):
        ed[:, h, :] = np.where(dist <= 128, np.exp(-slopes[h] * dist), 0.0)
    c["edec_d"] = ed
    cc = np.arange(256)[None, :]
    dist = np.abs(cc - 64 - kk)
    ec = np.zeros((128, 3, 8, 256), np.float32)
    for p, (_, dil) in enumerate(C_PATTERNS):
        for h in range(8):
            ec[:, p, h, :] = np.where(dist <= 64, np.exp(-slopes[h] * dist * dil), 0.0)
    c["edec_c"] = ec
    return c


CONSTS = None


def build_nc(layers=tuple(range(DEPTH)), debug=False, stop_after=None):
    nc = bass.Bass("TRN2", target_bir_lowering=False)
    dram = {}
    _uid = [0]

    def UQ(name):
        _uid[0] += 1
        return "%s_u%d" % (name, _uid[0])

    def din(name, shape, dt=F32):
        dram[name] = nc.dram_tensor(name, list(shape), dt, kind="ExternalInput").ap()
        return dram[name]

    x_in = din("x", [T, D])
    ev_w_in = din("ev_w_in", [2, D, 2048])
    ev_pool_w = din("ev_pool_w", [2, 4, 128, 128])
    ev_pool_scale = din("ev_pool_scale", [2, 512])
    ev_conv_w = din("ev_conv_w", [2, 3, 512])
    ev_w_out = din("ev_w_out", [2, 1024, 1024])
    od_w_in = din("od_w_in", [2, D, 2304])
    od_sink = din("od_sink", [2, 8])
    od_w_out = din("od_w_out", [2, 1024, 1024])
    router_w = din("router_w", [DEPTH, D, NE])
    router_b = din("router_b", [DEPTH, NE])
    exp_w_gu = din("exp_w_gu", [DEPTH, NE, D, 2048])
    exp_b_gu = din("exp_b_gu", [DEPTH, NE, 2048])
    exp_w_down = din("exp_w_down", [DEPTH, NE, 1024, D])
    exp_b_down = din("exp_b_down", [DEPTH, NE, D])
    ln_g = din("ln_g", [DEPTH, 2, D])
    ln_b = din("ln_b", [DEPTH, 2, D])
    cst = {}
    for k, v in CONSTS.items():
        cst[k] = din("c_" + k, v.shape, I32 if v.dtype == np.int32 else F32)

    y_out = nc.dram_tensor("y", [T, D], F32, kind="ExternalOutput").ap()
    okind = "ExternalOutput" if debug else "Internal"
    XA = nc.dram_tensor("XA", [T, D], F32, kind=okind).ap()
    X1 = nc.dram_tensor("X1", [T, D], F32, kind=okind).ap()
    XT = nc.dram_tensor("XT", [D, T], BF16, kind="Internal").ap()
    XS = nc.dram_tensor("XS", [T + 128, D], BF16, kind="Internal").ap()
    SLOT = nc.dram_tensor("SLOT", [NSLOT + 128, 2], I32, kind="Internal").ap()
    YS = nc.dram_tensor("YS", [NSLOT + 128, D], F32, kind="Internal").ap()
    dXA, dX1, dXT, dXS, dSLOT, dYS = (Buf(n) for n in ("XA", "X1", "XT", "XS", "SLOT", "YS"))
    dY = Buf("Y")

    with ExitStack() as es:
        sc = Sched(nc, es)
        sb = lambda name, shape, dt=F32: es.enter_context(nc.sbuf_tensor(UQ(name), list(shape), dt))

        ident = sb("ident", [128, 128])
        ident_bf = sb("ident_bf", [128, 128], BF16)
        ustrict_bf = sb("ustrict_bf", [128, 128], BF16)
        ones_bf = sb("ones_bf", [128, 128], BF16)
        ctmp = sb("ctmp", [128, 128])
        poolrc = sb("poolrc", [128, 64])
        tokidx = sb("tokidx", [128, NT], I32)
        G_all = sb("G_all", [128, NT, NE])
        L_all = sb("L_all", [128, NT, NE])
        M8_all = sb("M8_all", [128, NT, 8])
        M_all = sb("M_all", [128, NT * NE], BF16)
        SLK = sb("SLK", [128, TOPK, NT], U32)
        PAY = sb("PAY", [128, NT, TOPK, 2], I32)
        zrow = sb("zrow", [128, D], BF16)
        bC = Buf("consts")
        bG, bL, bM8, bM, bSLK, bPAY = (Buf(n) for n in ("G", "L", "M8", "M", "SLK", "PAY"))

        PS = [es.enter_context(nc.psum_tensor("ps%d" % i, [128, 1024], F32)) for i in range(4)]
        bPSh = [[Buf("ps%d_%d" % (i, hh)) for hh in range(2)] for i in range(4)]

        class _BP:
            def __getitem__(self, i):
                return _BPi(i)

        class _BPi(list):
            def __init__(self, i):
                super().__init__(bPSh[i])
        bPS = _BP()

        def ld(q, out_ap, in_ap, reads=(), writes=(), **kw):
            sc.dma(q, lambda h: h.dma_start(out=out_ap, in_=in_ap, **kw), reads=reads, writes=writes)

        ld("sp", ident[:], cst["ident"], writes=[bC])
        ld("sp", poolrc[:], cst["poolrc"], writes=[bC])
        ld("sp", tokidx[:], cst["tokidx"], writes=[bC])
        ld("pool", ident_bf[:], cst["ident"], writes=[bC])
        ld("pool", ustrict_bf[:], cst["ustrict"], writes=[bC])
        ld("pool", ones_bf[:], cst["ones"], writes=[bC])
        sc.op("dve", lambda h: h.memset(zrow[:], 0.0), writes=[bC])
        ld("sp", XS[T:T + 128, :], zrow[:], reads=[bC], writes=[dXS])
        with nc.sbuf_tensor(UQ("zf"), [128, D], F32) as zf:
            bzf = Buf("zf")
            sc.op("dve", lambda h: h.memset(zf[:], 0.0), writes=[bzf])
            ld("sp", YS[NSLOT:NSLOT + 128, :], zf[:], reads=[bzf], writes=[dYS])
            sc.barrier()

        def layer_norm_tile(z, bz, gt, bt_, bgb, out, bout, tmp):
            st, mv, rstd = tmp["st"], tmp["mv"], tmp["rstd"]
            bst = tmp["bst"]
            sc.op("dve", lambda h: h.bn_stats(out=st[:, 0, :], in_=z[:, 0:512]), reads=[bz], writes=[bst])
            sc.op("dve", lambda h: h.bn_stats(out=st[:, 1, :], in_=z[:, 512:1024]), reads=[bz], writes=[bst])
            sc.op("dve", lambda h: h.bn_aggr(out=mv[:], in_=st[:].rearrange("p a b -> p (a b)")), reads=[bst], writes=[bst])
            sc.op("dve", lambda h: h.tensor_scalar(out=rstd[:], in0=mv[:, 1:2], scalar1=LN_EPS, scalar2=None, op0=ALU.add), reads=[bst], writes=[bst])
            sc.op("act", lambda h: h.activation(out=rstd[:], in_=rstd[:], func=AF.Sqrt), reads=[bst], writes=[bst])
            sc.op("dve", lambda h: h.reciprocal(out=rstd[:], in_=rstd[:]), reads=[bst], writes=[bst])
            sc.op("dve", lambda h: h.tensor_scalar(out=z[:], in0=z[:], scalar1=mv[:, 0:1], scalar2=rstd[:, 0:1], op0=ALU.subtract, op1=ALU.mult), reads=[bz, bst], writes=[bz])
            sc.op("pool", lambda h: h.tensor_tensor(out=z[:], in0=z[:], in1=gt[:], op=ALU.mult), reads=[bz, bgb], writes=[bz])
            sc.op("pool", lambda h: h.tensor_tensor(out=out[:], in0=z[:], in1=bt_[:], op=ALU.add), reads=[bz, bgb], writes=[bout])

        def transpose_to_XT(xt_tile, bxt, i, ps, bps, xtb, bxtb):
            def f(h):
                ins = None
                for k in range(8):
                    ins = h.transpose(out=ps[:, k * 128:(k + 1) * 128], in_=xt_tile[:, k * 128:(k + 1) * 128], identity=ident[:])
                return ins
            sc.op("pe", f, reads=[bxt, bC], writes=[bps])
            sc.op("act", lambda h: h.activation(out=xtb[:].rearrange("p k t -> p (k t)"), in_=ps[:], func=AF.Copy), reads=[bps], writes=[bxtb])
            ld("sp", XT.rearrange("(k p) t -> p k t", p=128)[:, :, i * 128:(i + 1) * 128], xtb[:], reads=[bxtb], writes=[dXT])

        with ExitStack() as p0:
            xts = [p0.enter_context(nc.sbuf_tensor(UQ("p0x%d" % i), [128, D], F32)) for i in range(2)]
            bxts = [Buf("p0x%d" % i) for i in range(2)]
            xtbs = [p0.enter_context(nc.sbuf_tensor(UQ("p0b%d" % i), [128, 8, 128], BF16)) for i in range(2)]
            bxtbs = [Buf("p0b%d" % i) for i in range(2)]
            for i in range(NT):
                a = i % 2
                ld("sp", xts[a][:], x_in[i * 128:(i + 1) * 128, :], writes=[bxts[a]])
                transpose_to_XT(xts[a], bxts[a], i, PS[a], bPS[a], xtbs[a], bxtbs[a])
            sc.barrier()

        Xcur = x_in
        bXcur = Buf("xin")

        for L in layers:
            li = L // 2
            last = (L == layers[-1])
            with ExitStack() as p1:
                sbq = lambda name, shape, dt=F32: p1.enter_context(nc.sbuf_tensor(UQ(name), list(shape), dt))
                catT = sbq("catT", [128, 8, S], BF16); bcat = Buf("catT")
                for s in range(NSEQ):
                    t0 = s * S
                    with ExitStack() as pa:
                        sbp = lambda name, shape, dt=F32: pa.enter_context(nc.sbuf_tensor(UQ(name), list(shape), dt))
                        xT = sbp("xT", [128, 8, S], BF16); bxT = Buf("xT")
                        for k in range(8):
                            ld("sp", xT[:, k, :], XT[k * 128:(k + 1) * 128, t0:t0 + S], reads=[dXT], writes=[bxT])
                        pcnt = [0]
                        if L % 2 == 0:
                            win = sbp("win", [128, 8, 2048], BF16); bwin = Buf("win")
                            for k in range(8):
                                ld("pool", win[:, k, :], ev_w_in[li, k * 128:(k + 1) * 128, :], writes=[bwin], max_dma_last_dim=4096)
                            poolw = sbp("poolw", [128, 4, 128], BF16); bpw = Buf("poolw")
                            ld("pool", poolw[:], ev_pool_w[li].rearrange("g c d -> c g d"), writes=[bpw])
                            pscale = sbp("pscale", [128, 4]); convw = sbp("convw", [128, 3, 4])
                            ld("sp", pscale[:], ev_pool_scale[li].rearrange("(g p) -> p g", p=128), writes=[bpw], allow_slow_non_contiguous=True)
                            ld("sp", convw[:], ev_conv_w[li].rearrange("k (m p) -> p k m", p=128), writes=[bpw], allow_slow_non_contiguous=True)
                            WB = [sbp("wb%d" % i, [128, S + 2 * PAD]) for i in range(4)]
                            bWB = [Buf("wb%d" % i) for i in range(4)]
                            plb = sbp("plb", [128, S], BF16); bplb = Buf("plb")
                            etmp = sbp("etmp", [128, 16]); betmp = Buf("etmp")
                            for i in range(4):
                                sc.op("dve", lambda h, i=i: h.memset(WB[i][:], 0.0), writes=[bWB[i]])

                            def proj(fc, evac):
                                for tc in range(4):
                                    pi = pcnt[0] % 4
                                    pcnt[0] += 1
                                    ps = PS[pi]; bps = bPSh[pi][0]

                                    def f(h, tc=tc, ps=ps):
                                        ins = None
                                        for k in range(8):
                                            ins = h.matmul(ps[:, 0:512], lhsT=win[:, k, fc * 128:(fc + 1) * 128], rhs=xT[:, k, tc * 512:(tc + 1) * 512], start=(k == 0), stop=(k == 7))
                                        return ins
                                    sc.op("pe", f, reads=[bwin, bxT], writes=[bps])
                                    evac(tc, ps[:, 0:512], bps)

                            for g, w in enumerate(POOL_WINDOWS):
                                U, A, B = WB[0], WB[1], WB[2]
                                bU, bA, bB = bWB[0], bWB[1], bWB[2]
                                proj(g, lambda tc, pa_, bps: sc.op("act", lambda h: h.activation(out=U[:, PAD + tc * 512:PAD + (tc + 1) * 512], in_=pa_, func=AF.Copy), reads=[bps], writes=[bU]))
                                lo, n = PAD - 7, S + 14
                                sc.op("dve", lambda h: h.tensor_tensor(out=A[:, lo:lo + n], in0=U[:, lo - 1:lo - 1 + n], in1=U[:, lo:lo + n], op=ALU.add), reads=[bU], writes=[bA])
                                cur, bcur, oth, both = A, bA, B, bB
                                ext = 7
                                ww = 2
                                while ww < w:
                                    sh = ww // 2
                                    ext = ext - sh
                                    lo, n = PAD - ext, S + 2 * ext
                                    sc.op("dve", lambda h, cur=cur, oth=oth, lo=lo, n=n, sh=sh: h.tensor_tensor(out=oth[:, lo:lo + n], in0=cur[:, lo - sh:lo - sh + n], in1=cur[:, lo + sh:lo + sh + n], op=ALU.add), reads=[bcur], writes=[both])
                                    cur, bcur, oth, both = oth, both, cur, bcur
                                    ww *= 2
                                sc.op("dve", lambda h, cur=cur: h.scalar_tensor_tensor(out=plb[:], in0=cur[:, PAD:PAD + S], scalar=1.0 / w, in1=U[:, PAD:PAD + S], op0=ALU.mult, op1=ALU.subtract), reads=[bcur, bU], writes=[bplb])
                                sc.op("dve", lambda h, cur=cur: h.tensor_tensor(out=etmp[:, 0:8], in0=cur[:, PAD:PAD + 8], in1=poolrc[:, g * 16:g * 16 + 8], op=ALU.mult), reads=[bcur, bC], writes=[betmp])
                                sc.op("dve", lambda h, cur=cur: h.tensor_tensor(out=etmp[:, 8:16], in0=cur[:, PAD + S - 8:PAD + S], in1=poolrc[:, g * 16 + 8:g * 16 + 16], op=ALU.mult), reads=[bcur, bC], writes=[betmp])
                                sc.op("dve", lambda h: h.tensor_tensor(out=plb[:, 0:8], in0=etmp[:, 0:8], in1=U[:, PAD:PAD + 8], op=ALU.subtract), reads=[betmp, bU], writes=[bplb])
                                sc.op("dve", lambda h: h.tensor_tensor(out=plb[:, S - 8:S], in0=etmp[:, 8:16], in1=U[:, PAD + S - 8:PAD + S], op=ALU.subtract), reads=[betmp, bU], writes=[bplb])
                                for tc in range(4):
                                    pi = pcnt[0] % 4
                                    pcnt[0] += 1
                                    ps = PS[pi]; bps = bPSh[pi][0]
                                    sc.op("pe", lambda h, ps=ps, tc=tc: h.matmul(ps[:, 0:512], lhsT=poolw[:, g, :], rhs=plb[:, tc * 512:(tc + 1) * 512], start=True, stop=True), reads=[bpw, bplb], writes=[bps])
                                    sc.op("act", lambda h, ps=ps, tc=tc: h.activation(out=catT[:, g, tc * 512:(tc + 1) * 512], in_=ps[:, 0:512], func=AF.Copy, scale=pscale[:, g:g + 1]), reads=[bps, bpw], writes=[bcat])
                            for m in range(4):
                                Cb, U, A = WB[0], WB[3], WB[1]
                                bCb, bU, bA = bWB[0], bWB[3], bWB[1]
                                proj(8 + m, lambda tc, pa_, bps: sc.op("act", lambda h: h.activation(out=Cb[:, PAD + tc * 512:PAD + (tc + 1) * 512], in_=pa_, func=AF.Copy), reads=[bps], writes=[bCb]))
                                proj(12 + m, lambda tc, pa_, bps: sc.op("dve", lambda h: h.tensor_tensor(out=U[:, PAD + tc * 512:PAD + (tc + 1) * 512], in0=Cb[:, PAD + tc * 512:PAD + (tc + 1) * 512], in1=pa_, op=ALU.mult), reads=[bps, bCb], writes=[bU]))
                                sc.op("dve", lambda h: h.tensor_scalar(out=A[:, PAD:PAD + S], in0=U[:, PAD - 1:PAD - 1 + S], scalar1=convw[:, 0, m:m + 1], scalar2=None, op0=ALU.mult), reads=[bU, bpw], writes=[bA])
                                sc.op("dve", lambda h: h.scalar_tensor_tensor(out=A[:, PAD:PAD + S], in0=U[:, PAD:PAD + S], scalar=convw[:, 1, m:m + 1], in1=A[:, PAD:PAD + S], op0=ALU.mult, op1=ALU.add), reads=[bU, bpw, bA], writes=[bA])
                                sc.op("dve", lambda h: h.scalar_tensor_tensor(out=A[:, PAD:PAD + S], in0=U[:, PAD + 1:PAD + 1 + S], scalar=convw[:, 2, m:m + 1], in1=A[:, PAD:PAD + S], op0=ALU.mult, op1=ALU.add), reads=[bU, bpw, bA], writes=[bA])
                                proj(4 + m, lambda tc, pa_, bps: sc.op("dve", lambda h: h.tensor_tensor(out=catT[:, 4 + m, tc * 512:(tc + 1) * 512], in0=A[:, PAD + tc * 512:PAD + (tc + 1) * 512], in1=pa_, op=ALU.mult), reads=[bps, bA], writes=[bcat]))
                        else:
                            win = sbp("win", [128, 8, 2304], BF16); bwin = Buf("win")
                            for k in range(8):
                                ld("pool", win[:, k, :], od_w_in[li, k * 128:(k + 1) * 128, :], writes=[bwin], max_dma_last_dim=4096)
                            qT = sbp("qT", [128, S], BF16); bqT = Buf("qT")
                            kT = sbp("kT", [128, S], BF16); bkT = Buf("kT")
                            VV = sbp("VV", [128, 48, 128], BF16); bVV = Buf("VV")
                            PT = [sbp("PT%d" % i, [128, 16, 384], BF16) for i in range(2)]; bPT = [Buf("PT%d" % i) for i in range(2)]
                            Pr = [sbp("Pr%d" % i, [128, 384], BF16) for i in range(2)]; bPr = [Buf("Pr%d" % i) for i in range(2)]
                            acc_o = sbp("acc_o", [128, S]); bacc_o = Buf("acc_o")
                            acc_d = sbp("acc_d", [128, S]); bacc_d = Buf("acc_d")
                            etab = sbp("etab", [128, 3, 2, 384]); betab = Buf("etab")
                            esink = sbp("esink", [128, 8]); besink = Buf("esink")
                            ld("sp", esink[:], od_sink[li:li + 1, :].to_broadcast([128, 8]), writes=[besink])
                            sc.op("act", lambda h: h.activation(out=esink[:], in_=esink[:], func=AF.Exp), reads=[besink], writes=[besink])
                            ptc = [0]
                            sct = [0]

                            def projT(col0, dst, bdst, dup=False):
                                for tc in range(4):
                                    pi = pcnt[0] % 4
                                    pcnt[0] += 1
                                    ps = PS[pi // 2]; hh = pi % 2; bps = bPSh[pi // 2][hh]

                                    def f(h, tc=tc, ps=ps, hh=hh):
                                        ins = None
                                        for k in range(8):
                                            if not dup:
                                                ins = h.matmul(ps[:, hh * 512:(hh + 1) * 512], lhsT=win[:, k, col0:col0 + 128], rhs=xT[:, k, tc * 512:(tc + 1) * 512], start=(k == 0), stop=(k == 7))
                                            else:
                                                for half in range(2):
                                                    ins = h.matmul(ps[half * 64:(half + 1) * 64, hh * 512:(hh + 1) * 512], lhsT=win[:, k, col0:col0 + 64], rhs=xT[:, k, tc * 512:(tc + 1) * 512], start=(k == 0), stop=(k == 7))
                                        return ins
                                    sc.op("pe", f, reads=[bwin, bxT], writes=[bps])
                                    sc.op("act", lambda h, ps=ps, hh=hh, tc=tc: h.activation(out=dst[:, tc * 512:(tc + 1) * 512], in_=ps[:, hh * 512:(hh + 1) * 512], func=AF.Copy), reads=[bps], writes=[bdst])

                            def projV(col0, tiles):
                                for (vi, tstart, tstep) in tiles:
                                    pi = pcnt[0] % 4
                                    pcnt[0] += 1
                                    ps = PS[pi // 2]; hh = pi % 2; bps = bPSh[pi // 2][hh]

                                    def f(h, ps=ps, hh=hh, tstart=tstart, tstep=tstep):
                                        ins = None
                                        for k in range(8):
                                            ins = h.matmul(ps[:, hh * 512:hh * 512 + 128], lhsT=xT[:, k, tstart:tstart + 127 * tstep + 1:tstep], rhs=win[:, k, col0:col0 + 128], start=(k == 0), stop=(k == 7))
                                        return ins
                                    sc.op("pe", f, reads=[bwin, bxT], writes=[bps])
                                    sc.op("act", lambda h, ps=ps, hh=hh, vi=vi: h.activation(out=VV[:, vi, :], in_=ps[:, hh * 512:hh * 512 + 128], func=AF.Copy), reads=[bps], writes=[bVV])

                            def attend(hb, vcol, tabsel, patterns, first):
                                for pidx, (dil, vvb, W, rad) in enumerate(patterns):
                                    Ls = S // dil
                                    ntile = Ls // 128
                                    pt = PT[ptc[0] % 2]; bpt = bPT[ptc[0] % 2]
                                    ptc[0] += 1
                                    tb = etab[:, tabsel[pidx], hb // 64, :]
                                    for r in range(dil):
                                        for j in range(ntile):
                                            q0 = 128 * j - rad
                                            c_lo = max(0, -q0)
                                            c_hi = min(W, Ls - q0)
                                            nq = c_hi - c_lo
                                            si = sct[0] % 4
                                            sct[0] += 1
                                            ps = PS[si // 2]; hh = si % 2; bps = bPSh[si // 2][hh]
                                            kst = r + dil * (128 * j)
                                            qst = r + dil * (q0 + c_lo)
                                            sc.op("pe", lambda h, ps=ps, hh=hh, kst=kst, qst=qst, nq=nq: h.matmul(
                                                ps[:, hh * 512:hh * 512 + nq],
                                                lhsT=kT[hb:hb + 64, kst:kst + 127 * dil + 1:dil],
                                                rhs=qT[hb:hb + 64, qst:qst + (nq - 1) * dil + 1:dil], start=True, stop=True),
                                                reads=[bkT, bqT], writes=[bps])
                                            pr = Pr[si % 2]; bpr = bPr[si % 2]
                                            sc.op("act", lambda h, ps=ps, hh=hh, nq=nq, pr=pr: h.activation(out=pr[:, 0:nq], in_=ps[:, hh * 512:hh * 512 + nq], func=AF.Exp, scale=0.125), reads=[bps], writes=[bpr])
                                            ti = r * ntile + j
                                            sc.op("dve", lambda h, pr=pr, nq=nq, c_lo=c_lo, ti=ti: h.tensor_tensor(out=pt[:, ti, c_lo:c_lo + nq], in0=pr[:, 0:nq], in1=tb[:, c_lo:c_lo + nq], op=ALU.mult), reads=[bpr, betab], writes=[bpt])
                                    QB = rad
                                    nqb = 512 // QB
                                    for ch in range(S // 512):
                                        hh = ch % 2
                                        pso, bpso = PS[2], bPSh[2][hh]
                                        psd, bpsd = PS[3], bPSh[3][hh]

                                        def fpv(h, which, ps, ch=ch, hh=hh):
                                            ins = None
                                            for b in range(nqb):
                                                gq = ch * 512 + b * QB
                                                r = gq // Ls
                                                l0 = gq % Ls
                                                parts = []
                                                for j in range(ntile):
                                                    k_lo = max(128 * j, l0 - rad)
                                                    k_hi = min(128 * j + 128, l0 + QB + rad)
                                                    if k_hi <= k_lo:
                                                        continue
                                                    parts.append((j, k_lo - 128 * j, k_hi - 128 * j))
                                                parts = [(j, 0, 128) for (j, _a, _b) in parts]
                                                for n_, (j, p_lo, p_hi) in enumerate(parts):
                                                    ti = r * ntile + j
                                                    col = l0 - (128 * j - rad)
                                                    if which == 0:
                                                        lhsT = VV[p_lo:p_hi, vvb + ti, vcol:vcol + 64]
                                                    else:
                                                        lhsT = ones_bf[p_lo:p_hi, 0:64]
                                                    ins = h.matmul(ps[hb:hb + 64, hh * 512 + b * QB:hh * 512 + (b + 1) * QB], lhsT=lhsT,
                                                                   rhs=pt[p_lo:p_hi, ti, col:col + QB], start=(n_ == 0), stop=(n_ == len(parts) - 1))
                                            return ins
                                        sc.op("pe", lambda h, pso=pso: fpv(h, 0, pso), reads=[bVV, bpt], writes=[bpso])
                                        sc.op("pe", lambda h, psd=psd: fpv(h, 1, psd), reads=[bC, bpt], writes=[bpsd])
                                        if dil == 1:
                                            dso = acc_o[hb:hb + 64, ch * 512:(ch + 1) * 512]
                                            dsd = acc_d[hb:hb + 64, ch * 512:(ch + 1) * 512]
                                            srco = pso[hb:hb + 64, hh * 512:(hh + 1) * 512]
                                            srcd = psd[hb:hb + 64, hh * 512:(hh + 1) * 512]
                                        else:
                                            nr = 512 // Ls if Ls < 512 else 1
                                            if Ls >= 512:
                                                r = (ch * 512) // Ls
                                                l0 = (ch * 512) % Ls
                                                st_ = r + dil * l0
                                                dso = acc_o[hb:hb + 64, st_:st_ + 511 * dil + 1:dil]
                                                dsd = acc_d[hb:hb + 64, st_:st_ + 511 * dil + 1:dil]
                                                srco = pso[hb:hb + 64, hh * 512:(hh + 1) * 512]
                                                srcd = psd[hb:hb + 64, hh * 512:(hh + 1) * 512]
                                            else:
                                                r0 = (ch * 512) // Ls
                                                dso = acc_o[hb:hb + 64, :].rearrange("p (l r) -> p r l", r=dil)[:, r0:r0 + nr, :]
                                                dsd = acc_d[hb:hb + 64, :].rearrange("p (l r) -> p r l", r=dil)[:, r0:r0 + nr, :]
                                                srco = pso[hb:hb + 64, hh * 512:(hh + 1) * 512].rearrange("p (r l) -> p r l", r=nr)
                                                srcd = psd[hb:hb + 64, hh * 512:(hh + 1) * 512].rearrange("p (r l) -> p r l", r=nr)
                                        if first and pidx == 0:
                                            sc.op("act", lambda h, dso=dso, srco=srco: h.activation(out=dso, in_=srco, func=AF.Copy), reads=[bpso], writes=[bacc_o])
                                            sc.op("act", lambda h, dsd=dsd, srcd=srcd: h.activation(out=dsd, in_=srcd, func=AF.Copy), reads=[bpsd], writes=[bacc_d])
                                        else:
                                            sc.op("dve", lambda h, dso=dso, srco=srco: h.tensor_tensor(out=dso, in0=dso, in1=srco, op=ALU.add), reads=[bpso, bacc_o], writes=[bacc_o])
                                            sc.op("dve", lambda h, dsd=dsd, srcd=srcd: h.tensor_tensor(out=dsd, in0=dsd, in1=srcd, op=ALU.add), reads=[bpsd, bacc_d], writes=[bacc_d])

                            def finish(chunk, sink_heads=None):
                                if sink_heads is not None:
                                    for half, hd_ in enumerate(sink_heads):
                                        sc.op("dve", lambda h, half=half, hd_=hd_: h.tensor_scalar(out=acc_d[half * 64:(half + 1) * 64, :], in0=acc_d[half * 64:(half + 1) * 64, :], scalar1=esink[half * 64:(half + 1) * 64, hd_:hd_ + 1], scalar2=None, op0=ALU.add), reads=[bacc_d, besink], writes=[bacc_d])
                                sc.op("dve", lambda h: h.reciprocal(out=acc_d[:], in_=acc_d[:]), reads=[bacc_d], writes=[bacc_d])
                                sc.op("pool", lambda h: h.tensor_tensor(out=catT[:, chunk, :], in0=acc_o[:], in1=acc_d[:], op=ALU.mult), reads=[bacc_o, bacc_d], writes=[bcat])

                            for c in range(4):
                                ld("sp", etab[:, :, :, 0:256], cst["edec_c"][:, :, 2 * c:2 * c + 2, :], writes=[betab])
                                projT(0 + c * 128, qT, bqT)
                                projT(512 + c * 128, kT, bkT)
                                tiles = []
                                vvb = {}
                                vi = 0
                                for (_, dil) in C_PATTERNS:
                                    vvb[dil] = vi
                                    Ls = S // dil
                                    for r in range(dil):
                                        for j in range(Ls // 128):
                                            tiles.append((vi, r + dil * 128 * j, dil))
                                            vi += 1
                                projV(1024 + c * 128, tiles)
                                pats = [(dil, vvb[dil], 256, 64) for (_, dil) in C_PATTERNS]
                                for half in range(2):
                                    attend(half * 64, half * 64, [0, 1, 2], pats, True)
                                finish(c)
                            for c in range(4):
                                g = c // 2
                                ld("sp", etab[:, 0, :, :], cst["edec_d"][:, 2 * c:2 * c + 2, :], writes=[betab])
                                projT(1536 + c * 128, qT, bqT)
                                projT(2048 + g * 64, kT, bkT, dup=True)
                                projV(2176, [(j, 128 * j, 1) for j in range(16)])
                                for half in range(2):
                                    attend(half * 64, g * 64, [0], [(1, 0, 384, 128)], True)
                                finish(4 + c, sink_heads=(2 * c, 2 * c + 1))
                        sc.barrier()
                    with ExitStack() as pb:
                        sbp = lambda name, shape, dt=F32: pb.enter_context(nc.sbuf_tensor(UQ(name), list(shape), dt))
                        gtab = sbp("gtab", [128, D]); btab = sbp("btab", [128, D]); bgb = Buf("gb")
                        ld("sp", gtab[:], ln_g[L, 0:1, :].to_broadcast([128, D]), writes=[bgb])
                        ld("sp", btab[:], ln_b[L, 0:1, :].to_broadcast([128, D]), writes=[bgb])
                        rw = sbp("rw", [128, 8, NE]); brw = Buf("rw")
                        ld("sp", rw[:], router_w[L].rearrange("(k p) e -> p k e", p=128), writes=[brw])
                        rb = sbp("rb", [128, NE])
                        ld("sp", rb[:], router_b[L:L + 1, :].to_broadcast([128, NE]), writes=[brw])
                        wout = sbp("wout", [128, 8, D], BF16); bwout = Buf("wout")
                        w_out_src = (ev_w_out if L % 2 == 0 else od_w_out)[li]
                        for k in range(8):
                            ld("pool", wout[:, k, :], w_out_src[k * 128:(k + 1) * 128, :], writes=[bwout], max_dma_last_dim=4096)
                        lnt = {"st": sbp("st", [128, 2, 6]), "mv": sbp("mv", [128, 2]), "rstd": sbp("rstd", [128, 1]), "bst": Buf("st")}
                        zt = [sbp("zt%d" % i, [128, D]) for i in range(2)]; bzt = [Buf("zt%d" % i) for i in range(2)]
                        xres, bxres, x1t, bx1t = zt, bzt, zt, bzt
                        x1b = [sbp("x1b%d" % i, [128, D], BF16) for i in range(2)]; bx1b = [Buf("x1b%d" % i) for i in range(2)]
                        x1T = sbp("x1T", [128, 8, 128]); bx1T = Buf("x1T")
                        rsm = {k: sbp("r_" + k, [128, n]) for k, n in (("nmax", 1), ("ex", NE), ("msk", NE), ("ssum", 1))}
                        brs = Buf("rsm")
                        for tt in range(S // 128):
                            i = s * (S // 128) + tt
                            a = i % 2
                            ps = PS[a]; bps = bPS[a]

                            def f(h, ps=ps, tt=tt):
                                ins = None
                                for n2 in range(2):
                                    for k in range(8):
                                        ins = h.matmul(ps[:, n2 * 512:(n2 + 1) * 512], lhsT=catT[:, k, tt * 128:(tt + 1) * 128], rhs=wout[:, k, n2 * 512:(n2 + 1) * 512], start=(k == 0), stop=(k == 7))
                                return ins
                            sc.op("pe", f, reads=[bcat, bwout], writes=[bps])
                            ld("sp", xres[a][:], Xcur[i * 128:(i + 1) * 128, :], reads=[bXcur], writes=[bxres[a]])
                            sc.op("dve", lambda h, a=a, ps=ps: h.scalar_tensor_tensor(out=zt[a][:], in0=xres[a][:], scalar=ALPHA, in1=ps[:], op0=ALU.mult, op1=ALU.add), reads=[bxres[a], bps], writes=[bzt[a]])
                            layer_norm_tile(zt[a], bzt[a], gtab, btab, bgb, x1t[a], bx1t[a], lnt)
                            ld("sp", X1[i * 128:(i + 1) * 128, :], x1t[a][:], reads=[bx1t[a]], writes=[dX1])
                            sc.op("act", lambda h, a=a: h.activation(out=x1b[a][:], in_=x1t[a][:], func=AF.Copy), reads=[bx1t[a]], writes=[bx1b[a]])
                            ld("sp", XS[i * 128:(i + 1) * 128, :], x1b[a][:], reads=[bx1b[a]], writes=[dXS])
                            ps2 = PS[2 + a]; bps2 = bPS[2 + a]

                            def f2(h, a=a, ps2=ps2):
                                ins = None
                                for k in range(8):
                                    ins = h.transpose(out=ps2[:, k * 128:(k + 1) * 128], in_=x1t[a][:, k * 128:(k + 1) * 128], identity=ident[:])
                                return ins
                            sc.op("pe", f2, reads=[bx1t[a], bC], writes=[bps2])
                            sc.op("act", lambda h, ps2=ps2: h.activation(out=x1T[:].rearrange("p k t -> p (k t)"), in_=ps2[:], func=AF.Copy), reads=[bps2], writes=[bx1T])

                            def f3(h, ps2=ps2):
                                ins = None
                                for k in range(8):
                                    ins = h.matmul(ps2[:, 0:NE], lhsT=x1T[:, k, :], rhs=rw[:, k, :], start=(k == 0), stop=(k == 7))
                                return ins
                            sc.op("pe", f3, reads=[bx1T, brw], writes=[bps2])
                            Li = L_all[:, i, :]
                            sc.op("dve", lambda h, ps2=ps2, Li=Li: h.tensor_tensor(out=Li, in0=ps2[:, 0:NE], in1=rb[:], op=ALU.add), reads=[bps2, brw], writes=[bL])
                            m8 = M8_all[:, i, :]
                            sc.op("dve", lambda h, Li=Li, m8=m8: h.max(out=m8, in_=Li), reads=[bL], writes=[bM8])
                            sc.op("dve", lambda h, Li=Li, m8=m8: h.tensor_scalar(out=rsm["msk"][:], in0=Li, scalar1=m8[:, 3:4], scalar2=None, op0=ALU.is_ge), reads=[bL, bM8], writes=[brs])
                            sc.op("dve", lambda h, m8=m8: h.tensor_scalar(out=rsm["nmax"][:], in0=m8[:, 0:1], scalar1=-1.0, scalar2=None, op0=ALU.mult), reads=[bM8], writes=[brs])
                            sc.op("act", lambda h, Li=Li: h.activation(out=rsm["ex"][:], in_=Li, func=AF.Exp, bias=rsm["nmax"][:, 0:1]), reads=[bL, brs], writes=[brs])
                            sc.op("dve", lambda h: h.scalar_tensor_tensor(out=rsm["ex"][:], in0=rsm["ex"][:], scalar=1.0, in1=rsm["msk"][:], op0=ALU.mult, op1=ALU.mult, accum_out=rsm["ssum"][:]), reads=[brs], writes=[brs])
                            sc.op("dve", lambda h: h.reciprocal(out=rsm["ssum"][:], in_=rsm["ssum"][:]), reads=[brs], writes=[brs])
                            sc.op("dve", lambda h, i=i: h.tensor_scalar(out=G_all[:, i, :], in0=rsm["ex"][:], scalar1=rsm["ssum"][:, 0:1], scalar2=None, op0=ALU.mult), reads=[brs], writes=[bG])
                            sc.op("dve", lambda h, i=i: h.tensor_copy(out=M_all[:, i * NE:(i + 1) * NE], in_=rsm["msk"][:]), reads=[brs], writes=[bM])
                        sc.barrier()
            if stop_after == (L, "mix"):
                break

            with ExitStack() as p1b:
                sbp = lambda name, shape, dt=F32: p1b.enter_context(nc.sbuf_tensor(UQ(name), list(shape), dt))
                pos = sbp("pos", [128, NT, NE]); bpos = Buf("pos")
                carry = sbp("carry", [128, NT, NE]); bcar = Buf("carry")
                tot = sbp("tot", [128, NT, NE]); btot = Buf("tot")
                oh = sbp("oh", [128, NT, NE]); boh = Buf("oh")
                prod = sbp("prod", [128, NT, NE]); bprod = Buf("prod")
                slkf = sbp("slkf", [128, TOPK, NT]); bslkf = Buf("slkf")
                gk = sbp("gk", [128, TOPK, NT]); bgk = Buf("gk")
                sinit = sbp("sinit", [128, (NSLOT + 128) // 128, 2], I32); bsin = Buf("sinit")
                ecap = sbp("ecap", [128, NT * NE]); dumpt = sbp("dumpt", [128, NT * NE])
                ld("sp", ecap[:], cst["ecap"], writes=[bC])
                ld("sp", dumpt[:], cst["dump"], writes=[bC])
                ld("sp", sinit[:], cst["slotinit"], writes=[bsin])
                ld("sp", SLOT.rearrange("(j p) c -> p j c", p=128), sinit[:], reads=[bsin], writes=[dSLOT])

                def fW(h):
                    ins = None
                    for n2 in range(2):
                        ins = h.matmul(PS[0][:, n2 * 512:(n2 + 1) * 512], lhsT=ustrict_bf[:], rhs=M_all[:, n2 * 512:(n2 + 1) * 512], start=True, stop=True)
                    return ins
                sc.op("pe", fW, reads=[bC, bM], writes=[bPS[0]])

                def fT(h):
                    ins = None
                    for n2 in range(2):
                        ins = h.matmul(PS[1][:, n2 * 512:(n2 + 1) * 512], lhsT=ones_bf[:], rhs=M_all[:, n2 * 512:(n2 + 1) * 512], start=True, stop=True)
                    return ins
                sc.op("pe", fT, reads=[bC, bM], writes=[bPS[1]])
                sc.op("act", lambda h: h.activation(out=tot[:].rearrange("p a b -> p (a b)"), in_=PS[1][:], func=AF.Copy), reads=[bPS[1]], writes=[btot])
                sc.op("dve", lambda h: h.memset(carry[:, 0, :], 0.0), writes=[bcar])
                for i in range(1, NT):
                    sc.op("dve", lambda h, i=i: h.tensor_tensor(out=carry[:, i, :], in0=carry[:, i - 1, :], in1=tot[:, i - 1, :], op=ALU.add), reads=[bcar, btot], writes=[bcar])
                fl = lambda t: t[:].rearrange("p a b -> p (a b)")
                sc.op("dve", lambda h: h.tensor_tensor(out=fl(pos), in0=fl(carry), in1=PS[0][:], op=ALU.add), reads=[bcar, bPS[0]], writes=[bpos])
                sc.op("dve", lambda h: h.tensor_scalar(out=fl(oh), in0=fl(pos), scalar1=float(CAP) - 0.5, scalar2=None, op0=ALU.is_lt), reads=[bpos], writes=[boh])
                sc.op("dve", lambda h: h.tensor_tensor(out=fl(oh), in0=fl(oh), in1=M_all[:], op=ALU.mult), reads=[boh, bM], writes=[boh])
                sc.op("dve", lambda h: h.tensor_tensor(out=fl(pos), in0=fl(pos), in1=ecap[:], op=ALU.add), reads=[bpos, bC], writes=[bpos])
                sc.op("dve", lambda h: h.tensor_tensor(out=fl(pos), in0=fl(pos), in1=dumpt[:], op=ALU.subtract), reads=[bpos, bC], writes=[bpos])
                sc.op("dve", lambda h: h.tensor_tensor(out=fl(pos), in0=fl(pos), in1=fl(oh), op=ALU.mult), reads=[bpos, boh], writes=[bpos])
                sc.op("dve", lambda h: h.tensor_tensor(out=fl(pos), in0=fl(pos), in1=dumpt[:], op=ALU.add), reads=[bpos, bC], writes=[bpos])
                for k in range(TOPK):
                    sc.op("dve", lambda h, k=k: h.tensor_tensor(out=oh[:], in0=L_all[:], in1=M8_all[:, :, k:k + 1].to_broadcast([128, NT, NE]), op=ALU.is_equal), reads=[bL, bM8], writes=[boh])
                    sc.op("dve", lambda h: h.tensor_tensor(out=fl(prod), in0=fl(oh), in1=fl(pos), op=ALU.mult), reads=[boh, bpos], writes=[bprod])
                    sc.op("dve", lambda h, k=k: h.tensor_reduce(out=slkf[:, k, :], in_=prod[:], axis=AX.X, op=ALU.add), reads=[bprod], writes=[bslkf])
                    sc.op("dve", lambda h: h.tensor_tensor(out=fl(prod), in0=fl(oh), in1=fl(G_all), op=ALU.mult), reads=[boh, bG], writes=[bprod])
                    sc.op("dve", lambda h, k=k: h.tensor_reduce(out=gk[:, k, :], in_=prod[:], axis=AX.X, op=ALU.add), reads=[bprod], writes=[bgk])
                sc.op("dve", lambda h: h.tensor_copy(out=SLK[:], in_=slkf[:]), reads=[bslkf], writes=[bSLK])
                for k in range(TOPK):
                    sc.op("dve", lambda h, k=k: h.tensor_copy(out=PAY[:, :, k, 0], in_=tokidx[:]), reads=[bC], writes=[bPAY])
                    sc.op("dve", lambda h, k=k: h.tensor_copy(out=PAY[:, :, k, 1], in_=gk[:, k, :].bitcast(I32)), reads=[bgk], writes=[bPAY])
                for i in range(NT):
                    for k in range(TOPK):
                        sc.dma("pool", lambda h, i=i, k=k: h.indirect_dma_start(
                            out=SLOT, out_offset=bass.IndirectOffsetOnAxis(ap=SLK[:, k, i:i + 1], axis=0),
                            in_=PAY[:, i, k, :], in_offset=None), reads=[bSLK, bPAY, dSLOT], writes=[])
                sc.barrier()
            if stop_after == (L, "route"):
                break

            with ExitStack() as p2:
                sbp = lambda name, shape, dt=F32: p2.enter_context(nc.sbuf_tensor(UQ(name), list(shape), dt))
                wgu = [sbp("wgu%d" % i, [128, 8, 2048], BF16) for i in range(2)]; bwgu = [Buf("wgu%d" % i) for i in range(2)]
                wdn = [sbp("wdn%d" % i, [128, 8, D], BF16) for i in range(2)]; bwdn = [Buf("wdn%d" % i) for i in range(2)]
                braw = sbp("braw", [NE, 2048]); bbraw = Buf("braw")
                bgu = sbp("bgu", [128, 16, NE]); bbgu = Buf("bgu")
                idx = [sbp("idx%d" % i, [128, NJ, 2], I32) for i in range(3)]; bidx = [Buf("idx%d" % i) for i in range(3)]
                xg = [sbp("xg%d" % i, [128, D], BF16) for i in range(NJ)]; bxg = [Buf("xg%d" % i) for i in range(NJ)]
                xgT = [sbp("xgT%d" % i, [128, 8, CAP], BF16) for i in range(2)]; bxgT = [Buf("xgT%d" % i) for i in range(2)]
                actT = sbp("actT", [128, 8, CAP], BF16); bactT = Buf("actT")
                NH = CAP // 2
                eg = [sbp("eg%d" % i, [128, NH]) for i in range(4)]; beg = [Buf("eg%d" % i) for i in range(4)]
                esg = [sbp("esg%d" % i, [128, NH]) for i in range(4)]; besg = [Buf("esg%d" % i) for i in range(4)]
                el = [sbp("el%d" % i, [128, NH]) for i in range(4)]; bel = [Buf("el%d" % i) for i in range(4)]
                ysb = [sbp("ysb%d" % i, [128, D]) for i in range(2)]; bysb = [Buf("ysb%d" % i) for i in range(2)]

                ld("sp", braw[:], exp_b_gu[L], writes=[bbraw])

                def fb(h):
                    ins = None
                    for m in range(8):
                        for two in range(2):
                            c0 = (two * 8 + m) * NE
                            src = braw[:, :].rearrange("e (c two) -> e two c", two=2)[:, two, m * 128:(m + 1) * 128]
                            ins = h.transpose(out=PS[0][:, c0:c0 + NE], in_=src, identity=ident[0:NE, 0:NE])
                    return ins
                sc.op("pe", fb, reads=[bbraw, bC], writes=[bPS[0]])
                sc.op("act", lambda h: h.activation(out=bgu[:].rearrange("p a b -> p (a b)"), in_=PS[0][:, 0:16 * NE], func=AF.Copy), reads=[bPS[0]], writes=[bbgu])

                def load_w(e):
                    a = e % 2
                    for k in range(8):
                        ld("pool", wgu[a][:, k, :], exp_w_gu[L, e, k * 128:(k + 1) * 128, :], writes=[bwgu[a]], max_dma_last_dim=4096)
                    for k in range(8):
                        ld("pool", wdn[a][:, k, :], exp_w_down[L, e, k * 128:(k + 1) * 128, :], writes=[bwdn[a]], max_dma_last_dim=4096)

                def gather_issue(e):
                    a3 = e % 3
                    ld("sp", idx[a3][:], SLOT[e * CAP:(e + 1) * CAP, :].rearrange("(j p) c -> p j c", p=128), reads=[dSLOT], writes=[bidx[a3]])
                    for j in range(NJ):
                        sc.dma("pool", lambda h, a3=a3, j=j: h.indirect_dma_start(
                            out=xg[j][:], out_offset=None, in_=XS,
                            in_offset=bass.IndirectOffsetOnAxis(ap=idx[a3][:, j, 0:1].bitcast(U32), axis=0)),
                            reads=[bidx[a3], dXS], writes=[bxg[j]])

                tcount = [0]

                def transpose_x(e, j):
                    a = e % 2
                    pi = 2 + (tcount[0] % 2)
                    tcount[0] += 1
                    psb = PS[pi][:].bitcast(BF16)

                    def ft(h, psb=psb):
                        ins = None
                        for k in range(8):
                            ins = h.transpose(out=psb[:, k * 128:(k + 1) * 128], in_=xg[j][:, k * 128:(k + 1) * 128], identity=ident_bf[:])
                        return ins
                    sc.op("pe", ft, reads=[bxg[j], bC], writes=[bPS[pi]])
                    sc.op("act", lambda h, psb=psb: h.activation(out=xgT[a][:, :, j * 128:(j + 1) * 128], in_=psb[:, 0:1024].rearrange("p (k t) -> p k t", k=8), func=AF.Copy), reads=[bPS[pi]], writes=[bxgT[a]])

                gather_issue(0)
                load_w(0)
                for j in range(NJ):
                    transpose_x(0, j)
                ycount = [0]
                for e in range(NE):
                    a = e % 2
                    a3 = e % 3
                    if e + 1 < NE:
                        gather_issue(e + 1)
                        load_w(e + 1)
                    pending = []
                    pair = 0
                    for m in range(8):
                        for nh in range(2):
                            n0 = nh * NH
                            q = pair % 2
                            psg, bpsg = PS[0], bPSh[0][q]
                            psl, bpsl = PS[1], bPSh[1][q]

                            def fg(h, two, ps, m=m, n0=n0, q=q):
                                ins = None
                                wv = wgu[a][:].rearrange("p k (c two) -> p k two c", two=2)
                                for k in range(8):
                                    ins = h.matmul(ps[:, q * 512:q * 512 + NH], lhsT=wv[:, k, two, m * 128:(m + 1) * 128], rhs=xgT[a][:, k, n0:n0 + NH], start=(k == 0), stop=(k == 7))
                                return ins
                            ei = pair % 4
                            sc.op("pe", lambda h, fg=fg, psg=psg: fg(h, 0, psg), reads=[bwgu[a], bxgT[a]], writes=[bpsg])
                            sc.op("pe", lambda h, fg=fg, psl=psl: fg(h, 1, psl), reads=[bwgu[a], bxgT[a]], writes=[bpsl])
                            pg = psg[:, q * 512:q * 512 + NH]
                            pl = psl[:, q * 512:q * 512 + NH]
                            sc.op("dve", lambda h, pg=pg, ei=ei, m=m, e=e: h.tensor_scalar(out=eg[ei][:], in0=pg, scalar1=bgu[:, m, e:e + 1], scalar2=SW_LIMIT, op0=ALU.add, op1=ALU.min), reads=[bpsg, bbgu], writes=[beg[ei]])
                            sc.op("act", lambda h, pl=pl, ei=ei, m=m, e=e: h.activation(out=el[ei][:], in_=pl, func=AF.Identity, bias=bgu[:, 8 + m, e:e + 1]), reads=[bpsl, bbgu], writes=[bel[ei]])
                            sc.op("act", lambda h, ei=ei: h.activation(out=esg[ei][:], in_=eg[ei][:], func=AF.Sigmoid, scale=SW_ALPHA), reads=[beg[ei]], writes=[besg[ei]])
                            sc.op("pool", lambda h, ei=ei: h.tensor_tensor(out=eg[ei][:], in0=eg[ei][:], in1=esg[ei][:], op=ALU.mult), reads=[beg[ei], besg[ei]], writes=[beg[ei]])
                            for fn in pending:
                                fn()
                            pending = []

                            def fin(ei=ei, m=m, n0=n0):
                                sc.op("dve", lambda h: h.tensor_scalar(out=el[ei][:], in0=el[ei][:], scalar1=SW_LIMIT, scalar2=-SW_LIMIT, op0=ALU.min, op1=ALU.max), reads=[bel[ei]], writes=[bel[ei]])
                                sc.op("dve", lambda h: h.scalar_tensor_tensor(out=actT[:, m, n0:n0 + NH], in0=el[ei][:], scalar=1.0, in1=eg[ei][:], op0=ALU.add, op1=ALU.mult), reads=[beg[ei], bel[ei]], writes=[bactT])
                            pending.append(fin)
                            if e + 1 < NE and pair in (3, 5, 7, 9, 11, 13):
                                transpose_x(e + 1, (pair - 3) // 2)
                            pair += 1
                    for fn in pending:
                        fn()
                    for j in range(NJ):
                        pi = 2 + (ycount[0] % 2)
                        yb = ycount[0] % 2
                        ycount[0] += 1
                        ps, bps = PS[pi], bPS[pi]

                        def fd(h, ps=ps, j=j):
                            ins = None
                            for n2 in range(2):
                                for k in range(8):
                                    ins = h.matmul(ps[:, n2 * 512:(n2 + 1) * 512], lhsT=actT[:, k, j * 128:(j + 1) * 128], rhs=wdn[a][:, k, n2 * 512:(n2 + 1) * 512], start=(k == 0), stop=(k == 7))
                            return ins
                        sc.op("pe", fd, reads=[bactT, bwdn[a]], writes=[bps])
                        sc.op("act", lambda h, ps=ps, yb=yb, j=j: h.activation(out=ysb[yb][:], in_=ps[:], func=AF.Copy, scale=idx[a3][:, j, 1:2].bitcast(F32)), reads=[bps, bidx[a3]], writes=[bysb[yb]])
                        ld("sp", YS[e * CAP + j * 128:e * CAP + (j + 1) * 128, :], ysb[yb][:], reads=[bysb[yb]], writes=[dYS])
                sc.barrier()
            if stop_after == (L, "moe"):
                break

            with ExitStack() as p3:
                sbp = lambda name, shape, dt=F32: p3.enter_context(nc.sbuf_tensor(UQ(name), list(shape), dt))
                gtab = sbp("gtab2", [128, D]); btab = sbp("btab2", [128, D]); bgb = Buf("gb2")
                ld("sp", gtab[:], ln_g[L, 1:2, :].to_broadcast([128, D]), writes=[bgb])
                ld("sp", btab[:], ln_b[L, 1:2, :].to_broadcast([128, D]), writes=[bgb])
                bd = sbp("bd", [NE, D]); bbd = Buf("bd")
                ld("sp", bd[:], exp_b_down[L], writes=[bbd])
                lnt = {"st": sbp("st2", [128, 2, 6]), "mv": sbp("mv2", [128, 2]), "rstd": sbp("rstd2", [128, 1]), "bst": Buf("st2")}
                yk = [[sbp("yk%d_%d" % (a, k), [128, D]) for k in range(TOPK)] for a in range(2)]
                byk = [[Buf("yk%d_%d" % (a, k)) for k in range(TOPK)] for a in range(2)]
                x1r = [sbp("x1r%d" % a, [128, D]) for a in range(2)]; bx1r = [Buf("x1r%d" % a) for a in range(2)]
                zt = [sbp("z2_%d" % a, [128, D]) for a in range(2)]; bzt = [Buf("z2_%d" % a) for a in range(2)]
                x2t = [sbp("x2t%d" % a, [128, D]) for a in range(2)]; bx2t = [Buf("x2t%d" % a) for a in range(2)]
                xtb = [sbp("xtb%d" % a, [128, 8, 128], BF16) for a in range(2)]; bxtb = [Buf("xtb%d" % a) for a in range(2)]
                GT = sbp("GT", [NE, 128]); bGT = Buf("GT")
                Xnext = y_out if last else XA
                dXn = dY if last else dXA
                for i in range(NT):
                    a = i % 2
                    for k in range(TOPK):
                        sc.dma("pool", lambda h, a=a, k=k, i=i: h.indirect_dma_start(
                            out=yk[a][k][:], out_offset=None, in_=YS,
                            in_offset=bass.IndirectOffsetOnAxis(ap=SLK[:, k, i:i + 1], axis=0)),
                            reads=[bSLK, dYS], writes=[byk[a][k]])
                    ld("sp", x1r[a][:], X1[i * 128:(i + 1) * 128, :], reads=[dX1], writes=[bx1r[a]])
                    ps, bps = PS[a], bPS[a]
                    sc.op("pe", lambda h, ps=ps, i=i: h.transpose(out=ps[0:NE, 0:128], in_=G_all[:, i, :], identity=ident[:]), reads=[bG, bC], writes=[bps])
                    sc.op("act", lambda h, ps=ps: h.activation(out=GT[:], in_=ps[0:NE, 0:128], func=AF.Copy), reads=[bps], writes=[bGT])

                    def fgb(h, ps=ps):
                        ins = None
                        for n2 in range(2):
                            ins = h.matmul(ps[:, n2 * 512:(n2 + 1) * 512], lhsT=GT[:], rhs=bd[:, n2 * 512:(n2 + 1) * 512], start=True, stop=True)
                        return ins
                    sc.op("pe", fgb, reads=[bGT, bbd], writes=[bps])
                    sc.op("dve", lambda h, a=a, ps=ps: h.scalar_tensor_tensor(out=zt[a][:], in0=x1r[a][:], scalar=ALPHA, in1=ps[:], op0=ALU.mult, op1=ALU.add), reads=[bx1r[a], bps], writes=[bzt[a]])
                    sc.op("pool", lambda h, a=a: h.tensor_tensor(out=yk[a][0][:], in0=yk[a][0][:], in1=yk[a][1][:], op=ALU.add), reads=[byk[a][0], byk[a][1]], writes=[byk[a][0]])
                    sc.op("pool", lambda h, a=a: h.tensor_tensor(out=yk[a][2][:], in0=yk[a][2][:], in1=yk[a][3][:], op=ALU.add), reads=[byk[a][2], byk[a][3]], writes=[byk[a][2]])
                    sc.op("dve", lambda h, a=a: h.tensor_tensor(out=zt[a][:], in0=zt[a][:], in1=yk[a][0][:], op=ALU.add), reads=[bzt[a], byk[a][0]], writes=[bzt[a]])
                    sc.op("dve", lambda h, a=a: h.tensor_tensor(out=zt[a][:], in0=zt[a][:], in1=yk[a][2][:], op=ALU.add), reads=[bzt[a], byk[a][2]], writes=[bzt[a]])
                    layer_norm_tile(zt[a], bzt[a], gtab, btab, bgb, x2t[a], bx2t[a], lnt)
                    ld("sp", Xnext[i * 128:(i + 1) * 128, :], x2t[a][:], reads=[bx2t[a]], writes=[dXn])
                    if not last:
                        transpose_to_XT(x2t[a], bx2t[a], i, PS[2 + a], bPS[2 + a], xtb[a], bxtb[a])
                sc.barrier()
            Xcur = XA
            bXcur = dXA
        sc.barrier()
    return nc


def kernel(**inputs):
    global CONSTS
    if CONSTS is None:
        CONSTS = _consts()
    nc = build_nc()
    x = np.ascontiguousarray(inputs["x"], dtype=np.float32).reshape(NCORES, T, D)
    shared = {k: np.ascontiguousarray(v) for k, v in inputs.items() if k != "x"}
    for k, v in CONSTS.items():
        shared["c_" + k] = v
    in_maps = []
    for c in range(NCORES):
        m = dict(shared)
        m["x"] = x[c]
        in_maps.append(m)
    res = run_bass_kernel_spmd(nc, in_maps, core_ids=list(range(NCORES)))
    out = np.stack([np.asarray(r["y"]) for r in res.results], axis=0)
    return out.reshape(16, S, D).astype(np.float32)
```

```python
import numpy as np
from contextlib import ExitStack
import concourse.bass as bass
import concourse.mybir as mybir
from concourse.bass_utils import run_bass_kernel_spmd

F32 = mybir.dt.float32
BF16 = mybir.dt.bfloat16
I32 = mybir.dt.int32
U32 = mybir.dt.uint32
AF = mybir.ActivationFunctionType
ALU = mybir.AluOpType
AX = mybir.AxisListType

NCORES = 8
D = 1024
S = 2048
NSEQ = 2
T = NSEQ * S
NT = T // 128
DEPTH = 4
NE = 32
TOPK = 4
CAP = 768
NJ = CAP // 128
NSLOT = NE * CAP
ALPHA = float((2 * DEPTH) ** 0.25)
LN_EPS = 1e-5
PAD = 8
SW_LIMIT = 7.0
SW_ALPHA = 1.702
POOL_WINDOWS = (2, 4, 8, 16)
C_PATTERNS = ((128, 1), (512, 4), (2048, 16))


class Buf:
    __slots__ = ("name", "w", "r")

    def __init__(self, name):
        self.name = name
        self.w = None
        self.r = {}


class Sched:
    ENGS = ("pe", "act", "dve", "pool", "sp")

    def __init__(self, nc, es, n_lanes=40):
        self.nc = nc
        self.h = {"pe": nc.tensor, "act": nc.scalar, "dve": nc.vector, "pool": nc.gpsimd, "sp": nc.sync}
        self.sem = {e: es.enter_context(nc.semaphore("s_" + e)) for e in self.ENGS}
        self.cnt = {e: 0 for e in self.ENGS}
        self.known = {e: {} for e in self.ENGS}
        self.n_lanes = n_lanes
        self.lsem = [es.enter_context(nc.semaphore("l%d" % i)) for i in range(n_lanes)]
        self.lcnt = [0] * n_lanes
        self.next_lane = 0
        self.next_sw = 0
        self.n_hw = 24
        self.nwait = 0

    def _semof(self, key):
        return self.lsem[key[1]] if isinstance(key, tuple) else self.sem[key]

    def _need(self, e, ev, waits):
        if ev is None:
            return
        key, val = ev
        if key == e and e == "pe":
            return
        if self.known[e].get(key, 0) >= val:
            return
        if waits.get(key, 0) < val:
            waits[key] = val

    @staticmethod
    def _flat(bs):
        out = []
        for b in bs:
            if isinstance(b, (list, tuple)):
                out.extend(Sched._flat(b))
            else:
                out.append(b)
        return out

    def _deps(self, e, reads, writes):
        waits = {}
        for b in reads:
            self._need(e, b.w, waits)
        for b in writes:
            self._need(e, b.w, waits)
            for k, v in b.r.items():
                self._need(e, (k, v), waits)
        return waits

    def _emit_waits(self, e, waits):
        h = self.h[e]
        for k, v in waits.items():
            h.wait_ge(self._semof(k), v)
            self.known[e][k] = v
            self.nwait += 1

    def op(self, e, fn, reads=(), writes=()):
        reads = self._flat(reads); writes = self._flat(writes)
        waits = self._deps(e, reads, writes)
        self._emit_waits(e, waits)
        ins = fn(self.h[e])
        ins.then_inc(self.sem[e], 1)
        self.cnt[e] += 1
        v = self.cnt[e]
        for b in reads:
            b.r[e] = v
        for b in writes:
            b.w = (e, v)
            b.r = {}

    def dma(self, q, fn, reads=(), writes=()):
        if q == "pool":
            lane = self.n_hw + self.next_sw
            self.next_sw = (self.next_sw + 1) % (self.n_lanes - self.n_hw)
        else:
            lane = self.next_lane
            self.next_lane = (lane + 1) % self.n_hw
        key = ("L", lane)
        reads = self._flat(reads); writes = self._flat(writes)
        waits = self._deps(q, reads, writes)
        self._need(q, (key, self.lcnt[lane]), waits)
        self._emit_waits(q, waits)
        ins = fn(self.h[q])
        ins.then_inc(self.lsem[lane], 16)
        self.lcnt[lane] += 16
        v = self.lcnt[lane]
        for b in reads:
            b.r[key] = v
        for b in writes:
            b.w = (key, v)
            b.r = {}

    def barrier(self):
        for e in self.ENGS:
            waits = {}
            for o in self.ENGS:
                if o != e:
                    self._need(e, (o, self.cnt[o]), waits)
            for i in range(self.n_lanes):
                self._need(e, (("L", i), self.lcnt[i]), waits)
            self._emit_waits(e, waits)


def _consts():
    c = {}
    c["ident"] = np.eye(128, dtype=np.float32)
    c["ustrict"] = np.triu(np.ones((128, 128), np.float32), 1)
    c["ones"] = np.ones((128, 128), np.float32)
    rc = np.ones((4, 16), np.float32)
    for g, w in enumerate(POOL_WINDOWS):
        for t in range(8):
            lo = max(t - w // 2, 0)
            hi = min(t + w - w // 2, S)
            rc[g, t] = 1.0 / (hi - lo)
            tt = S - 8 + t
            lo = max(tt - w // 2, 0)
            hi = min(tt + w - w // 2, S)
            rc[g, 8 + t] = 1.0 / (hi - lo)
    c["poolrc"] = np.broadcast_to(rc.reshape(1, 64), (128, 64)).copy()
    ecap = (np.arange(NE, dtype=np.float32) * CAP)
    c["ecap"] = np.broadcast_to(np.tile(ecap, NT).reshape(1, NT * NE), (128, NT * NE)).copy()
    c["tokidx"] = (np.arange(NT, dtype=np.int32)[None, :] * 128 + np.arange(128, dtype=np.int32)[:, None]).astype(np.int32)
    c["dump"] = np.broadcast_to((NSLOT + np.arange(128, dtype=np.float32))[:, None], (128, NT * NE)).copy()
    init = np.zeros((128, (NSLOT + 128) // 128, 2), np.int32)
    init[:, :, 0] = T
    c["slotinit"] = init
    slopes = np.array([2.0 ** (-8.0 * (i + 1) / 8) for i in range(8)], np.float64)
    kk = np.arange(128)[:, None]
    cc = np.arange(384)[None, :]
    dist = np.abs(cc - 128 - kk)
    ed = np.zeros((128, 8, 384), np.float32)
    for h in range(8):
        ed[:, h, :] = np.where(dist <= 128, np.exp(-slopes[h] * dist), 0.0)
    c["edec_d"] = ed
    cc = np.arange(256)[None, :]
    dist = np.abs(cc - 64 - kk)
    ec = np.zeros((128, 3, 8, 256), np.float32)
    for p, (_, dil) in enumerate(C_PATTERNS):
        for h in range(8):
            ec[:, p, h, :] = np.where(dist <= 64, np.exp(-slopes[h] * dist * dil), 0.0)
    c["edec_c"] = ec
    return c


CONSTS = None


def build_nc(layers=tuple(range(DEPTH)), debug=False, stop_after=None):
    nc = bass.Bass("TRN2", target_bir_lowering=False)
    dram = {}
    _uid = [0]

    def UQ(name):
        _uid[0] += 1
        return "%s_u%d" % (name, _uid[0])

    def din(name, shape, dt=F32):
        dram[name] = nc.dram_tensor(name, list(shape), dt, kind="ExternalInput").ap()
        return dram[name]

    x_in = din("x", [T, D])
    ev_w_in = din("ev_w_in", [2, D, 2048])
    ev_pool_w = din("ev_pool_w", [2, 4, 128, 128])
    ev_pool_scale = din("ev_pool_scale", [2, 512])
    ev_conv_w = din("ev_conv_w", [2, 3, 512])
    ev_w_out = din("ev_w_out", [2, 1024, 1024])
    od_w_in = din("od_w_in", [2, D, 2304])
    od_sink = din("od_sink", [2, 8])
    od_w_out = din("od_w_out", [2, 1024, 1024])
    router_w = din("router_w", [DEPTH, D, NE])
    router_b = din("router_b", [DEPTH, NE])
    exp_w_gu = din("exp_w_gu", [DEPTH, NE, D, 2048])
    exp_b_gu = din("exp_b_gu", [DEPTH, NE, 2048])
    exp_w_down = din("exp_w_down", [DEPTH, NE, 1024, D])
    exp_b_down = din("exp_b_down", [DEPTH, NE, D])
    ln_g = din("ln_g", [DEPTH, 2, D])
    ln_b = din("ln_b", [DEPTH, 2, D])
    cst = {}
    for k, v in CONSTS.items():
        cst[k] = din("c_" + k, v.shape, I32 if v.dtype == np.int32 else F32)

    y_out = nc.dram_tensor("y", [T, D], F32, kind="ExternalOutput").ap()
    okind = "ExternalOutput" if debug else "Internal"
    XA = nc.dram_tensor("XA", [T, D], F32, kind=okind).ap()
    X1 = nc.dram_tensor("X1", [T, D], F32, kind=okind).ap()
    XT = nc.dram_tensor("XT", [D, T], BF16, kind="Internal").ap()
    XS = nc.dram_tensor("XS", [T + 128, D], BF16, kind="Internal").ap()
    SLOT = nc.dram_tensor("SLOT", [NSLOT + 128, 2], I32, kind="Internal").ap()
    YS = nc.dram_tensor("YS", [NSLOT + 128, D], F32, kind="Internal").ap()
    dXA, dX1, dXT, dXS, dSLOT, dYS = (Buf(n) for n in ("XA", "X1", "XT", "XS", "SLOT", "YS"))
    dY = Buf("Y")

    with ExitStack() as es:
        sc = Sched(nc, es)
        sb = lambda name, shape, dt=F32: es.enter_context(nc.sbuf_tensor(UQ(name), list(shape), dt))

        ident = sb("ident", [128, 128])
        ident_bf = sb("ident_bf", [128, 128], BF16)
        ustrict_bf = sb("ustrict_bf", [128, 128], BF16)
        ones_bf = sb("ones_bf", [128, 128], BF16)
        ctmp = sb("ctmp", [128, 128])
        poolrc = sb("poolrc", [128, 64])
        tokidx = sb("tokidx", [128, NT], I32)
        G_all = sb("G_all", [128, NT, NE])
        L_all = sb("L_all", [128, NT, NE])
        M8_all = sb("M8_all", [128, NT, 8])
        M_all = sb("M_all", [128, NT * NE], BF16)
        SLK = sb("SLK", [128, TOPK, NT], U32)
        PAY = sb("PAY", [128, NT, TOPK, 2], I32)
        zrow = sb("zrow", [128, D], BF16)
        bC = Buf("consts")
        bG, bL, bM8, bM, bSLK, bPAY = (Buf(n) for n in ("G", "L", "M8", "M", "SLK", "PAY"))

        PS = [es.enter_context(nc.psum_tensor("ps%d" % i, [128, 1024], F32)) for i in range(4)]
        bPSh = [[Buf("ps%d_%d" % (i, hh)) for hh in range(2)] for i in range(4)]

        class _BP:
            def __getitem__(self, i):
                return _BPi(i)

        class _BPi(list):
            def __init__(self, i):
                super().__init__(bPSh[i])
        bPS = _BP()

        def ld(q, out_ap, in_ap, reads=(), writes=(), **kw):
            sc.dma(q, lambda h: h.dma_start(out=out_ap, in_=in_ap, **kw), reads=reads, writes=writes)

        ld("sp", ident[:], cst["ident"], writes=[bC])
        ld("sp", poolrc[:], cst["poolrc"], writes=[bC])
        ld("sp", tokidx[:], cst["tokidx"], writes=[bC])
        ld("pool", ident_bf[:], cst["ident"], writes=[bC])
        ld("pool", ustrict_bf[:], cst["ustrict"], writes=[bC])
        ld("pool", ones_bf[:], cst["ones"], writes=[bC])
        sc.op("dve", lambda h: h.memset(zrow[:], 0.0), writes=[bC])
        ld("sp", XS[T:T + 128, :], zrow[:], reads=[bC], writes=[dXS])
        with nc.sbuf_tensor(UQ("zf"), [128, D], F32) as zf:
            bzf = Buf("zf")
            sc.op("dve", lambda h: h.memset(zf[:], 0.0), writes=[bzf])
            ld("sp", YS[NSLOT:NSLOT + 128, :], zf[:], reads=[bzf], writes=[dYS])
            sc.barrier()

        def layer_norm_tile(z, bz, gt, bt_, bgb, out, bout, tmp):
            st, mv, rstd = tmp["st"], tmp["mv"], tmp["rstd"]
            bst = tmp["bst"]
            sc.op("dve", lambda h: h.bn_stats(out=st[:, 0, :], in_=z[:, 0:512]), reads=[bz], writes=[bst])
            sc.op("dve", lambda h: h.bn_stats(out=st[:, 1, :], in_=z[:, 512:1024]), reads=[bz], writes=[bst])
            sc.op("dve", lambda h: h.bn_aggr(out=mv[:], in_=st[:].rearrange("p a b -> p (a b)")), reads=[bst], writes=[bst])
            sc.op("dve", lambda h: h.tensor_scalar(out=rstd[:], in0=mv[:, 1:2], scalar1=LN_EPS, scalar2=None, op0=ALU.add), reads=[bst], writes=[bst])
            sc.op("act", lambda h: h.activation(out=rstd[:], in_=rstd[:], func=AF.Sqrt), reads=[bst], writes=[bst])
            sc.op("dve", lambda h: h.reciprocal(out=rstd[:], in_=rstd[:]), reads=[bst], writes=[bst])
            sc.op("dve", lambda h: h.tensor_scalar(out=z[:], in0=z[:], scalar1=mv[:, 0:1], scalar2=rstd[:, 0:1], op0=ALU.subtract, op1=ALU.mult), reads=[bz, bst], writes=[bz])
            sc.op("pool", lambda h: h.tensor_tensor(out=z[:], in0=z[:], in1=gt[:], op=ALU.mult), reads=[bz, bgb], writes=[bz])
            sc.op("pool", lambda h: h.tensor_tensor(out=out[:], in0=z[:], in1=bt_[:], op=ALU.add), reads=[bz, bgb], writes=[bout])

        def transpose_to_XT(xt_tile, bxt, i, ps, bps, xtb, bxtb):
            def f(h):
                ins = None
                for k in range(8):
                    ins = h.transpose(out=ps[:, k * 128:(k + 1) * 128], in_=xt_tile[:, k * 128:(k + 1) * 128], identity=ident[:])
                return ins
            sc.op("pe", f, reads=[bxt, bC], writes=[bps])
            sc.op("act", lambda h: h.activation(out=xtb[:].rearrange("p k t -> p (k t)"), in_=ps[:], func=AF.Copy), reads=[bps], writes=[bxtb])
            ld("sp", XT.rearrange("(k p) t -> p k t", p=128)[:, :, i * 128:(i + 1) * 128], xtb[:], reads=[bxtb], writes=[])

        with ExitStack() as p0:
            xts = [p0.enter_context(nc.sbuf_tensor(UQ("p0x%d" % i), [128, D], F32)) for i in range(2)]
            bxts = [Buf("p0x%d" % i) for i in range(2)]
            xtbs = [p0.enter_context(nc.sbuf_tensor(UQ("p0b%d" % i), [128, 8, 128], BF16)) for i in range(2)]
            bxtbs = [Buf("p0b%d" % i) for i in range(2)]
            for i in range(NT):
                a = i % 2
                ld("sp", xts[a][:], x_in[i * 128:(i + 1) * 128, :], writes=[bxts[a]])
                transpose_to_XT(xts[a], bxts[a], i, PS[a], bPS[a], xtbs[a], bxtbs[a])
            sc.barrier()

        Xcur = x_in
        bXcur = Buf("xin")

        for L in layers:
            li = L // 2
            last = (L == layers[-1])
            with ExitStack() as p1:
                sbq = lambda name, shape, dt=F32: p1.enter_context(nc.sbuf_tensor(UQ(name), list(shape), dt))
                catT = sbq("catT", [128, 8, S], BF16); bcat = Buf("catT")
                for s in range(NSEQ):
                    t0 = s * S
                    with ExitStack() as pa:
                        sbp = lambda name, shape, dt=F32: pa.enter_context(nc.sbuf_tensor(UQ(name), list(shape), dt))
                        xT = sbp("xT", [128, 8, S], BF16); bxT = [Buf("xT%d" % k) for k in range(8)]
                        for k in range(8):
                            ld("sp", xT[:, k, :], XT[k * 128:(k + 1) * 128, t0:t0 + S], reads=[dXT], writes=[bxT[k]])
                        pcnt = [0]
                        if L % 2 == 0:
                            win = sbp("win", [128, 8, 2048], BF16); bwin = [Buf("win%d" % k) for k in range(8)]
                            for k in range(8):
                                ld("pool", win[:, k, :], ev_w_in[li, k * 128:(k + 1) * 128, :], writes=[bwin[k]], max_dma_last_dim=4096)
                            poolw = sbp("poolw", [128, 4, 128], BF16); bpw = Buf("poolw")
                            ld("pool", poolw[:], ev_pool_w[li].rearrange("g c d -> c g d"), writes=[bpw])
                            pscale = sbp("pscale", [128, 4]); convw = sbp("convw", [128, 3, 4])
                            ld("sp", pscale[:], ev_pool_scale[li].rearrange("(g p) -> p g", p=128), writes=[bpw], allow_slow_non_contiguous=True)
                            ld("sp", convw[:], ev_conv_w[li].rearrange("k (m p) -> p k m", p=128), writes=[bpw], allow_slow_non_contiguous=True)
                            WB = [sbp("wb%d" % i, [128, S + 2 * PAD]) for i in range(4)]
                            bWB = [Buf("wb%d" % i) for i in range(4)]
                            plb = sbp("plb", [128, S], BF16); bplb = Buf("plb")
                            etmp = sbp("etmp", [128, 16]); betmp = Buf("etmp")
                            for i in range(4):
                                sc.op("dve", lambda h, i=i: h.memset(WB[i][:], 0.0), writes=[bWB[i]])

                            def proj(fc, evac):
                                for tc in range(4):
                                    pi = pcnt[0] % 4
                                    pcnt[0] += 1
                                    ps = PS[pi]; bps = bPSh[pi][0]

                                    def f(h, tc=tc, ps=ps):
                                        ins = None
                                        for k in range(8):
                                            ins = h.matmul(ps[:, 0:512], lhsT=win[:, k, fc * 128:(fc + 1) * 128], rhs=xT[:, k, tc * 512:(tc + 1) * 512], start=(k == 0), stop=(k == 7))
                                        return ins
                                    sc.op("pe", f, reads=[bwin, bxT], writes=[bps])
                                    evac(tc, ps[:, 0:512], bps)

                            for g, w in enumerate(POOL_WINDOWS):
                                U, A, B = WB[0], WB[1], WB[2]
                                bU, bA, bB = bWB[0], bWB[1], bWB[2]
                                proj(g, lambda tc, pa_, bps: sc.op("act", lambda h: h.activation(out=U[:, PAD + tc * 512:PAD + (tc + 1) * 512], in_=pa_, func=AF.Copy), reads=[bps], writes=[bU]))
                                lo, n = PAD - 7, S + 14
                                sc.op("dve", lambda h: h.tensor_tensor(out=A[:, lo:lo + n], in0=U[:, lo - 1:lo - 1 + n], in1=U[:, lo:lo + n], op=ALU.add), reads=[bU], writes=[bA])
                                cur, bcur, oth, both = A, bA, B, bB
                                ext = 7
                                ww = 2
                                while ww < w:
                                    sh = ww // 2
                                    ext = ext - sh
                                    lo, n = PAD - ext, S + 2 * ext
                                    sc.op("dve", lambda h, cur=cur, oth=oth, lo=lo, n=n, sh=sh: h.tensor_tensor(out=oth[:, lo:lo + n], in0=cur[:, lo - sh:lo - sh + n], in1=cur[:, lo + sh:lo + sh + n], op=ALU.add), reads=[bcur], writes=[both])
                                    cur, bcur, oth, both = oth, both, cur, bcur
                                    ww *= 2
                                sc.op("dve", lambda h, cur=cur: h.scalar_tensor_tensor(out=plb[:], in0=cur[:, PAD:PAD + S], scalar=1.0 / w, in1=U[:, PAD:PAD + S], op0=ALU.mult, op1=ALU.subtract), reads=[bcur, bU], writes=[bplb])
                                sc.op("dve", lambda h, cur=cur: h.tensor_tensor(out=etmp[:, 0:8], in0=cur[:, PAD:PAD + 8], in1=poolrc[:, g * 16:g * 16 + 8], op=ALU.mult), reads=[bcur, bC], writes=[betmp])
                                sc.op("dve", lambda h, cur=cur: h.tensor_tensor(out=etmp[:, 8:16], in0=cur[:, PAD + S - 8:PAD + S], in1=poolrc[:, g * 16 + 8:g * 16 + 16], op=ALU.mult), reads=[bcur, bC], writes=[betmp])
                                sc.op("dve", lambda h: h.tensor_tensor(out=plb[:, 0:8], in0=etmp[:, 0:8], in1=U[:, PAD:PAD + 8], op=ALU.subtract), reads=[betmp, bU], writes=[bplb])
                                sc.op("dve", lambda h: h.tensor_tensor(out=plb[:, S - 8:S], in0=etmp[:, 8:16], in1=U[:, PAD + S - 8:PAD + S], op=ALU.subtract), reads=[betmp, bU], writes=[bplb])
                                for tc in range(4):
                                    pi = pcnt[0] % 4
                                    pcnt[0] += 1
                                    ps = PS[pi]; bps = bPSh[pi][0]
                                    sc.op("pe", lambda h, ps=ps, tc=tc: h.matmul(ps[:, 0:512], lhsT=poolw[:, g, :], rhs=plb[:, tc * 512:(tc + 1) * 512], start=True, stop=True), reads=[bpw, bplb], writes=[bps])
                                    sc.op("act", lambda h, ps=ps, tc=tc: h.activation(out=catT[:, g, tc * 512:(tc + 1) * 512], in_=ps[:, 0:512], func=AF.Copy, scale=pscale[:, g:g + 1]), reads=[bps, bpw], writes=[bcat])
                            for m in range(4):
                                Cb, U, A = WB[0], WB[3], WB[1]
                                bCb, bU, bA = bWB[0], bWB[3], bWB[1]
                                proj(8 + m, lambda tc, pa_, bps: sc.op("act", lambda h: h.activation(out=Cb[:, PAD + tc * 512:PAD + (tc + 1) * 512], in_=pa_, func=AF.Copy), reads=[bps], writes=[bCb]))
                                proj(12 + m, lambda tc, pa_, bps: sc.op("dve", lambda h: h.tensor_tensor(out=U[:, PAD + tc * 512:PAD + (tc + 1) * 512], in0=Cb[:, PAD + tc * 512:PAD + (tc + 1) * 512], in1=pa_, op=ALU.mult), reads=[bps, bCb], writes=[bU]))
                                sc.op("dve", lambda h: h.tensor_scalar(out=A[:, PAD:PAD + S], in0=U[:, PAD - 1:PAD - 1 + S], scalar1=convw[:, 0, m:m + 1], scalar2=None, op0=ALU.mult), reads=[bU, bpw], writes=[bA])
                                sc.op("dve", lambda h: h.scalar_tensor_tensor(out=A[:, PAD:PAD + S], in0=U[:, PAD:PAD + S], scalar=convw[:, 1, m:m + 1], in1=A[:, PAD:PAD + S], op0=ALU.mult, op1=ALU.add), reads=[bU, bpw, bA], writes=[bA])
                                sc.op("dve", lambda h: h.scalar_tensor_tensor(out=A[:, PAD:PAD + S], in0=U[:, PAD + 1:PAD + 1 + S], scalar=convw[:, 2, m:m + 1], in1=A[:, PAD:PAD + S], op0=ALU.mult, op1=ALU.add), reads=[bU, bpw, bA], writes=[bA])
                                proj(4 + m, lambda tc, pa_, bps: sc.op("dve", lambda h: h.tensor_tensor(out=catT[:, 4 + m, tc * 512:(tc + 1) * 512], in0=A[:, PAD + tc * 512:PAD + (tc + 1) * 512], in1=pa_, op=ALU.mult), reads=[bps, bA], writes=[bcat]))
                        else:
                            win = sbp("win", [128, 8, 2304], BF16); bwin = [Buf("win%d" % k) for k in range(8)]
                            for k in range(8):
                                ld("pool", win[:, k, :], od_w_in[li, k * 128:(k + 1) * 128, :], writes=[bwin[k]], max_dma_last_dim=4096)
                            qT = sbp("qT", [128, S], BF16); bqT = Buf("qT")
                            kT = sbp("kT", [128, S], BF16); bkT = Buf("kT")
                            VV = sbp("VV", [128, 48, 128], BF16); bVV = Buf("VV")
                            PT = [sbp("PT%d" % i, [128, 16, 384], BF16) for i in range(2)]; bPT = [Buf("PT%d" % i) for i in range(2)]
                            Pr = [sbp("Pr%d" % i, [128, 384], BF16) for i in range(2)]; bPr = [Buf("Pr%d" % i) for i in range(2)]
                            acc_o = sbp("acc_o", [128, S]); bacc_o = Buf("acc_o")
                            acc_d = sbp("acc_d", [128, S]); bacc_d = Buf("acc_d")
                            etab = sbp("etab", [128, 3, 2, 384]); betab = Buf("etab")
                            esink = sbp("esink", [128, 8]); besink = Buf("esink")
                            ld("sp", esink[:], od_sink[li:li + 1, :].to_broadcast([128, 8]), writes=[besink])
                            sc.op("act", lambda h: h.activation(out=esink[:], in_=esink[:], func=AF.Exp), reads=[besink], writes=[besink])
                            ptc = [0]
                            sct = [0]

                            def projT(col0, dst, bdst, dup=False):
                                for tc in range(4):
                                    pi = pcnt[0] % 4
                                    pcnt[0] += 1
                                    ps = PS[pi // 2]; hh = pi % 2; bps = bPSh[pi // 2][hh]

                                    def f(h, tc=tc, ps=ps, hh=hh):
                                        ins = None
                                        for k in range(8):
                                            if not dup:
                                                ins = h.matmul(ps[:, hh * 512:(hh + 1) * 512], lhsT=win[:, k, col0:col0 + 128], rhs=xT[:, k, tc * 512:(tc + 1) * 512], start=(k == 0), stop=(k == 7))
                                            else:
                                                for half in range(2):
                                                    ins = h.matmul(ps[half * 64:(half + 1) * 64, hh * 512:(hh + 1) * 512], lhsT=win[:, k, col0:col0 + 64], rhs=xT[:, k, tc * 512:(tc + 1) * 512], start=(k == 0), stop=(k == 7))
                                        return ins
                                    sc.op("pe", f, reads=[bwin, bxT], writes=[bps])
                                    sc.op("act", lambda h, ps=ps, hh=hh, tc=tc: h.activation(out=dst[:, tc * 512:(tc + 1) * 512], in_=ps[:, hh * 512:(hh + 1) * 512], func=AF.Copy), reads=[bps], writes=[bdst])

                            def projV(col0, tiles):
                                for (vi, tstart, tstep) in tiles:
                                    pi = pcnt[0] % 4
                                    pcnt[0] += 1
                                    ps = PS[pi // 2]; hh = pi % 2; bps = bPSh[pi // 2][hh]

                                    def f(h, ps=ps, hh=hh, tstart=tstart, tstep=tstep):
                                        ins = None
                                        for k in range(8):
                                            ins = h.matmul(ps[:, hh * 512:hh * 512 + 128], lhsT=xT[:, k, tstart:tstart + 127 * tstep + 1:tstep], rhs=win[:, k, col0:col0 + 128], start=(k == 0), stop=(k == 7))
                                        return ins
                                    sc.op("pe", f, reads=[bwin, bxT], writes=[bps])
                                    sc.op("act", lambda h, ps=ps, hh=hh, vi=vi: h.activation(out=VV[:, vi, :], in_=ps[:, hh * 512:hh * 512 + 128], func=AF.Copy), reads=[bps], writes=[bVV])

                            def attend(hb, vcol, tabsel, patterns, first):
                                for pidx, (dil, vvb, W, rad) in enumerate(patterns):
                                    Ls = S // dil
                                    ntile = Ls // 128
                                    pt = PT[ptc[0] % 2]; bpt = bPT[ptc[0] % 2]
                                    ptc[0] += 1
                                    tb = etab[:, tabsel[pidx], hb // 64, :]
                                    for r in range(dil):
                                        for j in range(ntile):
                                            q0 = 128 * j - rad
                                            c_lo = max(0, -q0)
                                            c_hi = min(W, Ls - q0)
                                            nq = c_hi - c_lo
                                            si = sct[0] % 4
                                            sct[0] += 1
                                            ps = PS[si // 2]; hh = si % 2; bps = bPSh[si // 2][hh]
                                            kst = r + dil * (128 * j)
                                            qst = r + dil * (q0 + c_lo)
                                            sc.op("pe", lambda h, ps=ps, hh=hh, kst=kst, qst=qst, nq=nq: h.matmul(
                                                ps[:, hh * 512:hh * 512 + nq],
                                                lhsT=kT[hb:hb + 64, kst:kst + 127 * dil + 1:dil],
                                                rhs=qT[hb:hb + 64, qst:qst + (nq - 1) * dil + 1:dil], start=True, stop=True),
                                                reads=[bkT, bqT], writes=[bps])
                                            pr = Pr[si % 2]; bpr = bPr[si % 2]
                                            sc.op("act", lambda h, ps=ps, hh=hh, nq=nq, pr=pr: h.activation(out=pr[:, 0:nq], in_=ps[:, hh * 512:hh * 512 + nq], func=AF.Exp, scale=0.125), reads=[bps], writes=[bpr])
                                            ti = r * ntile + j
                                            sc.op("dve", lambda h, pr=pr, nq=nq, c_lo=c_lo, ti=ti: h.tensor_tensor(out=pt[:, ti, c_lo:c_lo + nq], in0=pr[:, 0:nq], in1=tb[:, c_lo:c_lo + nq], op=ALU.mult), reads=[bpr, betab], writes=[bpt])
                                    QB = rad
                                    nqb = 512 // QB
                                    for ch in range(S // 512):
                                        hh = ch % 2
                                        pso, bpso = PS[2], bPSh[2][hh]
                                        psd, bpsd = PS[3], bPSh[3][hh]

                                        def fpv(h, which, ps, ch=ch, hh=hh):
                                            ins = None
                                            for b in range(nqb):
                                                gq = ch * 512 + b * QB
                                                r = gq // Ls
                                                l0 = gq % Ls
                                                parts = []
                                                for j in range(ntile):
                                                    k_lo = max(128 * j, l0 - rad)
                                                    k_hi = min(128 * j + 128, l0 + QB + rad)
                                                    if k_hi <= k_lo:
                                                        continue
                                                    parts.append((j, k_lo - 128 * j, k_hi - 128 * j))
                                                parts = [(j, 0, 128) for (j, _a, _b) in parts]
                                                for n_, (j, p_lo, p_hi) in enumerate(parts):
                                                    ti = r * ntile + j
                                                    col = l0 - (128 * j - rad)
                                                    if which == 0:
                                                        lhsT = VV[p_lo:p_hi, vvb + ti, vcol:vcol + 64]
                                                    else:
                                                        lhsT = ones_bf[p_lo:p_hi, 0:64]
                                                    ins = h.matmul(ps[hb:hb + 64, hh * 512 + b * QB:hh * 512 + (b + 1) * QB], lhsT=lhsT,
                                                                   rhs=pt[p_lo:p_hi, ti, col:col + QB], start=(n_ == 0), stop=(n_ == len(parts) - 1))
                                            return ins
                                        sc.op("pe", lambda h, pso=pso: fpv(h, 0, pso), reads=[bVV, bpt], writes=[bpso])
                                        sc.op("pe", lambda h, psd=psd: fpv(h, 1, psd), reads=[bC, bpt], writes=[bpsd])
                                        if dil == 1:
                                            dso = acc_o[hb:hb + 64, ch * 512:(ch + 1) * 512]
                                            dsd = acc_d[hb:hb + 64, ch * 512:(ch + 1) * 512]
                                            srco = pso[hb:hb + 64, hh * 512:(hh + 1) * 512]
                                            srcd = psd[hb:hb + 64, hh * 512:(hh + 1) * 512]
                                        else:
                                            nr = 512 // Ls if Ls < 512 else 1
                                            if Ls >= 512:
                                                r = (ch * 512) // Ls
                                                l0 = (ch * 512) % Ls
                                                st_ = r + dil * l0
                                                dso = acc_o[hb:hb + 64, st_:st_ + 511 * dil + 1:dil]
                                                dsd = acc_d[hb:hb + 64, st_:st_ + 511 * dil + 1:dil]
                                                srco = pso[hb:hb + 64, hh * 512:(hh + 1) * 512]
                                                srcd = psd[hb:hb + 64, hh * 512:(hh + 1) * 512]
                                            else:
                                                r0 = (ch * 512) // Ls
                                                dso = acc_o[hb:hb + 64, :].rearrange("p (l r) -> p r l", r=dil)[:, r0:r0 + nr, :]
                                                dsd = acc_d[hb:hb + 64, :].rearrange("p (l r) -> p r l", r=dil)[:, r0:r0 + nr, :]
                                                srco = pso[hb:hb + 64, hh * 512:(hh + 1) * 512].rearrange("p (r l) -> p r l", r=nr)
                                                srcd = psd[hb:hb + 64, hh * 512:(hh + 1) * 512].rearrange("p (r l) -> p r l", r=nr)
                                        if first and pidx == 0:
                                            sc.op("act", lambda h, dso=dso, srco=srco: h.activation(out=dso, in_=srco, func=AF.Copy), reads=[bpso], writes=[bacc_o])
                                            sc.op("act", lambda h, dsd=dsd, srcd=srcd: h.activation(out=dsd, in_=srcd, func=AF.Copy), reads=[bpsd], writes=[bacc_d])
                                        else:
                                            sc.op("dve", lambda h, dso=dso, srco=srco: h.tensor_tensor(out=dso, in0=dso, in1=srco, op=ALU.add), reads=[bpso, bacc_o], writes=[bacc_o])
                                            sc.op("dve", lambda h, dsd=dsd, srcd=srcd: h.tensor_tensor(out=dsd, in0=dsd, in1=srcd, op=ALU.add), reads=[bpsd, bacc_d], writes=[bacc_d])

                            def finish(chunk, sink_heads=None):
                                if sink_heads is not None:
                                    for half, hd_ in enumerate(sink_heads):
                                        sc.op("dve", lambda h, half=half, hd_=hd_: h.tensor_scalar(out=acc_d[half * 64:(half + 1) * 64, :], in0=acc_d[half * 64:(half + 1) * 64, :], scalar1=esink[half * 64:(half + 1) * 64, hd_:hd_ + 1], scalar2=None, op0=ALU.add), reads=[bacc_d, besink], writes=[bacc_d])
                                sc.op("dve", lambda h: h.reciprocal(out=acc_d[:], in_=acc_d[:]), reads=[bacc_d], writes=[bacc_d])
                                sc.op("pool", lambda h: h.tensor_tensor(out=catT[:, chunk, :], in0=acc_o[:], in1=acc_d[:], op=ALU.mult), reads=[bacc_o, bacc_d], writes=[bcat])

                            for c in range(4):
                                ld("sp", etab[:, :, :, 0:256], cst["edec_c"][:, :, 2 * c:2 * c + 2, :], writes=[betab])
                                projT(0 + c * 128, qT, bqT)
                                projT(512 + c * 128, kT, bkT)
                                tiles = []
                                vvb = {}
                                vi = 0
                                for (_, dil) in C_PATTERNS:
                                    vvb[dil] = vi
                                    Ls = S // dil
                                    for r in range(dil):
                                        for j in range(Ls // 128):
                                            tiles.append((vi, r + dil * 128 * j, dil))
                                            vi += 1
                                projV(1024 + c * 128, tiles)
                                pats = [(dil, vvb[dil], 256, 64) for (_, dil) in C_PATTERNS]
                                for half in range(2):
                                    attend(half * 64, half * 64, [0, 1, 2], pats, True)
                                finish(c)
                            for c in range(4):
                                g = c // 2
                                ld("sp", etab[:, 0, :, :], cst["edec_d"][:, 2 * c:2 * c + 2, :], writes=[betab])
                                projT(1536 + c * 128, qT, bqT)
                                projT(2048 + g * 64, kT, bkT, dup=True)
                                projV(2176, [(j, 128 * j, 1) for j in range(16)])
                                for half in range(2):
                                    attend(half * 64, g * 64, [0], [(1, 0, 384, 128)], True)
                                finish(4 + c, sink_heads=(2 * c, 2 * c + 1))
                        sc.barrier()
                    with ExitStack() as pb:
                        sbp = lambda name, shape, dt=F32: pb.enter_context(nc.sbuf_tensor(UQ(name), list(shape), dt))
                        gtab = sbp("gtab", [128, D]); btab = sbp("btab", [128, D]); bgb = Buf("gb")
                        ld("sp", gtab[:], ln_g[L, 0:1, :].to_broadcast([128, D]), writes=[bgb])
                        ld("sp", btab[:], ln_b[L, 0:1, :].to_broadcast([128, D]), writes=[bgb])
                        rw = sbp("rw", [128, 8, NE]); brw = Buf("rw")
                        ld("sp", rw[:], router_w[L].rearrange("(k p) e -> p k e", p=128), writes=[brw])
                        rb = sbp("rb", [128, NE])
                        ld("sp", rb[:], router_b[L:L + 1, :].to_broadcast([128, NE]), writes=[brw])
                        wout = sbp("wout", [128, 8, D], BF16); bwout = [Buf("wout%d" % k) for k in range(8)]
                        w_out_src = (ev_w_out if L % 2 == 0 else od_w_out)[li]
                        for k in range(8):
                            ld("pool", wout[:, k, :], w_out_src[k * 128:(k + 1) * 128, :], writes=[bwout[k]], max_dma_last_dim=4096)
                        lnt = {"st": sbp("st", [128, 2, 6]), "mv": sbp("mv", [128, 2]), "rstd": sbp("rstd", [128, 1]), "bst": Buf("st")}
                        zt = [sbp("zt%d" % i, [128, D]) for i in range(2)]; bzt = [Buf("zt%d" % i) for i in range(2)]
                        xres, bxres, x1t, bx1t = zt, bzt, zt, bzt
                        x1b = [sbp("x1b%d" % i, [128, D], BF16) for i in range(2)]; bx1b = [Buf("x1b%d" % i) for i in range(2)]
                        x1T = sbp("x1T", [128, 8, 128]); bx1T = Buf("x1T")
                        rsm = {k: sbp("r_" + k, [128, n]) for k, n in (("nmax", 1), ("ex", NE), ("msk", NE), ("ssum", 1))}
                        brs = Buf("rsm")
                        for tt in range(S // 128):
                            i = s * (S // 128) + tt
                            a = i % 2
                            ps = PS[a]; bps = bPS[a]

                            def f(h, ps=ps, tt=tt):
                                ins = None
                                for n2 in range(2):
                                    for k in range(8):
                                        ins = h.matmul(ps[:, n2 * 512:(n2 + 1) * 512], lhsT=catT[:, k, tt * 128:(tt + 1) * 128], rhs=wout[:, k, n2 * 512:(n2 + 1) * 512], start=(k == 0), stop=(k == 7))
                                return ins
                            sc.op("pe", f, reads=[bcat, bwout], writes=[bps])
                            ld("sp", xres[a][:], Xcur[i * 128:(i + 1) * 128, :], reads=[bXcur], writes=[bxres[a]])
                            sc.op("dve", lambda h, a=a, ps=ps: h.scalar_tensor_tensor(out=zt[a][:], in0=xres[a][:], scalar=ALPHA, in1=ps[:], op0=ALU.mult, op1=ALU.add), reads=[bxres[a], bps], writes=[bzt[a]])
                            layer_norm_tile(zt[a], bzt[a], gtab, btab, bgb, x1t[a], bx1t[a], lnt)
                            ld("sp", X1[i * 128:(i + 1) * 128, :], x1t[a][:], reads=[bx1t[a]], writes=[])
                            sc.op("act", lambda h, a=a: h.activation(out=x1b[a][:], in_=x1t[a][:], func=AF.Copy), reads=[bx1t[a]], writes=[bx1b[a]])
                            ld("sp", XS[i * 128:(i + 1) * 128, :], x1b[a][:], reads=[bx1b[a]], writes=[])
                            ps2 = PS[2 + a]; bps2 = bPS[2 + a]

                            def f2(h, a=a, ps2=ps2):
                                ins = None
                                for k in range(8):
                                    ins = h.transpose(out=ps2[:, k * 128:(k + 1) * 128], in_=x1t[a][:, k * 128:(k + 1) * 128], identity=ident[:])
                                return ins
                            sc.op("pe", f2, reads=[bx1t[a], bC], writes=[bps2])
                            sc.op("act", lambda h, ps2=ps2: h.activation(out=x1T[:].rearrange("p k t -> p (k t)"), in_=ps2[:], func=AF.Copy), reads=[bps2], writes=[bx1T])

                            def f3(h, ps2=ps2):
                                ins = None
                                for k in range(8):
                                    ins = h.matmul(ps2[:, 0:NE], lhsT=x1T[:, k, :], rhs=rw[:, k, :], start=(k == 0), stop=(k == 7))
                                return ins
                            sc.op("pe", f3, reads=[bx1T, brw], writes=[bps2])
                            Li = L_all[:, i, :]
                            sc.op("dve", lambda h, ps2=ps2, Li=Li: h.tensor_tensor(out=Li, in0=ps2[:, 0:NE], in1=rb[:], op=ALU.add), reads=[bps2, brw], writes=[bL])
                            m8 = M8_all[:, i, :]
                            sc.op("dve", lambda h, Li=Li, m8=m8: h.max(out=m8, in_=Li), reads=[bL], writes=[bM8])
                            sc.op("dve", lambda h, Li=Li, m8=m8: h.tensor_scalar(out=rsm["msk"][:], in0=Li, scalar1=m8[:, 3:4], scalar2=None, op0=ALU.is_ge), reads=[bL, bM8], writes=[brs])
                            sc.op("dve", lambda h, m8=m8: h.tensor_scalar(out=rsm["nmax"][:], in0=m8[:, 0:1], scalar1=-1.0, scalar2=None, op0=ALU.mult), reads=[bM8], writes=[brs])
                            sc.op("act", lambda h, Li=Li: h.activation(out=rsm["ex"][:], in_=Li, func=AF.Exp, bias=rsm["nmax"][:, 0:1]), reads=[bL, brs], writes=[brs])
                            sc.op("dve", lambda h: h.scalar_tensor_tensor(out=rsm["ex"][:], in0=rsm["ex"][:], scalar=1.0, in1=rsm["msk"][:], op0=ALU.mult, op1=ALU.mult, accum_out=rsm["ssum"][:]), reads=[brs], writes=[brs])
                            sc.op("dve", lambda h: h.reciprocal(out=rsm["ssum"][:], in_=rsm["ssum"][:]), reads=[brs], writes=[brs])
                            sc.op("dve", lambda h, i=i: h.tensor_scalar(out=G_all[:, i, :], in0=rsm["ex"][:], scalar1=rsm["ssum"][:, 0:1], scalar2=None, op0=ALU.mult), reads=[brs], writes=[bG])
                            sc.op("dve", lambda h, i=i: h.tensor_copy(out=M_all[:, i * NE:(i + 1) * NE], in_=rsm["msk"][:]), reads=[brs], writes=[bM])
                        sc.barrier()
            if stop_after == (L, "mix"):
                break

            with ExitStack() as p1b:
                sbp = lambda name, shape, dt=F32: p1b.enter_context(nc.sbuf_tensor(UQ(name), list(shape), dt))
                pos = sbp("pos", [128, NT, NE]); bpos = Buf("pos")
                carry = sbp("carry", [128, NT, NE]); bcar = Buf("carry")
                tot = sbp("tot", [128, NT, NE]); btot = Buf("tot")
                oh = sbp("oh", [128, NT, NE]); boh = Buf("oh")
                prod = sbp("prod", [128, NT, NE]); bprod = Buf("prod")
                slkf = sbp("slkf", [128, TOPK, NT]); bslkf = Buf("slkf")
                gk = sbp("gk", [128, TOPK, NT]); bgk = Buf("gk")
                sinit = sbp("sinit", [128, (NSLOT + 128) // 128, 2], I32); bsin = Buf("sinit")
                ecap = sbp("ecap", [128, NT * NE]); dumpt = sbp("dumpt", [128, NT * NE])
                ld("sp", ecap[:], cst["ecap"], writes=[bC])
                ld("sp", dumpt[:], cst["dump"], writes=[bC])
                ld("sp", sinit[:], cst["slotinit"], writes=[bsin])
                ld("sp", SLOT.rearrange("(j p) c -> p j c", p=128), sinit[:], reads=[bsin], writes=[dSLOT])

                def fW(h):
                    ins = None
                    for n2 in range(2):
                        ins = h.matmul(PS[0][:, n2 * 512:(n2 + 1) * 512], lhsT=ustrict_bf[:], rhs=M_all[:, n2 * 512:(n2 + 1) * 512], start=True, stop=True)
                    return ins
                sc.op("pe", fW, reads=[bC, bM], writes=[bPS[0]])

                def fT(h):
                    ins = None
                    for n2 in range(2):
                        ins = h.matmul(PS[1][:, n2 * 512:(n2 + 1) * 512], lhsT=ones_bf[:], rhs=M_all[:, n2 * 512:(n2 + 1) * 512], start=True, stop=True)
                    return ins
                sc.op("pe", fT, reads=[bC, bM], writes=[bPS[1]])
                sc.op("act", lambda h: h.activation(out=tot[:].rearrange("p a b -> p (a b)"), in_=PS[1][:], func=AF.Copy), reads=[bPS[1]], writes=[btot])
                sc.op("dve", lambda h: h.memset(carry[:, 0, :], 0.0), writes=[bcar])
                for i in range(1, NT):
                    sc.op("dve", lambda h, i=i: h.tensor_tensor(out=carry[:, i, :], in0=carry[:, i - 1, :], in1=tot[:, i - 1, :], op=ALU.add), reads=[bcar, btot], writes=[bcar])
                fl = lambda t: t[:].rearrange("p a b -> p (a b)")
                sc.op("dve", lambda h: h.tensor_tensor(out=fl(pos), in0=fl(carry), in1=PS[0][:], op=ALU.add), reads=[bcar, bPS[0]], writes=[bpos])
                sc.op("dve", lambda h: h.tensor_scalar(out=fl(oh), in0=fl(pos), scalar1=float(CAP) - 0.5, scalar2=None, op0=ALU.is_lt), reads=[bpos], writes=[boh])
                sc.op("dve", lambda h: h.tensor_tensor(out=fl(oh), in0=fl(oh), in1=M_all[:], op=ALU.mult), reads=[boh, bM], writes=[boh])
                sc.op("dve", lambda h: h.tensor_tensor(out=fl(pos), in0=fl(pos), in1=ecap[:], op=ALU.add), reads=[bpos, bC], writes=[bpos])
                sc.op("dve", lambda h: h.tensor_tensor(out=fl(pos), in0=fl(pos), in1=dumpt[:], op=ALU.subtract), reads=[bpos, bC], writes=[bpos])
                sc.op("dve", lambda h: h.tensor_tensor(out=fl(pos), in0=fl(pos), in1=fl(oh), op=ALU.mult), reads=[bpos, boh], writes=[bpos])
                sc.op("dve", lambda h: h.tensor_tensor(out=fl(pos), in0=fl(pos), in1=dumpt[:], op=ALU.add), reads=[bpos, bC], writes=[bpos])
                for k in range(TOPK):
                    sc.op("dve", lambda h, k=k: h.tensor_tensor(out=oh[:], in0=L_all[:], in1=M8_all[:, :, k:k + 1].to_broadcast([128, NT, NE]), op=ALU.is_equal), reads=[bL, bM8], writes=[boh])
                    sc.op("dve", lambda h: h.tensor_tensor(out=fl(prod), in0=fl(oh), in1=fl(pos), op=ALU.mult), reads=[boh, bpos], writes=[bprod])
                    sc.op("dve", lambda h, k=k: h.tensor_reduce(out=slkf[:, k, :], in_=prod[:], axis=AX.X, op=ALU.add), reads=[bprod], writes=[bslkf])
                    sc.op("dve", lambda h: h.tensor_tensor(out=fl(prod), in0=fl(oh), in1=fl(G_all), op=ALU.mult), reads=[boh, bG], writes=[bprod])
                    sc.op("dve", lambda h, k=k: h.tensor_reduce(out=gk[:, k, :], in_=prod[:], axis=AX.X, op=ALU.add), reads=[bprod], writes=[bgk])
                sc.op("dve", lambda h: h.tensor_copy(out=SLK[:], in_=slkf[:]), reads=[bslkf], writes=[bSLK])
                for k in range(TOPK):
                    sc.op("dve", lambda h, k=k: h.tensor_copy(out=PAY[:, :, k, 0], in_=tokidx[:]), reads=[bC], writes=[bPAY])
                    sc.op("dve", lambda h, k=k: h.tensor_copy(out=PAY[:, :, k, 1], in_=gk[:, k, :].bitcast(I32)), reads=[bgk], writes=[bPAY])
                for i in range(NT):
                    for k in range(TOPK):
                        sc.dma("pool", lambda h, i=i, k=k: h.indirect_dma_start(
                            out=SLOT, out_offset=bass.IndirectOffsetOnAxis(ap=SLK[:, k, i:i + 1], axis=0),
                            in_=PAY[:, i, k, :], in_offset=None), reads=[bSLK, bPAY, dSLOT], writes=[])
                sc.barrier()
            if stop_after == (L, "route"):
                break

            with ExitStack() as p2:
                sbp = lambda name, shape, dt=F32: p2.enter_context(nc.sbuf_tensor(UQ(name), list(shape), dt))
                wgu = [sbp("wgu%d" % i, [128, 8, 2048], BF16) for i in range(2)]; bwgu = [[Buf("wgu%d_%d" % (i, k)) for k in range(8)] for i in range(2)]
                wdn = [sbp("wdn%d" % i, [128, 8, D], BF16) for i in range(2)]; bwdn = [[Buf("wdn%d_%d" % (i, k)) for k in range(8)] for i in range(2)]
                braw = sbp("braw", [NE, 2048]); bbraw = Buf("braw")
                bgu = sbp("bgu", [128, 16, NE]); bbgu = Buf("bgu")
                idx = [sbp("idx%d" % i, [128, NJ, 2], I32) for i in range(3)]; bidx = [Buf("idx%d" % i) for i in range(3)]
                xg = [sbp("xg%d" % i, [128, D], BF16) for i in range(NJ)]; bxg = [Buf("xg%d" % i) for i in range(NJ)]
                xgT = [sbp("xgT%d" % i, [128, 8, CAP], BF16) for i in range(2)]; bxgT = [Buf("xgT%d" % i) for i in range(2)]
                actT = sbp("actT", [128, 8, CAP], BF16); bactT = Buf("actT")
                NH = CAP // 2
                eg = [sbp("eg%d" % i, [128, NH]) for i in range(4)]; beg = [Buf("eg%d" % i) for i in range(4)]
                esg = [sbp("esg%d" % i, [128, NH]) for i in range(4)]; besg = [Buf("esg%d" % i) for i in range(4)]
                el = [sbp("el%d" % i, [128, NH]) for i in range(4)]; bel = [Buf("el%d" % i) for i in range(4)]
                ysb = [sbp("ysb%d" % i, [128, D]) for i in range(2)]; bysb = [Buf("ysb%d" % i) for i in range(2)]

                ld("sp", braw[:], exp_b_gu[L], writes=[bbraw])

                def fb(h):
                    ins = None
                    for m in range(8):
                        for two in range(2):
                            c0 = (two * 8 + m) * NE
                            src = braw[:, :].rearrange("e (c two) -> e two c", two=2)[:, two, m * 128:(m + 1) * 128]
                            ins = h.transpose(out=PS[0][:, c0:c0 + NE], in_=src, identity=ident[0:NE, 0:NE])
                    return ins
                sc.op("pe", fb, reads=[bbraw, bC], writes=[bPS[0]])
                sc.op("act", lambda h: h.activation(out=bgu[:].rearrange("p a b -> p (a b)"), in_=PS[0][:, 0:16 * NE], func=AF.Copy), reads=[bPS[0]], writes=[bbgu])

                def load_w(e):
                    a = e % 2
                    for k in range(8):
                        ld("pool", wgu[a][:, k, :], exp_w_gu[L, e, k * 128:(k + 1) * 128, :], writes=[bwgu[a][k]], max_dma_last_dim=4096)
                    for k in range(8):
                        ld("pool", wdn[a][:, k, :], exp_w_down[L, e, k * 128:(k + 1) * 128, :], writes=[bwdn[a][k]], max_dma_last_dim=4096)

                def gather_issue(e):
                    a3 = e % 3
                    ld("sp", idx[a3][:], SLOT[e * CAP:(e + 1) * CAP, :].rearrange("(j p) c -> p j c", p=128), reads=[dSLOT], writes=[bidx[a3]])
                    for j in range(NJ):
                        sc.dma("pool", lambda h, a3=a3, j=j: h.indirect_dma_start(
                            out=xg[j][:], out_offset=None, in_=XS,
                            in_offset=bass.IndirectOffsetOnAxis(ap=idx[a3][:, j, 0:1].bitcast(U32), axis=0)),
                            reads=[bidx[a3], dXS], writes=[bxg[j]])

                tcount = [0]

                def transpose_x(e, j):
                    a = e % 2
                    pi = 2 + (tcount[0] % 2)
                    tcount[0] += 1
                    psb = PS[pi][:].bitcast(BF16)

                    def ft(h, psb=psb):
                        ins = None
                        for k in range(8):
                            ins = h.transpose(out=psb[:, k * 128:(k + 1) * 128], in_=xg[j][:, k * 128:(k + 1) * 128], identity=ident_bf[:])
                        return ins
                    sc.op("pe", ft, reads=[bxg[j], bC], writes=[bPS[pi]])
                    sc.op("act", lambda h, psb=psb: h.activation(out=xgT[a][:, :, j * 128:(j + 1) * 128], in_=psb[:, 0:1024].rearrange("p (k t) -> p k t", k=8), func=AF.Copy), reads=[bPS[pi]], writes=[bxgT[a]])

                gather_issue(0)
                load_w(0)
                for j in range(NJ):
                    transpose_x(0, j)
                ycount = [0]
                for e in range(NE):
                    a = e % 2
                    a3 = e % 3
                    if e + 1 < NE:
                        gather_issue(e + 1)
                        load_w(e + 1)
                    pending = []
                    pair = 0
                    for m in range(8):
                        for nh in range(2):
                            n0 = nh * NH
                            q = pair % 2
                            psg, bpsg = PS[0], bPSh[0][q]
                            psl, bpsl = PS[1], bPSh[1][q]

                            def fg(h, two, ps, m=m, n0=n0, q=q):
                                ins = None
                                wv = wgu[a][:].rearrange("p k (c two) -> p k two c", two=2)
                                for k in range(8):
                                    ins = h.matmul(ps[:, q * 512:q * 512 + NH], lhsT=wv[:, k, two, m * 128:(m + 1) * 128], rhs=xgT[a][:, k, n0:n0 + NH], start=(k == 0), stop=(k == 7))
                                return ins
                            ei = pair % 4
                            sc.op("pe", lambda h, fg=fg, psg=psg: fg(h, 0, psg), reads=[bwgu[a], bxgT[a]], writes=[bpsg])
                            sc.op("pe", lambda h, fg=fg, psl=psl: fg(h, 1, psl), reads=[bwgu[a], bxgT[a]], writes=[bpsl])
                            pg = psg[:, q * 512:q * 512 + NH]
                            pl = psl[:, q * 512:q * 512 + NH]
                            sc.op("dve", lambda h, pg=pg, ei=ei, m=m, e=e: h.tensor_scalar(out=eg[ei][:], in0=pg, scalar1=bgu[:, m, e:e + 1], scalar2=SW_LIMIT, op0=ALU.add, op1=ALU.min), reads=[bpsg, bbgu], writes=[beg[ei]])
                            sc.op("act", lambda h, pl=pl, ei=ei, m=m, e=e: h.activation(out=el[ei][:], in_=pl, func=AF.Identity, bias=bgu[:, 8 + m, e:e + 1]), reads=[bpsl, bbgu], writes=[bel[ei]])
                            sc.op("act", lambda h, ei=ei: h.activation(out=esg[ei][:], in_=eg[ei][:], func=AF.Sigmoid, scale=SW_ALPHA), reads=[beg[ei]], writes=[besg[ei]])
                            sc.op("pool", lambda h, ei=ei: h.tensor_tensor(out=eg[ei][:], in0=eg[ei][:], in1=esg[ei][:], op=ALU.mult), reads=[beg[ei], besg[ei]], writes=[beg[ei]])
                            for fn in pending:
                                fn()
                            pending = []

                            def fin(ei=ei, m=m, n0=n0):
                                sc.op("dve", lambda h: h.tensor_scalar(out=el[ei][:], in0=el[ei][:], scalar1=SW_LIMIT, scalar2=-SW_LIMIT, op0=ALU.min, op1=ALU.max), reads=[bel[ei]], writes=[bel[ei]])
                                sc.op("dve", lambda h: h.scalar_tensor_tensor(out=actT[:, m, n0:n0 + NH], in0=el[ei][:], scalar=1.0, in1=eg[ei][:], op0=ALU.add, op1=ALU.mult), reads=[beg[ei], bel[ei]], writes=[bactT])
                            pending.append(fin)
                            if e + 1 < NE and pair in (3, 5, 7, 9, 11, 13):
                                transpose_x(e + 1, (pair - 3) // 2)
                            pair += 1
                    for fn in pending:
                        fn()
                    for j in range(NJ):
                        pi = 2 + (ycount[0] % 2)
                        yb = ycount[0] % 2
                        ycount[0] += 1
                        ps, bps = PS[pi], bPS[pi]

                        def fd(h, ps=ps, j=j):
                            ins = None
                            for n2 in range(2):
                                for k in range(8):
                                    ins = h.matmul(ps[:, n2 * 512:(n2 + 1) * 512], lhsT=actT[:, k, j * 128:(j + 1) * 128], rhs=wdn[a][:, k, n2 * 512:(n2 + 1) * 512], start=(k == 0), stop=(k == 7))
                            return ins
                        sc.op("pe", fd, reads=[bactT, bwdn[a]], writes=[bps])
                        sc.op("act", lambda h, ps=ps, yb=yb, j=j: h.activation(out=ysb[yb][:], in_=ps[:], func=AF.Copy, scale=idx[a3][:, j, 1:2].bitcast(F32)), reads=[bps, bidx[a3]], writes=[bysb[yb]])
                        ld("sp", YS[e * CAP + j * 128:e * CAP + (j + 1) * 128, :], ysb[yb][:], reads=[bysb[yb]], writes=[])
                sc.barrier()
            if stop_after == (L, "moe"):
                break

            with ExitStack() as p3:
                sbp = lambda name, shape, dt=F32: p3.enter_context(nc.sbuf_tensor(UQ(name), list(shape), dt))
                gtab = sbp("gtab2", [128, D]); btab = sbp("btab2", [128, D]); bgb = Buf("gb2")
                ld("sp", gtab[:], ln_g[L, 1:2, :].to_broadcast([128, D]), writes=[bgb])
                ld("sp", btab[:], ln_b[L, 1:2, :].to_broadcast([128, D]), writes=[bgb])
                bd = sbp("bd", [NE, D]); bbd = Buf("bd")
                ld("sp", bd[:], exp_b_down[L], writes=[bbd])
                lnt = {"st": sbp("st2", [128, 2, 6]), "mv": sbp("mv2", [128, 2]), "rstd": sbp("rstd2", [128, 1]), "bst": Buf("st2")}
                yk = [[sbp("yk%d_%d" % (a, k), [128, D]) for k in range(TOPK)] for a in range(2)]
                byk = [[Buf("yk%d_%d" % (a, k)) for k in range(TOPK)] for a in range(2)]
                x1r = [sbp("x1r%d" % a, [128, D]) for a in range(2)]; bx1r = [Buf("x1r%d" % a) for a in range(2)]
                zt = [sbp("z2_%d" % a, [128, D]) for a in range(2)]; bzt = [Buf("z2_%d" % a) for a in range(2)]
                x2t = [sbp("x2t%d" % a, [128, D]) for a in range(2)]; bx2t = [Buf("x2t%d" % a) for a in range(2)]
                xtb = [sbp("xtb%d" % a, [128, 8, 128], BF16) for a in range(2)]; bxtb = [Buf("xtb%d" % a) for a in range(2)]
                GT = sbp("GT", [NE, 128]); bGT = Buf("GT")
                Xnext = y_out if last else XA
                dXn = dY if last else dXA
                for i in range(NT):
                    a = i % 2
                    for k in range(TOPK):
                        sc.dma("pool", lambda h, a=a, k=k, i=i: h.indirect_dma_start(
                            out=yk[a][k][:], out_offset=None, in_=YS,
                            in_offset=bass.IndirectOffsetOnAxis(ap=SLK[:, k, i:i + 1], axis=0)),
                            reads=[bSLK, dYS], writes=[byk[a][k]])
                    ld("sp", x1r[a][:], X1[i * 128:(i + 1) * 128, :], reads=[dX1], writes=[bx1r[a]])
                    ps, bps = PS[a], bPS[a]
                    sc.op("pe", lambda h, ps=ps, i=i: h.transpose(out=ps[0:NE, 0:128], in_=G_all[:, i, :], identity=ident[:]), reads=[bG, bC], writes=[bps])
                    sc.op("act", lambda h, ps=ps: h.activation(out=GT[:], in_=ps[0:NE, 0:128], func=AF.Copy), reads=[bps], writes=[bGT])

                    def fgb(h, ps=ps):
                        ins = None
                        for n2 in range(2):
                            ins = h.matmul(ps[:, n2 * 512:(n2 + 1) * 512], lhsT=GT[:], rhs=bd[:, n2 * 512:(n2 + 1) * 512], start=True, stop=True)
                        return ins
                    sc.op("pe", fgb, reads=[bGT, bbd], writes=[bps])
                    sc.op("dve", lambda h, a=a, ps=ps: h.scalar_tensor_tensor(out=zt[a][:], in0=x1r[a][:], scalar=ALPHA, in1=ps[:], op0=ALU.mult, op1=ALU.add), reads=[bx1r[a], bps], writes=[bzt[a]])
                    sc.op("pool", lambda h, a=a: h.tensor_tensor(out=yk[a][0][:], in0=yk[a][0][:], in1=yk[a][1][:], op=ALU.add), reads=[byk[a][0], byk[a][1]], writes=[byk[a][0]])
                    sc.op("pool", lambda h, a=a: h.tensor_tensor(out=yk[a][2][:], in0=yk[a][2][:], in1=yk[a][3][:], op=ALU.add), reads=[byk[a][2], byk[a][3]], writes=[byk[a][2]])
                    sc.op("dve", lambda h, a=a: h.tensor_tensor(out=zt[a][:], in0=zt[a][:], in1=yk[a][0][:], op=ALU.add), reads=[bzt[a], byk[a][0]], writes=[bzt[a]])
                    sc.op("dve", lambda h, a=a: h.tensor_tensor(out=zt[a][:], in0=zt[a][:], in1=yk[a][2][:], op=ALU.add), reads=[bzt[a], byk[a][2]], writes=[bzt[a]])
                    layer_norm_tile(zt[a], bzt[a], gtab, btab, bgb, x2t[a], bx2t[a], lnt)
                    ld("sp", Xnext[i * 128:(i + 1) * 128, :], x2t[a][:], reads=[bx2t[a]], writes=[])
                    if not last:
                        transpose_to_XT(x2t[a], bx2t[a], i, PS[2 + a], bPS[2 + a], xtb[a], bxtb[a])
                sc.barrier()
            Xcur = XA
            bXcur = dXA
        sc.barrier()
    return nc


def kernel(**inputs):
    global CONSTS
    if CONSTS is None:
        CONSTS = _consts()
    nc = build_nc()
    x = np.ascontiguousarray(inputs["x"], dtype=np.float32).reshape(NCORES, T, D)
    shared = {k: np.ascontiguousarray(v) for k, v in inputs.items() if k != "x"}
    for k, v in CONSTS.items():
        shared["c_" + k] = v
    in_maps = []
    for c in range(NCORES):
        m = dict(shared)
        m["x"] = x[c]
        in_maps.append(m)
    res = run_bass_kernel_spmd(nc, in_maps, core_ids=list(range(NCORES)))
    out = np.stack([np.asarray(r["y"]) for r in res.results], axis=0)
    return out.reshape(16, S, D).astype(np.float32)
```

```python
import numpy as np
from contextlib import ExitStack
import concourse.bass as bass
import concourse.mybir as mybir
from concourse.bass_utils import run_bass_kernel_spmd

F32 = mybir.dt.float32
BF16 = mybir.dt.bfloat16
I32 = mybir.dt.int32
U32 = mybir.dt.uint32
AF = mybir.ActivationFunctionType
ALU = mybir.AluOpType
AX = mybir.AxisListType

NCORES = 8
D = 1024
S = 2048
NSEQ = 2
T = NSEQ * S
NT = T // 128
DEPTH = 4
NE = 32
TOPK = 4
CAP = 768
NJ = CAP // 128
NSLOT = NE * CAP
ALPHA = float((2 * DEPTH) ** 0.25)
LN_EPS = 1e-5
PAD = 8
SW_LIMIT = 7.0
SW_ALPHA = 1.702
POOL_WINDOWS = (2, 4, 8, 16)
C_PATTERNS = ((128, 1), (512, 4), (2048, 16))


class Buf:
    __slots__ = ("name", "w", "r")

    def __init__(self, name):
        self.name = name
        self.w = None
        self.r = {}


class Sched:
    ENGS = ("pe", "act", "dve", "pool", "sp")

    def __init__(self, nc, es, n_lanes=40):
        self.nc = nc
        self.h = {"pe": nc.tensor, "act": nc.scalar, "dve": nc.vector, "pool": nc.gpsimd, "sp": nc.sync}
        self.sem = {e: es.enter_context(nc.semaphore("s_" + e)) for e in self.ENGS}
        self.cnt = {e: 0 for e in self.ENGS}
        self.known = {e: {} for e in self.ENGS}
        self.n_lanes = n_lanes
        self.lsem = [es.enter_context(nc.semaphore("l%d" % i)) for i in range(n_lanes)]
        self.lcnt = [0] * n_lanes
        self.next_lane = 0
        self.next_sw = 0
        self.n_hw = 24
        self.nwait = 0

    def _semof(self, key):
        return self.lsem[key[1]] if isinstance(key, tuple) else self.sem[key]

    def _need(self, e, ev, waits):
        if ev is None:
            return
        key, val = ev
        if key == e and e == "pe":
            return
        if self.known[e].get(key, 0) >= val:
            return
        if waits.get(key, 0) < val:
            waits[key] = val

    @staticmethod
    def _flat(bs):
        out = []
        for b in bs:
            if isinstance(b, (list, tuple)):
                out.extend(Sched._flat(b))
            else:
                out.append(b)
        return out

    def _deps(self, e, reads, writes):
        waits = {}
        for b in reads:
            self._need(e, b.w, waits)
        for b in writes:
            self._need(e, b.w, waits)
            for k, v in b.r.items():
                self._need(e, (k, v), waits)
        return waits

    def _emit_waits(self, e, waits):
        h = self.h[e]
        for k, v in waits.items():
            h.wait_ge(self._semof(k), v)
            self.known[e][k] = v
            self.nwait += 1

    def op(self, e, fn, reads=(), writes=()):
        reads = self._flat(reads); writes = self._flat(writes)
        waits = self._deps(e, reads, writes)
        self._emit_waits(e, waits)
        ins = fn(self.h[e])
        ins.then_inc(self.sem[e], 1)
        self.cnt[e] += 1
        v = self.cnt[e]
        for b in reads:
            b.r[e] = v
        for b in writes:
            b.w = (e, v)
            b.r = {}

    def dma(self, q, fn, reads=(), writes=()):
        if q == "pool":
            lane = self.n_hw + self.next_sw
            self.next_sw = (self.next_sw + 1) % (self.n_lanes - self.n_hw)
        else:
            lane = self.next_lane
            self.next_lane = (lane + 1) % self.n_hw
        key = ("L", lane)
        reads = self._flat(reads); writes = self._flat(writes)
        waits = self._deps(q, reads, writes)
        self._need(q, (key, self.lcnt[lane]), waits)
        self._emit_waits(q, waits)
        ins = fn(self.h[q])
        ins.then_inc(self.lsem[lane], 16)
        self.lcnt[lane] += 16
        v = self.lcnt[lane]
        for b in reads:
            b.r[key] = v
        for b in writes:
            b.w = (key, v)
            b.r = {}

    def barrier(self):
        for e in self.ENGS:
            waits = {}
            for o in self.ENGS:
                if o != e:
                    self._need(e, (o, self.cnt[o]), waits)
            for i in range(self.n_lanes):
                self._need(e, (("L", i), self.lcnt[i]), waits)
            self._emit_waits(e, waits)


def _consts():
    c = {}
    c["ident"] = np.eye(128, dtype=np.float32)
    c["ustrict"] = np.triu(np.ones((128, 128), np.float32), 1)
    c["ones"] = np.ones((128, 128), np.float32)
    rc = np.ones((4, 16), np.float32)
    for g, w in enumerate(POOL_WINDOWS):
        for t in range(8):
            lo = max(t - w // 2, 0)
            hi = min(t + w - w // 2, S)
            rc[g, t] = 1.0 / (hi - lo)
            tt = S - 8 + t
            lo = max(tt - w // 2, 0)
            hi = min(tt + w - w // 2, S)
            rc[g, 8 + t] = 1.0 / (hi - lo)
    c["poolrc"] = np.broadcast_to(rc.reshape(1, 64), (128, 64)).copy()
    ecap = (np.arange(NE, dtype=np.float32) * CAP)
    c["ecap"] = np.broadcast_to(np.tile(ecap, NT).reshape(1, NT * NE), (128, NT * NE)).copy()
    c["tokidx"] = (np.arange(NT, dtype=np.int32)[None, :] * 128 + np.arange(128, dtype=np.int32)[:, None]).astype(np.int32)
    c["dump"] = np.broadcast_to((NSLOT + np.arange(128, dtype=np.float32))[:, None], (128, NT * NE)).copy()
    init = np.zeros((128, (NSLOT + 128) // 128, 2), np.int32)
    init[:, :, 0] = T
    c["slotinit"] = init
    slopes = np.array([2.0 ** (-8.0 * (i + 1) / 8) for i in range(8)], np.float64)
    kk = np.arange(128)[:, None]
    cc = np.arange(384)[None, :]
    dist = np.abs(cc - 128 - kk)
    ed = np.zeros((128, 8, 384), np.float32)
    for h in range(8):
        ed[:, h, :] = np.where(dist <= 128, np.exp(-slopes[h] * dist), 0.0)
    c["edec_d"] = ed
    cc = np.arange(256)[None, :]
    dist = np.abs(cc - 64 - kk)
    ec = np.zeros((128, 3, 8, 256), np.float32)
    for p, (_, dil) in enumerate(C_PATTERNS):
        for h in range(8):
            ec[:, p, h, :] = np.where(dist <= 64, np.exp(-slopes[h] * dist * dil), 0.0)
    c["edec_c"] = ec
    return c


CONSTS = None


def build_nc(layers=tuple(range(DEPTH)), debug=False, stop_after=None):
    nc = bass.Bass("TRN2", target_bir_lowering=False)
    dram = {}
    _uid = [0]

    def UQ(name):
        _uid[0] += 1
        return "%s_u%d" % (name, _uid[0])

    def din(name, shape, dt=F32):
        dram[name] = nc.dram_tensor(name, list(shape), dt, kind="ExternalInput").ap()
        return dram[name]

    x_in = din("x", [T, D])
    ev_w_in = din("ev_w_in", [2, D, 2048])
    ev_pool_w = din("ev_pool_w", [2, 4, 128, 128])
    ev_pool_scale = din("ev_pool_scale", [2, 512])
    ev_conv_w = din("ev_conv_w", [2, 3, 512])
    ev_w_out = din("ev_w_out", [2, 1024, 1024])
    od_w_in = din("od_w_in", [2, D, 2304])
    od_sink = din("od_sink", [2, 8])
    od_w_out = din("od_w_out", [2, 1024, 1024])
    router_w = din("router_w", [DEPTH, D, NE])
    router_b = din("router_b", [DEPTH, NE])
    exp_w_gu = din("exp_w_gu", [DEPTH, NE, D, 2048])
    exp_b_gu = din("exp_b_gu", [DEPTH, NE, 2048])
    exp_w_down = din("exp_w_down", [DEPTH, NE, 1024, D])
    exp_b_down = din("exp_b_down", [DEPTH, NE, D])
    ln_g = din("ln_g", [DEPTH, 2, D])
    ln_b = din("ln_b", [DEPTH, 2, D])
    cst = {}
    for k, v in CONSTS.items():
        cst[k] = din("c_" + k, v.shape, I32 if v.dtype == np.int32 else F32)

    y_out = nc.dram_tensor("y", [T, D], F32, kind="ExternalOutput").ap()
    okind = "ExternalOutput" if debug else "Internal"
    XA = nc.dram_tensor("XA", [T, D], F32, kind=okind).ap()
    X1 = nc.dram_tensor("X1", [T, D], F32, kind=okind).ap()
    XT = nc.dram_tensor("XT", [D, T], BF16, kind="Internal").ap()
    XS = nc.dram_tensor("XS", [T + 128, D], BF16, kind="Internal").ap()
    SLOT = nc.dram_tensor("SLOT", [NSLOT + 128, 2], I32, kind="Internal").ap()
    YS = nc.dram_tensor("YS", [NSLOT + 128, D], F32, kind="Internal").ap()
    dXA, dX1, dXT, dXS, dSLOT, dYS = (Buf(n) for n in ("XA", "X1", "XT", "XS", "SLOT", "YS"))
    dY = Buf("Y")

    with ExitStack() as es:
        sc = Sched(nc, es)
        sb = lambda name, shape, dt=F32: es.enter_context(nc.sbuf_tensor(UQ(name), list(shape), dt))

        ident = sb("ident", [128, 128])
        ident_bf = sb("ident_bf", [128, 128], BF16)
        ustrict_bf = sb("ustrict_bf", [128, 128], BF16)
        ones_bf = sb("ones_bf", [128, 128], BF16)
        ctmp = sb("ctmp", [128, 128])
        poolrc = sb("poolrc", [128, 64])
        tokidx = sb("tokidx", [128, NT], I32)
        G_all = sb("G_all", [128, NT, NE])
        L_all = sb("L_all", [128, NT, NE])
        M8_all = sb("M8_all", [128, NT, 8])
        M_all = sb("M_all", [128, NT * NE], BF16)
        SLK = sb("SLK", [128, TOPK, NT], U32)
        PAY = sb("PAY", [128, NT, TOPK, 2], I32)
        zrow = sb("zrow", [128, D], BF16)
        bC = Buf("consts")
        bG, bL, bM8, bM, bSLK, bPAY = (Buf(n) for n in ("G", "L", "M8", "M", "SLK", "PAY"))

        PS = [es.enter_context(nc.psum_tensor("ps%d" % i, [128, 1024], F32)) for i in range(4)]
        bPSh = [[Buf("ps%d_%d" % (i, hh)) for hh in range(2)] for i in range(4)]

        class _BP:
            def __getitem__(self, i):
                return _BPi(i)

        class _BPi(list):
            def __init__(self, i):
                super().__init__(bPSh[i])
        bPS = _BP()

        def ld(q, out_ap, in_ap, reads=(), writes=(), **kw):
            sc.dma(q, lambda h: h.dma_start(out=out_ap, in_=in_ap, **kw), reads=reads, writes=writes)

        ld("sp", ident[:], cst["ident"], writes=[bC])
        ld("sp", poolrc[:], cst["poolrc"], writes=[bC])
        ld("sp", tokidx[:], cst["tokidx"], writes=[bC])
        ld("pool", ident_bf[:], cst["ident"], writes=[bC])
        ld("pool", ustrict_bf[:], cst["ustrict"], writes=[bC])
        ld("pool", ones_bf[:], cst["ones"], writes=[bC])
        sc.op("dve", lambda h: h.memset(zrow[:], 0.0), writes=[bC])
        ld("sp", XS[T:T + 128, :], zrow[:], reads=[bC], writes=[dXS])
        with nc.sbuf_tensor(UQ("zf"), [128, D], F32) as zf:
            bzf = Buf("zf")
            sc.op("dve", lambda h: h.memset(zf[:], 0.0), writes=[bzf])
            ld("sp", YS[NSLOT:NSLOT + 128, :], zf[:], reads=[bzf], writes=[dYS])
            sc.barrier()

        def layer_norm_tile(z, bz, gt, bt_, bgb, out, bout, tmp):
            st, mv, rstd = tmp["st"], tmp["mv"], tmp["rstd"]
            bst = tmp["bst"]
            sc.op("dve", lambda h: h.bn_stats(out=st[:, 0, :], in_=z[:, 0:512]), reads=[bz], writes=[bst])
            sc.op("dve", lambda h: h.bn_stats(out=st[:, 1, :], in_=z[:, 512:1024]), reads=[bz], writes=[bst])
            sc.op("dve", lambda h: h.bn_aggr(out=mv[:], in_=st[:].rearrange("p a b -> p (a b)")), reads=[bst], writes=[bst])
            sc.op("dve", lambda h: h.tensor_scalar(out=rstd[:], in0=mv[:, 1:2], scalar1=LN_EPS, scalar2=None, op0=ALU.add), reads=[bst], writes=[bst])
            sc.op("act", lambda h: h.activation(out=rstd[:], in_=rstd[:], func=AF.Sqrt), reads=[bst], writes=[bst])
            sc.op("dve", lambda h: h.reciprocal(out=rstd[:], in_=rstd[:]), reads=[bst], writes=[bst])
            sc.op("dve", lambda h: h.scalar_tensor_tensor(out=z[:], in0=z[:], scalar=mv[:, 0:1], in1=gt[:], op0=ALU.subtract, op1=ALU.mult), reads=[bz, bst, bgb], writes=[bz])
            sc.op("dve", lambda h: h.scalar_tensor_tensor(out=out[:], in0=z[:], scalar=rstd[:, 0:1], in1=bt_[:], op0=ALU.mult, op1=ALU.add), reads=[bz, bst, bgb], writes=[bout])

        def transpose_to_XT(xt_tile, bxt, i, ps, bps, xtb, bxtb):
            def f(h):
                ins = None
                for k in range(8):
                    ins = h.transpose(out=ps[:, k * 128:(k + 1) * 128], in_=xt_tile[:, k * 128:(k + 1) * 128], identity=ident[:])
                return ins
            sc.op("pe", f, reads=[bxt, bC], writes=[bps])
            sc.op("act", lambda h: h.activation(out=xtb[:].rearrange("p k t -> p (k t)"), in_=ps[:], func=AF.Copy), reads=[bps], writes=[bxtb])
            ld("sp", XT.rearrange("(k p) t -> p k t", p=128)[:, :, i * 128:(i + 1) * 128], xtb[:], reads=[bxtb], writes=[])

        with ExitStack() as p0:
            xts = [p0.enter_context(nc.sbuf_tensor(UQ("p0x%d" % i), [128, D], F32)) for i in range(2)]
            bxts = [Buf("p0x%d" % i) for i in range(2)]
            xtbs = [p0.enter_context(nc.sbuf_tensor(UQ("p0b%d" % i), [128, 8, 128], BF16)) for i in range(2)]
            bxtbs = [Buf("p0b%d" % i) for i in range(2)]
            for i in range(NT):
                a = i % 2
                ld("sp", xts[a][:], x_in[i * 128:(i + 1) * 128, :], writes=[bxts[a]])
                transpose_to_XT(xts[a], bxts[a], i, PS[a], bPS[a], xtbs[a], bxtbs[a])
            sc.barrier()

        Xcur = x_in
        bXcur = Buf("xin")

        for L in layers:
            li = L // 2
            last = (L == layers[-1])
            with ExitStack() as p1:
                sbq = lambda name, shape, dt=F32: p1.enter_context(nc.sbuf_tensor(UQ(name), list(shape), dt))
                catT = sbq("catT", [128, 8, S], BF16); bcat = Buf("catT")
                for s in range(NSEQ):
                    t0 = s * S
                    with ExitStack() as pa:
                        sbp = lambda name, shape, dt=F32: pa.enter_context(nc.sbuf_tensor(UQ(name), list(shape), dt))
                        xT = sbp("xT", [128, 8, S], BF16); bxT = [Buf("xT%d" % k) for k in range(8)]
                        for k in range(8):
                            ld("sp", xT[:, k, :], XT[k * 128:(k + 1) * 128, t0:t0 + S], reads=[dXT], writes=[bxT[k]])
                        pcnt = [0]
                        if L % 2 == 0:
                            win = sbp("win", [128, 8, 2048], BF16); bwin = [Buf("win%d" % k) for k in range(8)]
                            for k in range(8):
                                ld("pool", win[:, k, :], ev_w_in[li, k * 128:(k + 1) * 128, :], writes=[bwin[k]], max_dma_last_dim=4096)
                            poolw = sbp("poolw", [128, 4, 128], BF16); bpw = Buf("poolw")
                            ld("pool", poolw[:], ev_pool_w[li].rearrange("g c d -> c g d"), writes=[bpw])
                            pscale = sbp("pscale", [128, 4]); convw = sbp("convw", [128, 3, 4])
                            ld("sp", pscale[:], ev_pool_scale[li].rearrange("(g p) -> p g", p=128), writes=[bpw], allow_slow_non_contiguous=True)
                            ld("sp", convw[:], ev_conv_w[li].rearrange("k (m p) -> p k m", p=128), writes=[bpw], allow_slow_non_contiguous=True)
                            WB = [sbp("wb%d" % i, [128, S + 2 * PAD]) for i in range(4)]
                            bWB = [Buf("wb%d" % i) for i in range(4)]
                            plb = sbp("plb", [128, S], BF16); bplb = Buf("plb")
                            etmp = sbp("etmp", [128, 16]); betmp = Buf("etmp")
                            for i in range(4):
                                sc.op("dve", lambda h, i=i: h.memset(WB[i][:], 0.0), writes=[bWB[i]])

                            def proj(fc, evac):
                                for tc in range(4):
                                    pi = pcnt[0] % 4
                                    pcnt[0] += 1
                                    ps = PS[pi]; bps = bPSh[pi][0]

                                    def f(h, tc=tc, ps=ps):
                                        ins = None
                                        for k in range(8):
                                            ins = h.matmul(ps[:, 0:512], lhsT=win[:, k, fc * 128:(fc + 1) * 128], rhs=xT[:, k, tc * 512:(tc + 1) * 512], start=(k == 0), stop=(k == 7))
                                        return ins
                                    sc.op("pe", f, reads=[bwin, bxT], writes=[bps])
                                    evac(tc, ps[:, 0:512], bps)

                            for g, w in enumerate(POOL_WINDOWS):
                                U, A, B = WB[0], WB[1], WB[2]
                                bU, bA, bB = bWB[0], bWB[1], bWB[2]
                                proj(g, lambda tc, pa_, bps: sc.op("act", lambda h: h.activation(out=U[:, PAD + tc * 512:PAD + (tc + 1) * 512], in_=pa_, func=AF.Copy), reads=[bps], writes=[bU]))
                                lo, n = PAD - 7, S + 14
                                sc.op("dve", lambda h: h.tensor_tensor(out=A[:, lo:lo + n], in0=U[:, lo - 1:lo - 1 + n], in1=U[:, lo:lo + n], op=ALU.add), reads=[bU], writes=[bA])
                                cur, bcur, oth, both = A, bA, B, bB
                                ext = 7
                                ww = 2
                                while ww < w:
                                    sh = ww // 2
                                    ext = ext - sh
                                    lo, n = PAD - ext, S + 2 * ext
                                    sc.op("dve", lambda h, cur=cur, oth=oth, lo=lo, n=n, sh=sh: h.tensor_tensor(out=oth[:, lo:lo + n], in0=cur[:, lo - sh:lo - sh + n], in1=cur[:, lo + sh:lo + sh + n], op=ALU.add), reads=[bcur], writes=[both])
                                    cur, bcur, oth, both = oth, both, cur, bcur
                                    ww *= 2
                                sc.op("dve", lambda h, cur=cur: h.scalar_tensor_tensor(out=plb[:], in0=cur[:, PAD:PAD + S], scalar=1.0 / w, in1=U[:, PAD:PAD + S], op0=ALU.mult, op1=ALU.subtract), reads=[bcur, bU], writes=[bplb])
                                sc.op("dve", lambda h, cur=cur: h.tensor_tensor(out=etmp[:, 0:8], in0=cur[:, PAD:PAD + 8], in1=poolrc[:, g * 16:g * 16 + 8], op=ALU.mult), reads=[bcur, bC], writes=[betmp])
                                sc.op("dve", lambda h, cur=cur: h.tensor_tensor(out=etmp[:, 8:16], in0=cur[:, PAD + S - 8:PAD + S], in1=poolrc[:, g * 16 + 8:g * 16 + 16], op=ALU.mult), reads=[bcur, bC], writes=[betmp])
                                sc.op("dve", lambda h: h.tensor_tensor(out=plb[:, 0:8], in0=etmp[:, 0:8], in1=U[:, PAD:PAD + 8], op=ALU.subtract), reads=[betmp, bU], writes=[bplb])
                                sc.op("dve", lambda h: h.tensor_tensor(out=plb[:, S - 8:S], in0=etmp[:, 8:16], in1=U[:, PAD + S - 8:PAD + S], op=ALU.subtract), reads=[betmp, bU], writes=[bplb])
                                for tc in range(4):
                                    pi = pcnt[0] % 4
                                    pcnt[0] += 1
                                    ps = PS[pi]; bps = bPSh[pi][0]
                                    sc.op("pe", lambda h, ps=ps, tc=tc: h.matmul(ps[:, 0:512], lhsT=poolw[:, g, :], rhs=plb[:, tc * 512:(tc + 1) * 512], start=True, stop=True), reads=[bpw, bplb], writes=[bps])
                                    sc.op("act", lambda h, ps=ps, tc=tc: h.activation(out=catT[:, g, tc * 512:(tc + 1) * 512], in_=ps[:, 0:512], func=AF.Copy, scale=pscale[:, g:g + 1]), reads=[bps, bpw], writes=[bcat])
                            for m in range(4):
                                Cb, U, A = WB[0], WB[3], WB[1]
                                bCb, bU, bA = bWB[0], bWB[3], bWB[1]
                                proj(8 + m, lambda tc, pa_, bps: sc.op("act", lambda h: h.activation(out=Cb[:, PAD + tc * 512:PAD + (tc + 1) * 512], in_=pa_, func=AF.Copy), reads=[bps], writes=[bCb]))
                                proj(12 + m, lambda tc, pa_, bps: sc.op("dve", lambda h: h.tensor_tensor(out=U[:, PAD + tc * 512:PAD + (tc + 1) * 512], in0=Cb[:, PAD + tc * 512:PAD + (tc + 1) * 512], in1=pa_, op=ALU.mult), reads=[bps, bCb], writes=[bU]))
                                sc.op("dve", lambda h: h.tensor_scalar(out=A[:, PAD:PAD + S], in0=U[:, PAD - 1:PAD - 1 + S], scalar1=convw[:, 0, m:m + 1], scalar2=None, op0=ALU.mult), reads=[bU, bpw], writes=[bA])
                                sc.op("dve", lambda h: h.scalar_tensor_tensor(out=A[:, PAD:PAD + S], in0=U[:, PAD:PAD + S], scalar=convw[:, 1, m:m + 1], in1=A[:, PAD:PAD + S], op0=ALU.mult, op1=ALU.add), reads=[bU, bpw, bA], writes=[bA])
                                sc.op("dve", lambda h: h.scalar_tensor_tensor(out=A[:, PAD:PAD + S], in0=U[:, PAD + 1:PAD + 1 + S], scalar=convw[:, 2, m:m + 1], in1=A[:, PAD:PAD + S], op0=ALU.mult, op1=ALU.add), reads=[bU, bpw, bA], writes=[bA])
                                proj(4 + m, lambda tc, pa_, bps: sc.op("dve", lambda h: h.tensor_tensor(out=catT[:, 4 + m, tc * 512:(tc + 1) * 512], in0=A[:, PAD + tc * 512:PAD + (tc + 1) * 512], in1=pa_, op=ALU.mult), reads=[bps, bA], writes=[bcat]))
                        else:
                            win = sbp("win", [128, 8, 2304], BF16); bwin = [Buf("win%d" % k) for k in range(8)]
                            for k in range(8):
                                ld("pool", win[:, k, :], od_w_in[li, k * 128:(k + 1) * 128, :], writes=[bwin[k]], max_dma_last_dim=4096)
                            qT = sbp("qT", [128, S], BF16); bqT = Buf("qT")
                            kT = sbp("kT", [128, S], BF16); bkT = Buf("kT")
                            VV = sbp("VV", [128, 48, 128], BF16); bVV = Buf("VV")
                            PT = [sbp("PT%d" % i, [128, 16, 384], BF16) for i in range(2)]; bPT = [Buf("PT%d" % i) for i in range(2)]
                            Pr = [sbp("Pr%d" % i, [128, 384], BF16) for i in range(2)]; bPr = [Buf("Pr%d" % i) for i in range(2)]
                            acc_o = sbp("acc_o", [128, S]); bacc_o = Buf("acc_o")
                            acc_d = sbp("acc_d", [128, S]); bacc_d = Buf("acc_d")
                            etab = sbp("etab", [128, 3, 2, 384]); betab = Buf("etab")
                            esink = sbp("esink", [128, 8]); besink = Buf("esink")
                            ld("sp", esink[:], od_sink[li:li + 1, :].to_broadcast([128, 8]), writes=[besink])
                            sc.op("act", lambda h: h.activation(out=esink[:], in_=esink[:], func=AF.Exp), reads=[besink], writes=[besink])
                            ptc = [0]
                            sct = [0]

                            def projT(col0, dst, bdst, dup=False):
                                for tc in range(4):
                                    pi = pcnt[0] % 4
                                    pcnt[0] += 1
                                    ps = PS[pi // 2]; hh = pi % 2; bps = bPSh[pi // 2][hh]

                                    def f(h, tc=tc, ps=ps, hh=hh):
                                        ins = None
                                        for k in range(8):
                                            if not dup:
                                                ins = h.matmul(ps[:, hh * 512:(hh + 1) * 512], lhsT=win[:, k, col0:col0 + 128], rhs=xT[:, k, tc * 512:(tc + 1) * 512], start=(k == 0), stop=(k == 7))
                                            else:
                                                for half in range(2):
                                                    ins = h.matmul(ps[half * 64:(half + 1) * 64, hh * 512:(hh + 1) * 512], lhsT=win[:, k, col0:col0 + 64], rhs=xT[:, k, tc * 512:(tc + 1) * 512], start=(k == 0), stop=(k == 7))
                                        return ins
                                    sc.op("pe", f, reads=[bwin, bxT], writes=[bps])
                                    sc.op("act", lambda h, ps=ps, hh=hh, tc=tc: h.activation(out=dst[:, tc * 512:(tc + 1) * 512], in_=ps[:, hh * 512:(hh + 1) * 512], func=AF.Copy), reads=[bps], writes=[bdst])

                            def projV(col0, tiles):
                                for (vi, tstart, tstep) in tiles:
                                    pi = pcnt[0] % 4
                                    pcnt[0] += 1
                                    ps = PS[pi // 2]; hh = pi % 2; bps = bPSh[pi // 2][hh]

                                    def f(h, ps=ps, hh=hh, tstart=tstart, tstep=tstep):
                                        ins = None
                                        for k in range(8):
                                            ins = h.matmul(ps[:, hh * 512:hh * 512 + 128], lhsT=xT[:, k, tstart:tstart + 127 * tstep + 1:tstep], rhs=win[:, k, col0:col0 + 128], start=(k == 0), stop=(k == 7))
                                        return ins
                                    sc.op("pe", f, reads=[bwin, bxT], writes=[bps])
                                    sc.op("act", lambda h, ps=ps, hh=hh, vi=vi: h.activation(out=VV[:, vi, :], in_=ps[:, hh * 512:hh * 512 + 128], func=AF.Copy), reads=[bps], writes=[bVV])

                            def att_scores(job):
                                hb, vcol, tsel, pidx, (dil, vvb, W, rad), first, jn = job
                                if True:
                                    Ls = S // dil
                                    ntile = Ls // 128
                                    pt = PT[jn % 2]; bpt = bPT[jn % 2]
                                    tb = etab[:, tsel, hb // 64, :]
                                    for r in range(dil):
                                        for j in range(ntile):
                                            q0 = 128 * j - rad
                                            c_lo = max(0, -q0)
                                            c_hi = min(W, Ls - q0)
                                            nq = c_hi - c_lo
                                            si = sct[0] % 4
                                            sct[0] += 1
                                            ps = PS[si // 2]; hh = si % 2; bps = bPSh[si // 2][hh]
                                            kst = r + dil * (128 * j)
                                            qst = r + dil * (q0 + c_lo)
                                            sc.op("pe", lambda h, ps=ps, hh=hh, kst=kst, qst=qst, nq=nq: h.matmul(
                                                ps[:, hh * 512:hh * 512 + nq],
                                                lhsT=kT[hb:hb + 64, kst:kst + 127 * dil + 1:dil],
                                                rhs=qT[hb:hb + 64, qst:qst + (nq - 1) * dil + 1:dil], start=True, stop=True),
                                                reads=[bkT, bqT], writes=[bps])
                                            pr = Pr[si % 2]; bpr = bPr[si % 2]
                                            sc.op("act", lambda h, ps=ps, hh=hh, nq=nq, pr=pr: h.activation(out=pr[:, 0:nq], in_=ps[:, hh * 512:hh * 512 + nq], func=AF.Exp, scale=0.125), reads=[bps], writes=[bpr])
                                            ti = r * ntile + j
                                            sc.op("dve", lambda h, pr=pr, nq=nq, c_lo=c_lo, ti=ti: h.tensor_tensor(out=pt[:, ti, c_lo:c_lo + nq], in0=pr[:, 0:nq], in1=tb[:, c_lo:c_lo + nq], op=ALU.mult), reads=[bpr, betab], writes=[bpt])
                            def att_pv(job):
                                hb, vcol, tsel, pidx, (dil, vvb, W, rad), first, jn = job
                                if True:
                                    Ls = S // dil
                                    ntile = Ls // 128
                                    pt = PT[jn % 2]; bpt = bPT[jn % 2]
                                    QB = rad
                                    nqb = 512 // QB
                                    for ch in range(S // 512):
                                        hh = ch % 2
                                        pso, bpso = PS[2], bPSh[2][hh]
                                        psd, bpsd = PS[3], bPSh[3][hh]

                                        def fpv(h, which, ps, ch=ch, hh=hh):
                                            ins = None
                                            for b in range(nqb):
                                                gq = ch * 512 + b * QB
                                                r = gq // Ls
                                                l0 = gq % Ls
                                                parts = []
                                                for j in range(ntile):
                                                    k_lo = max(128 * j, l0 - rad)
                                                    k_hi = min(128 * j + 128, l0 + QB + rad)
                                                    if k_hi <= k_lo:
                                                        continue
                                                    parts.append((j, k_lo - 128 * j, k_hi - 128 * j))
                                                parts = [(j, 0, 128) for (j, _a, _b) in parts]
                                                for n_, (j, p_lo, p_hi) in enumerate(parts):
                                                    ti = r * ntile + j
                                                    col = l0 - (128 * j - rad)
                                                    if which == 0:
                                                        lhsT = VV[p_lo:p_hi, vvb + ti, vcol:vcol + 64]
                                                    else:
                                                        lhsT = ones_bf[p_lo:p_hi, 0:64]
                                                    ins = h.matmul(ps[hb:hb + 64, hh * 512 + b * QB:hh * 512 + (b + 1) * QB], lhsT=lhsT,
                                                                   rhs=pt[p_lo:p_hi, ti, col:col + QB], start=(n_ == 0), stop=(n_ == len(parts) - 1))
                                            return ins
                                        sc.op("pe", lambda h, pso=pso: fpv(h, 0, pso), reads=[bVV, bpt], writes=[bpso])
                                        sc.op("pe", lambda h, psd=psd: fpv(h, 1, psd), reads=[bC, bpt], writes=[bpsd])
                                        if dil == 1:
                                            dso = acc_o[hb:hb + 64, ch * 512:(ch + 1) * 512]
                                            dsd = acc_d[hb:hb + 64, ch * 512:(ch + 1) * 512]
                                            srco = pso[hb:hb + 64, hh * 512:(hh + 1) * 512]
                                            srcd = psd[hb:hb + 64, hh * 512:(hh + 1) * 512]
                                        else:
                                            nr = 512 // Ls if Ls < 512 else 1
                                            if Ls >= 512:
                                                r = (ch * 512) // Ls
                                                l0 = (ch * 512) % Ls
                                                st_ = r + dil * l0
                                                dso = acc_o[hb:hb + 64, st_:st_ + 511 * dil + 1:dil]
                                                dsd = acc_d[hb:hb + 64, st_:st_ + 511 * dil + 1:dil]
                                                srco = pso[hb:hb + 64, hh * 512:(hh + 1) * 512]
                                                srcd = psd[hb:hb + 64, hh * 512:(hh + 1) * 512]
                                            else:
                                                r0 = (ch * 512) // Ls
                                                dso = acc_o[hb:hb + 64, :].rearrange("p (l r) -> p r l", r=dil)[:, r0:r0 + nr, :]
                                                dsd = acc_d[hb:hb + 64, :].rearrange("p (l r) -> p r l", r=dil)[:, r0:r0 + nr, :]
                                                srco = pso[hb:hb + 64, hh * 512:(hh + 1) * 512].rearrange("p (r l) -> p r l", r=nr)
                                                srcd = psd[hb:hb + 64, hh * 512:(hh + 1) * 512].rearrange("p (r l) -> p r l", r=nr)
                                        if first and pidx == 0:
                                            sc.op("act", lambda h, dso=dso, srco=srco: h.activation(out=dso, in_=srco, func=AF.Copy), reads=[bpso], writes=[bacc_o])
                                            sc.op("act", lambda h, dsd=dsd, srcd=srcd: h.activation(out=dsd, in_=srcd, func=AF.Copy), reads=[bpsd], writes=[bacc_d])
                                        else:
                                            sc.op("dve", lambda h, dso=dso, srco=srco: h.tensor_tensor(out=dso, in0=dso, in1=srco, op=ALU.add), reads=[bpso, bacc_o], writes=[bacc_o])
                                            sc.op("dve", lambda h, dsd=dsd, srcd=srcd: h.tensor_tensor(out=dsd, in0=dsd, in1=srcd, op=ALU.add), reads=[bpsd, bacc_d], writes=[bacc_d])


                            def attend_all(jobs):
                                prev = None
                                for job in jobs:
                                    att_scores(job)
                                    if prev is not None:
                                        att_pv(prev)
                                    prev = job
                                att_pv(prev)

                            def finish(chunk, sink_heads=None):
                                if sink_heads is not None:
                                    for half, hd_ in enumerate(sink_heads):
                                        sc.op("dve", lambda h, half=half, hd_=hd_: h.tensor_scalar(out=acc_d[half * 64:(half + 1) * 64, :], in0=acc_d[half * 64:(half + 1) * 64, :], scalar1=esink[half * 64:(half + 1) * 64, hd_:hd_ + 1], scalar2=None, op0=ALU.add), reads=[bacc_d, besink], writes=[bacc_d])
                                sc.op("dve", lambda h: h.reciprocal(out=acc_d[:], in_=acc_d[:]), reads=[bacc_d], writes=[bacc_d])
                                sc.op("pool", lambda h: h.tensor_tensor(out=catT[:, chunk, :], in0=acc_o[:], in1=acc_d[:], op=ALU.mult), reads=[bacc_o, bacc_d], writes=[bcat])

                            for c in range(4):
                                ld("sp", etab[:, :, :, 0:256], cst["edec_c"][:, :, 2 * c:2 * c + 2, :], writes=[betab])
                                projT(0 + c * 128, qT, bqT)
                                projT(512 + c * 128, kT, bkT)
                                tiles = []
                                vvb = {}
                                vi = 0
                                for (_, dil) in C_PATTERNS:
                                    vvb[dil] = vi
                                    Ls = S // dil
                                    for r in range(dil):
                                        for j in range(Ls // 128):
                                            tiles.append((vi, r + dil * 128 * j, dil))
                                            vi += 1
                                projV(1024 + c * 128, tiles)
                                pats = [(dil, vvb[dil], 256, 64) for (_, dil) in C_PATTERNS]
                                jobs = []
                                for half in range(2):
                                    for pidx, pat in enumerate(pats):
                                        jobs.append((half * 64, half * 64, pidx, pidx, pat, True, ptc[0]))
                                        ptc[0] += 1
                                attend_all(jobs)
                                finish(c)
                            for c in range(4):
                                g = c // 2
                                ld("sp", etab[:, 0, :, :], cst["edec_d"][:, 2 * c:2 * c + 2, :], writes=[betab])
                                projT(1536 + c * 128, qT, bqT)
                                projT(2048 + g * 64, kT, bkT, dup=True)
                                projV(2176, [(j, 128 * j, 1) for j in range(16)])
                                jobs = []
                                for half in range(2):
                                    jobs.append((half * 64, g * 64, 0, 0, (1, 0, 384, 128), True, ptc[0]))
                                    ptc[0] += 1
                                attend_all(jobs)
                                finish(4 + c, sink_heads=(2 * c, 2 * c + 1))
                        sc.barrier()
                    with ExitStack() as pb:
                        sbp = lambda name, shape, dt=F32: pb.enter_context(nc.sbuf_tensor(UQ(name), list(shape), dt))
                        gtab = sbp("gtab", [128, D]); btab = sbp("btab", [128, D]); bgb = Buf("gb")
                        ld("sp", gtab[:], ln_g[L, 0:1, :].to_broadcast([128, D]), writes=[bgb])
                        ld("sp", btab[:], ln_b[L, 0:1, :].to_broadcast([128, D]), writes=[bgb])
                        rw = sbp("rw", [128, 8, NE]); brw = Buf("rw")
                        ld("sp", rw[:], router_w[L].rearrange("(k p) e -> p k e", p=128), writes=[brw])
                        rb = sbp("rb", [128, NE])
                        ld("sp", rb[:], router_b[L:L + 1, :].to_broadcast([128, NE]), writes=[brw])
                        wout = sbp("wout", [128, 8, D], BF16); bwout = [Buf("wout%d" % k) for k in range(8)]
                        w_out_src = (ev_w_out if L % 2 == 0 else od_w_out)[li]
                        for k in range(8):
                            ld("pool", wout[:, k, :], w_out_src[k * 128:(k + 1) * 128, :], writes=[bwout[k]], max_dma_last_dim=4096)
                        lnt = {"st": sbp("st", [128, 2, 6]), "mv": sbp("mv", [128, 2]), "rstd": sbp("rstd", [128, 1]), "bst": Buf("st")}
                        zt = [sbp("zt%d" % i, [128, D]) for i in range(2)]; bzt = [Buf("zt%d" % i) for i in range(2)]
                        xres, bxres, x1t, bx1t = zt, bzt, zt, bzt
                        x1b = [sbp("x1b%d" % i, [128, D], BF16) for i in range(2)]; bx1b = [Buf("x1b%d" % i) for i in range(2)]
                        x1T = sbp("x1T", [128, 8, 128]); bx1T = Buf("x1T")
                        rsm = {k: sbp("r_" + k, [128, n]) for k, n in (("nmax", 1), ("ex", NE), ("msk", NE), ("ssum", 1))}
                        brs = Buf("rsm")
                        for tt in range(S // 128):
                            i = s * (S // 128) + tt
                            a = i % 2
                            ps = PS[a]; bps = bPS[a]

                            def f(h, ps=ps, tt=tt):
                                ins = None
                                for n2 in range(2):
                                    for k in range(8):
                                        ins = h.matmul(ps[:, n2 * 512:(n2 + 1) * 512], lhsT=catT[:, k, tt * 128:(tt + 1) * 128], rhs=wout[:, k, n2 * 512:(n2 + 1) * 512], start=(k == 0), stop=(k == 7))
                                return ins
                            sc.op("pe", f, reads=[bcat, bwout], writes=[bps])
                            ld("sp", xres[a][:], Xcur[i * 128:(i + 1) * 128, :], reads=[bXcur], writes=[bxres[a]])
                            sc.op("dve", lambda h, a=a, ps=ps: h.scalar_tensor_tensor(out=zt[a][:], in0=xres[a][:], scalar=ALPHA, in1=ps[:], op0=ALU.mult, op1=ALU.add), reads=[bxres[a], bps], writes=[bzt[a]])
                            layer_norm_tile(zt[a], bzt[a], gtab, btab, bgb, x1t[a], bx1t[a], lnt)
                            ld("sp", X1[i * 128:(i + 1) * 128, :], x1t[a][:], reads=[bx1t[a]], writes=[])
                            sc.op("act", lambda h, a=a: h.activation(out=x1b[a][:], in_=x1t[a][:], func=AF.Copy), reads=[bx1t[a]], writes=[bx1b[a]])
                            ld("sp", XS[i * 128:(i + 1) * 128, :], x1b[a][:], reads=[bx1b[a]], writes=[])
                            ps2 = PS[2 + a]; bps2 = bPS[2 + a]

                            def f2(h, a=a, ps2=ps2):
                                ins = None
                                for k in range(8):
                                    ins = h.transpose(out=ps2[:, k * 128:(k + 1) * 128], in_=x1t[a][:, k * 128:(k + 1) * 128], identity=ident[:])
                                return ins
                            sc.op("pe", f2, reads=[bx1t[a], bC], writes=[bps2])
                            sc.op("act", lambda h, ps2=ps2: h.activation(out=x1T[:].rearrange("p k t -> p (k t)"), in_=ps2[:], func=AF.Copy), reads=[bps2], writes=[bx1T])

                            def f3(h, ps2=ps2):
                                ins = None
                                for k in range(8):
                                    ins = h.matmul(ps2[:, 0:NE], lhsT=x1T[:, k, :], rhs=rw[:, k, :], start=(k == 0), stop=(k == 7))
                                return ins
                            sc.op("pe", f3, reads=[bx1T, brw], writes=[bps2])
                            Li = L_all[:, i, :]
                            sc.op("dve", lambda h, ps2=ps2, Li=Li: h.tensor_tensor(out=Li, in0=ps2[:, 0:NE], in1=rb[:], op=ALU.add), reads=[bps2, brw], writes=[bL])
                            m8 = M8_all[:, i, :]
                            sc.op("dve", lambda h, Li=Li, m8=m8: h.max(out=m8, in_=Li), reads=[bL], writes=[bM8])
                            sc.op("dve", lambda h, Li=Li, m8=m8: h.tensor_scalar(out=rsm["msk"][:], in0=Li, scalar1=m8[:, 3:4], scalar2=None, op0=ALU.is_ge), reads=[bL, bM8], writes=[brs])
                            sc.op("dve", lambda h, m8=m8: h.tensor_scalar(out=rsm["nmax"][:], in0=m8[:, 0:1], scalar1=-1.0, scalar2=None, op0=ALU.mult), reads=[bM8], writes=[brs])
                            sc.op("act", lambda h, Li=Li: h.activation(out=rsm["ex"][:], in_=Li, func=AF.Exp, bias=rsm["nmax"][:, 0:1]), reads=[bL, brs], writes=[brs])
                            sc.op("dve", lambda h: h.scalar_tensor_tensor(out=rsm["ex"][:], in0=rsm["ex"][:], scalar=1.0, in1=rsm["msk"][:], op0=ALU.mult, op1=ALU.mult, accum_out=rsm["ssum"][:]), reads=[brs], writes=[brs])
                            sc.op("dve", lambda h: h.reciprocal(out=rsm["ssum"][:], in_=rsm["ssum"][:]), reads=[brs], writes=[brs])
                            sc.op("dve", lambda h, i=i: h.tensor_scalar(out=G_all[:, i, :], in0=rsm["ex"][:], scalar1=rsm["ssum"][:, 0:1], scalar2=None, op0=ALU.mult), reads=[brs], writes=[bG])
                            sc.op("dve", lambda h, i=i: h.tensor_copy(out=M_all[:, i * NE:(i + 1) * NE], in_=rsm["msk"][:]), reads=[brs], writes=[bM])
                        sc.barrier()
            if stop_after == (L, "mix"):
                break

            with ExitStack() as p1b:
                sbp = lambda name, shape, dt=F32: p1b.enter_context(nc.sbuf_tensor(UQ(name), list(shape), dt))
                pos = sbp("pos", [128, NT, NE]); bpos = Buf("pos")
                carry = sbp("carry", [128, NT, NE]); bcar = Buf("carry")
                tot = sbp("tot", [128, NT, NE]); btot = Buf("tot")
                oh = sbp("oh", [128, NT, NE]); boh = Buf("oh")
                prod = sbp("prod", [128, NT, NE]); bprod = Buf("prod")
                slkf = sbp("slkf", [128, TOPK, NT]); bslkf = Buf("slkf")
                gk = sbp("gk", [128, TOPK, NT]); bgk = Buf("gk")
                sinit = sbp("sinit", [128, (NSLOT + 128) // 128, 2], I32); bsin = Buf("sinit")
                ecap = sbp("ecap", [128, NT * NE]); dumpt = sbp("dumpt", [128, NT * NE])
                ld("sp", ecap[:], cst["ecap"], writes=[bC])
                ld("sp", dumpt[:], cst["dump"], writes=[bC])
                ld("sp", sinit[:], cst["slotinit"], writes=[bsin])
                ld("sp", SLOT.rearrange("(j p) c -> p j c", p=128), sinit[:], reads=[bsin], writes=[dSLOT])

                def fW(h):
                    ins = None
                    for n2 in range(2):
                        ins = h.matmul(PS[0][:, n2 * 512:(n2 + 1) * 512], lhsT=ustrict_bf[:], rhs=M_all[:, n2 * 512:(n2 + 1) * 512], start=True, stop=True)
                    return ins
                sc.op("pe", fW, reads=[bC, bM], writes=[bPS[0]])

                def fT(h):
                    ins = None
                    for n2 in range(2):
                        ins = h.matmul(PS[1][:, n2 * 512:(n2 + 1) * 512], lhsT=ones_bf[:], rhs=M_all[:, n2 * 512:(n2 + 1) * 512], start=True, stop=True)
                    return ins
                sc.op("pe", fT, reads=[bC, bM], writes=[bPS[1]])
                sc.op("act", lambda h: h.activation(out=tot[:].rearrange("p a b -> p (a b)"), in_=PS[1][:], func=AF.Copy), reads=[bPS[1]], writes=[btot])
                sc.op("dve", lambda h: h.memset(carry[:, 0, :], 0.0), writes=[bcar])
                for i in range(1, NT):
                    sc.op("dve", lambda h, i=i: h.tensor_tensor(out=carry[:, i, :], in0=carry[:, i - 1, :], in1=tot[:, i - 1, :], op=ALU.add), reads=[bcar, btot], writes=[bcar])
                fl = lambda t: t[:].rearrange("p a b -> p (a b)")
                sc.op("dve", lambda h: h.tensor_tensor(out=fl(pos), in0=fl(carry), in1=PS[0][:], op=ALU.add), reads=[bcar, bPS[0]], writes=[bpos])
                sc.op("dve", lambda h: h.tensor_scalar(out=fl(oh), in0=fl(pos), scalar1=float(CAP) - 0.5, scalar2=None, op0=ALU.is_lt), reads=[bpos], writes=[boh])
                sc.op("dve", lambda h: h.tensor_tensor(out=fl(oh), in0=fl(oh), in1=M_all[:], op=ALU.mult), reads=[boh, bM], writes=[boh])
                sc.op("dve", lambda h: h.tensor_tensor(out=fl(pos), in0=fl(pos), in1=ecap[:], op=ALU.add), reads=[bpos, bC], writes=[bpos])
                sc.op("dve", lambda h: h.tensor_tensor(out=fl(pos), in0=fl(pos), in1=dumpt[:], op=ALU.subtract), reads=[bpos, bC], writes=[bpos])
                sc.op("dve", lambda h: h.tensor_tensor(out=fl(pos), in0=fl(pos), in1=fl(oh), op=ALU.mult), reads=[bpos, boh], writes=[bpos])
                sc.op("dve", lambda h: h.tensor_tensor(out=fl(pos), in0=fl(pos), in1=dumpt[:], op=ALU.add), reads=[bpos, bC], writes=[bpos])
                for k in range(TOPK):
                    sc.op("dve", lambda h, k=k: h.tensor_tensor(out=oh[:], in0=L_all[:], in1=M8_all[:, :, k:k + 1].to_broadcast([128, NT, NE]), op=ALU.is_equal), reads=[bL, bM8], writes=[boh])
                    sc.op("dve", lambda h: h.tensor_tensor(out=fl(prod), in0=fl(oh), in1=fl(pos), op=ALU.mult), reads=[boh, bpos], writes=[bprod])
                    sc.op("dve", lambda h, k=k: h.tensor_reduce(out=slkf[:, k, :], in_=prod[:], axis=AX.X, op=ALU.add), reads=[bprod], writes=[bslkf])
                    sc.op("dve", lambda h: h.tensor_tensor(out=fl(prod), in0=fl(oh), in1=fl(G_all), op=ALU.mult), reads=[boh, bG], writes=[bprod])
                    sc.op("dve", lambda h, k=k: h.tensor_reduce(out=gk[:, k, :], in_=prod[:], axis=AX.X, op=ALU.add), reads=[bprod], writes=[bgk])
                sc.op("dve", lambda h: h.tensor_copy(out=SLK[:], in_=slkf[:]), reads=[bslkf], writes=[bSLK])
                for k in range(TOPK):
                    sc.op("dve", lambda h, k=k: h.tensor_copy(out=PAY[:, :, k, 0], in_=tokidx[:]), reads=[bC], writes=[bPAY])
                    sc.op("dve", lambda h, k=k: h.tensor_copy(out=PAY[:, :, k, 1], in_=gk[:, k, :].bitcast(I32)), reads=[bgk], writes=[bPAY])
                for i in range(NT):
                    for k in range(TOPK):
                        sc.dma("pool", lambda h, i=i, k=k: h.indirect_dma_start(
                            out=SLOT, out_offset=bass.IndirectOffsetOnAxis(ap=SLK[:, k, i:i + 1], axis=0),
                            in_=PAY[:, i, k, :], in_offset=None), reads=[bSLK, bPAY, dSLOT], writes=[])
                sc.barrier()
            if stop_after == (L, "route"):
                break

            with ExitStack() as p2:
                sbp = lambda name, shape, dt=F32: p2.enter_context(nc.sbuf_tensor(UQ(name), list(shape), dt))
                wgu = [sbp("wgu%d" % i, [128, 8, 2048], BF16) for i in range(2)]; bwgu = [[Buf("wgu%d_%d" % (i, k)) for k in range(8)] for i in range(2)]
                wdn = [sbp("wdn%d" % i, [128, 8, D], BF16) for i in range(2)]; bwdn = [[Buf("wdn%d_%d" % (i, k)) for k in range(8)] for i in range(2)]
                braw = sbp("braw", [NE, 2048]); bbraw = Buf("braw")
                bgu = sbp("bgu", [128, 16, NE]); bbgu = Buf("bgu")
                idx = [sbp("idx%d" % i, [128, NJ, 2], I32) for i in range(3)]; bidx = [Buf("idx%d" % i) for i in range(3)]
                xg = [sbp("xg%d" % i, [128, D], BF16) for i in range(NJ)]; bxg = [Buf("xg%d" % i) for i in range(NJ)]
                xgT = [sbp("xgT%d" % i, [128, 8, CAP], BF16) for i in range(2)]; bxgT = [Buf("xgT%d" % i) for i in range(2)]
                actT = sbp("actT", [128, 8, CAP], BF16); bactT = Buf("actT")
                NH = CAP // 2
                eg = [sbp("eg%d" % i, [128, NH]) for i in range(4)]; beg = [Buf("eg%d" % i) for i in range(4)]
                esg = [sbp("esg%d" % i, [128, NH]) for i in range(4)]; besg = [Buf("esg%d" % i) for i in range(4)]
                el = [sbp("el%d" % i, [128, NH]) for i in range(4)]; bel = [Buf("el%d" % i) for i in range(4)]
                ysb = [sbp("ysb%d" % i, [128, D]) for i in range(2)]; bysb = [Buf("ysb%d" % i) for i in range(2)]

                ld("sp", braw[:], exp_b_gu[L], writes=[bbraw])

                def fb(h):
                    ins = None
                    for m in range(8):
                        for two in range(2):
                            c0 = (two * 8 + m) * NE
                            src = braw[:, :].rearrange("e (c two) -> e two c", two=2)[:, two, m * 128:(m + 1) * 128]
                            ins = h.transpose(out=PS[0][:, c0:c0 + NE], in_=src, identity=ident[0:NE, 0:NE])
                    return ins
                sc.op("pe", fb, reads=[bbraw, bC], writes=[bPS[0]])
                sc.op("act", lambda h: h.activation(out=bgu[:].rearrange("p a b -> p (a b)"), in_=PS[0][:, 0:16 * NE], func=AF.Copy), reads=[bPS[0]], writes=[bbgu])

                def load_w(e):
                    a = e % 2
                    for k in range(8):
                        ld("pool", wgu[a][:, k, :], exp_w_gu[L, e, k * 128:(k + 1) * 128, :], writes=[bwgu[a][k]], max_dma_last_dim=4096)
                    for k in range(8):
                        ld("pool", wdn[a][:, k, :], exp_w_down[L, e, k * 128:(k + 1) * 128, :], writes=[bwdn[a][k]], max_dma_last_dim=4096)

                def gather_issue(e):
                    a3 = e % 3
                    ld("sp", idx[a3][:], SLOT[e * CAP:(e + 1) * CAP, :].rearrange("(j p) c -> p j c", p=128), reads=[dSLOT], writes=[bidx[a3]])
                    for j in range(NJ):
                        sc.dma("pool", lambda h, a3=a3, j=j: h.indirect_dma_start(
                            out=xg[j][:], out_offset=None, in_=XS,
                            in_offset=bass.IndirectOffsetOnAxis(ap=idx[a3][:, j, 0:1].bitcast(U32), axis=0)),
                            reads=[bidx[a3], dXS], writes=[bxg[j]])

                tcount = [0]

                def transpose_x(e, j):
                    a = e % 2
                    pi = 2 + (tcount[0] % 2)
                    tcount[0] += 1
                    psb = PS[pi][:].bitcast(BF16)

                    def ft(h, psb=psb):
                        ins = None
                        for k in range(8):
                            ins = h.transpose(out=psb[:, k * 128:(k + 1) * 128], in_=xg[j][:, k * 128:(k + 1) * 128], identity=ident_bf[:])
                        return ins
                    sc.op("pe", ft, reads=[bxg[j], bC], writes=[bPS[pi]])
                    sc.op("act", lambda h, psb=psb: h.activation(out=xgT[a][:, :, j * 128:(j + 1) * 128], in_=psb[:, 0:1024].rearrange("p (k t) -> p k t", k=8), func=AF.Copy), reads=[bPS[pi]], writes=[bxgT[a]])

                gather_issue(0)
                load_w(0)
                for j in range(NJ):
                    transpose_x(0, j)
                ycount = [0]
                for e in range(NE):
                    a = e % 2
                    a3 = e % 3
                    if e + 1 < NE:
                        gather_issue(e + 1)
                        load_w(e + 1)
                    pending = []
                    pair = 0
                    for m in range(8):
                        for nh in range(2):
                            n0 = nh * NH
                            q = pair % 2
                            psg, bpsg = PS[0], bPSh[0][q]
                            psl, bpsl = PS[1], bPSh[1][q]

                            def fg(h, two, ps, m=m, n0=n0, q=q):
                                ins = None
                                wv = wgu[a][:].rearrange("p k (c two) -> p k two c", two=2)
                                for k in range(8):
                                    ins = h.matmul(ps[:, q * 512:q * 512 + NH], lhsT=wv[:, k, two, m * 128:(m + 1) * 128], rhs=xgT[a][:, k, n0:n0 + NH], start=(k == 0), stop=(k == 7))
                                return ins
                            ei = pair % 4
                            sc.op("pe", lambda h, fg=fg, psg=psg: fg(h, 0, psg), reads=[bwgu[a], bxgT[a]], writes=[bpsg])
                            sc.op("pe", lambda h, fg=fg, psl=psl: fg(h, 1, psl), reads=[bwgu[a], bxgT[a]], writes=[bpsl])
                            pg = psg[:, q * 512:q * 512 + NH]
                            pl = psl[:, q * 512:q * 512 + NH]
                            sc.op("dve", lambda h, pg=pg, ei=ei, m=m, e=e: h.tensor_scalar(out=eg[ei][:], in0=pg, scalar1=bgu[:, m, e:e + 1], scalar2=SW_LIMIT, op0=ALU.add, op1=ALU.min), reads=[bpsg, bbgu], writes=[beg[ei]])
                            sc.op("act", lambda h, pl=pl, ei=ei, m=m, e=e: h.activation(out=el[ei][:], in_=pl, func=AF.Identity, bias=bgu[:, 8 + m, e:e + 1]), reads=[bpsl, bbgu], writes=[bel[ei]])
                            sc.op("act", lambda h, ei=ei: h.activation(out=esg[ei][:], in_=eg[ei][:], func=AF.Sigmoid, scale=SW_ALPHA), reads=[beg[ei]], writes=[besg[ei]])
                            sc.op("pool", lambda h, ei=ei: h.tensor_tensor(out=eg[ei][:], in0=eg[ei][:], in1=esg[ei][:], op=ALU.mult), reads=[beg[ei], besg[ei]], writes=[beg[ei]])
                            for fn in pending:
                                fn()
                            pending = []

                            def fin(ei=ei, m=m, n0=n0):
                                sc.op("dve", lambda h: h.tensor_scalar(out=el[ei][:], in0=el[ei][:], scalar1=SW_LIMIT, scalar2=-SW_LIMIT, op0=ALU.min, op1=ALU.max), reads=[bel[ei]], writes=[bel[ei]])
                                sc.op("dve", lambda h: h.scalar_tensor_tensor(out=actT[:, m, n0:n0 + NH], in0=el[ei][:], scalar=1.0, in1=eg[ei][:], op0=ALU.add, op1=ALU.mult), reads=[beg[ei], bel[ei]], writes=[bactT])
                            pending.append(fin)
                            if e + 1 < NE and pair in (3, 5, 7, 9, 11, 13):
                                transpose_x(e + 1, (pair - 3) // 2)
                            pair += 1
                    for fn in pending:
                        fn()
                    for j in range(NJ):
                        pi = 2 + (ycount[0] % 2)
                        yb = ycount[0] % 2
                        ycount[0] += 1
                        ps, bps = PS[pi], bPS[pi]

                        def fd(h, ps=ps, j=j):
                            ins = None
                            for n2 in range(2):
                                for k in range(8):
                                    ins = h.matmul(ps[:, n2 * 512:(n2 + 1) * 512], lhsT=actT[:, k, j * 128:(j + 1) * 128], rhs=wdn[a][:, k, n2 * 512:(n2 + 1) * 512], start=(k == 0), stop=(k == 7))
                            return ins
                        sc.op("pe", fd, reads=[bactT, bwdn[a]], writes=[bps])
                        sc.op("act", lambda h, ps=ps, yb=yb, j=j: h.activation(out=ysb[yb][:], in_=ps[:], func=AF.Copy, scale=idx[a3][:, j, 1:2].bitcast(F32)), reads=[bps, bidx[a3]], writes=[bysb[yb]])
                        ld("sp", YS[e * CAP + j * 128:e * CAP + (j + 1) * 128, :], ysb[yb][:], reads=[bysb[yb]], writes=[])
                sc.barrier()
            if stop_after == (L, "moe"):
                break

            with ExitStack() as p3:
                sbp = lambda name, shape, dt=F32: p3.enter_context(nc.sbuf_tensor(UQ(name), list(shape), dt))
                gtab = sbp("gtab2", [128, D]); btab = sbp("btab2", [128, D]); bgb = Buf("gb2")
                ld("sp", gtab[:], ln_g[L, 1:2, :].to_broadcast([128, D]), writes=[bgb])
                ld("sp", btab[:], ln_b[L, 1:2, :].to_broadcast([128, D]), writes=[bgb])
                bd = sbp("bd", [NE, D]); bbd = Buf("bd")
                ld("sp", bd[:], exp_b_down[L], writes=[bbd])
                lnt = {"st": sbp("st2", [128, 2, 6]), "mv": sbp("mv2", [128, 2]), "rstd": sbp("rstd2", [128, 1]), "bst": Buf("st2")}
                yk = [[sbp("yk%d_%d" % (a, k), [128, D]) for k in range(TOPK)] for a in range(2)]
                byk = [[Buf("yk%d_%d" % (a, k)) for k in range(TOPK)] for a in range(2)]
                x1r = [sbp("x1r%d" % a, [128, D]) for a in range(2)]; bx1r = [Buf("x1r%d" % a) for a in range(2)]
                zt = [sbp("z2_%d" % a, [128, D]) for a in range(2)]; bzt = [Buf("z2_%d" % a) for a in range(2)]
                x2t = [sbp("x2t%d" % a, [128, D]) for a in range(2)]; bx2t = [Buf("x2t%d" % a) for a in range(2)]
                xtb = [sbp("xtb%d" % a, [128, 8, 128], BF16) for a in range(2)]; bxtb = [Buf("xtb%d" % a) for a in range(2)]
                GT = sbp("GT", [NE, 128]); bGT = Buf("GT")
                Xnext = y_out if last else XA
                dXn = dY if last else dXA
                for i in range(NT):
                    a = i % 2
                    for k in range(TOPK):
                        sc.dma("pool", lambda h, a=a, k=k, i=i: h.indirect_dma_start(
                            out=yk[a][k][:], out_offset=None, in_=YS,
                            in_offset=bass.IndirectOffsetOnAxis(ap=SLK[:, k, i:i + 1], axis=0)),
                            reads=[bSLK, dYS], writes=[byk[a][k]])
                    ld("sp", x1r[a][:], X1[i * 128:(i + 1) * 128, :], reads=[dX1], writes=[bx1r[a]])
                    ps, bps = PS[a], bPS[a]
                    sc.op("pe", lambda h, ps=ps, i=i: h.transpose(out=ps[0:NE, 0:128], in_=G_all[:, i, :], identity=ident[:]), reads=[bG, bC], writes=[bps])
                    sc.op("act", lambda h, ps=ps: h.activation(out=GT[:], in_=ps[0:NE, 0:128], func=AF.Copy), reads=[bps], writes=[bGT])

                    def fgb(h, ps=ps):
                        ins = None
                        for n2 in range(2):
                            ins = h.matmul(ps[:, n2 * 512:(n2 + 1) * 512], lhsT=GT[:], rhs=bd[:, n2 * 512:(n2 + 1) * 512], start=True, stop=True)
                        return ins
                    sc.op("pe", fgb, reads=[bGT, bbd], writes=[bps])
                    sc.op("dve", lambda h, a=a, ps=ps: h.scalar_tensor_tensor(out=zt[a][:], in0=x1r[a][:], scalar=ALPHA, in1=ps[:], op0=ALU.mult, op1=ALU.add), reads=[bx1r[a], bps], writes=[bzt[a]])
                    sc.op("pool", lambda h, a=a: h.tensor_tensor(out=yk[a][0][:], in0=yk[a][0][:], in1=yk[a][1][:], op=ALU.add), reads=[byk[a][0], byk[a][1]], writes=[byk[a][0]])
                    sc.op("dve", lambda h, a=a: h.tensor_tensor(out=zt[a][:], in0=zt[a][:], in1=yk[a][2][:], op=ALU.add), reads=[bzt[a], byk[a][2]], writes=[bzt[a]])
                    sc.op("dve", lambda h, a=a: h.tensor_tensor(out=zt[a][:], in0=zt[a][:], in1=yk[a][3][:], op=ALU.add), reads=[bzt[a], byk[a][3]], writes=[bzt[a]])
                    sc.op("dve", lambda h, a=a: h.tensor_tensor(out=zt[a][:], in0=zt[a][:], in1=yk[a][0][:], op=ALU.add), reads=[bzt[a], byk[a][0]], writes=[bzt[a]])
                    layer_norm_tile(zt[a], bzt[a], gtab, btab, bgb, x2t[a], bx2t[a], lnt)
                    ld("sp", Xnext[i * 128:(i + 1) * 128, :], x2t[a][:], reads=[bx2t[a]], writes=[])
                    if not last:
                        transpose_to_XT(x2t[a], bx2t[a], i, PS[2 + a], bPS[2 + a], xtb[a], bxtb[a])
                sc.barrier()
            Xcur = XA
            bXcur = dXA
        sc.barrier()
    return nc


def kernel(**inputs):
    global CONSTS
    if CONSTS is None:
        CONSTS = _consts()
    nc = build_nc()
    x = np.ascontiguousarray(inputs["x"], dtype=np.float32).reshape(NCORES, T, D)
    shared = {k: np.ascontiguousarray(v) for k, v in inputs.items() if k != "x"}
    for k, v in CONSTS.items():
        shared["c_" + k] = v
    in_maps = []
    for c in range(NCORES):
        m = dict(shared)
        m["x"] = x[c]
        in_maps.append(m)
    res = run_bass_kernel_spmd(nc, in_maps, core_ids=list(range(NCORES)))
    out = np.stack([np.asarray(r["y"]) for r in res.results], axis=0)
    return out.reshape(16, S, D).astype(np.float32)
```

```python
import numpy as np
from contextlib import ExitStack
import concourse.bass as bass
import concourse.mybir as mybir
from concourse.bass_utils import run_bass_kernel_spmd

F32 = mybir.dt.float32
BF16 = mybir.dt.bfloat16
I32 = mybir.dt.int32
U32 = mybir.dt.uint32
AF = mybir.ActivationFunctionType
ALU = mybir.AluOpType
AX = mybir.AxisListType

NCORES = 8
D = 1024
S = 2048
NSEQ = 2
T = NSEQ * S
NT = T // 128
DEPTH = 4
NE = 32
TOPK = 4
CAP = 768
NJ = CAP // 128
NSLOT = NE * CAP
ALPHA = float((2 * DEPTH) ** 0.25)
LN_EPS = 1e-5
PAD = 8
SW_LIMIT = 7.0
SW_ALPHA = 1.702
POOL_WINDOWS = (2, 4, 8, 16)
C_PATTERNS = ((128, 1), (512, 4), (2048, 16))


class Buf:
    __slots__ = ("name", "w", "r")

    def __init__(self, name):
        self.name = name
        self.w = None
        self.r = {}


class Sched:
    ENGS = ("pe", "act", "dve", "pool", "sp")

    def __init__(self, nc, es, n_lanes=40):
        self.nc = nc
        self.h = {"pe": nc.tensor, "act": nc.scalar, "dve": nc.vector, "pool": nc.gpsimd, "sp": nc.sync}
        self.sem = {e: es.enter_context(nc.semaphore("s_" + e)) for e in self.ENGS}
        self.cnt = {e: 0 for e in self.ENGS}
        self.known = {e: {} for e in self.ENGS}
        self.n_lanes = n_lanes
        self.lsem = [es.enter_context(nc.semaphore("l%d" % i)) for i in range(n_lanes)]
        self.lcnt = [0] * n_lanes
        self.next_lane = 0
        self.next_sw = 0
        self.n_hw = 24
        self.nwait = 0

    def _semof(self, key):
        return self.lsem[key[1]] if isinstance(key, tuple) else self.sem[key]

    def _need(self, e, ev, waits):
        if ev is None:
            return
        key, val = ev
        if key == e and e == "pe":
            return
        if self.known[e].get(key, 0) >= val:
            return
        if waits.get(key, 0) < val:
            waits[key] = val

    @staticmethod
    def _flat(bs):
        out = []
        for b in bs:
            if isinstance(b, (list, tuple)):
                out.extend(Sched._flat(b))
            else:
                out.append(b)
        return out

    def _deps(self, e, reads, writes):
        waits = {}
        for b in reads:
            self._need(e, b.w, waits)
        for b in writes:
            self._need(e, b.w, waits)
            for k, v in b.r.items():
                self._need(e, (k, v), waits)
        return waits

    def _emit_waits(self, e, waits):
        h = self.h[e]
        for k, v in waits.items():
            h.wait_ge(self._semof(k), v)
            self.known[e][k] = v
            self.nwait += 1

    def op(self, e, fn, reads=(), writes=()):
        reads = self._flat(reads); writes = self._flat(writes)
        waits = self._deps(e, reads, writes)
        self._emit_waits(e, waits)
        ins = fn(self.h[e])
        ins.then_inc(self.sem[e], 1)
        self.cnt[e] += 1
        v = self.cnt[e]
        for b in reads:
            b.r[e] = v
        for b in writes:
            b.w = (e, v)
            b.r = {}

    def dma(self, q, fn, reads=(), writes=()):
        if q == "pool":
            lane = self.n_hw + self.next_sw
            self.next_sw = (self.next_sw + 1) % (self.n_lanes - self.n_hw)
        else:
            lane = self.next_lane
            self.next_lane = (lane + 1) % self.n_hw
        key = ("L", lane)
        reads = self._flat(reads); writes = self._flat(writes)
        waits = self._deps(q, reads, writes)
        self._need(q, (key, self.lcnt[lane]), waits)
        self._emit_waits(q, waits)
        ins = fn(self.h[q])
        ins.then_inc(self.lsem[lane], 16)
        self.lcnt[lane] += 16
        v = self.lcnt[lane]
        for b in reads:
            b.r[key] = v
        for b in writes:
            b.w = (key, v)
            b.r = {}

    def barrier(self):
        for e in self.ENGS:
            waits = {}
            for o in self.ENGS:
                if o != e:
                    self._need(e, (o, self.cnt[o]), waits)
            for i in range(self.n_lanes):
                self._need(e, (("L", i), self.lcnt[i]), waits)
            self._emit_waits(e, waits)


def _consts():
    c = {}
    c["ident"] = np.eye(128, dtype=np.float32)
    c["ustrict"] = np.triu(np.ones((128, 128), np.float32), 1)
    c["ones"] = np.ones((128, 128), np.float32)
    rc = np.ones((4, 16), np.float32)
    for g, w in enumerate(POOL_WINDOWS):
        for t in range(8):
            lo = max(t - w // 2, 0)
            hi = min(t + w - w // 2, S)
            rc[g, t] = 1.0 / (hi - lo)
            tt = S - 8 + t
            lo = max(tt - w // 2, 0)
            hi = min(tt + w - w // 2, S)
            rc[g, 8 + t] = 1.0 / (hi - lo)
    c["poolrc"] = np.broadcast_to(rc.reshape(1, 64), (128, 64)).copy()
    ecap = (np.arange(NE, dtype=np.float32) * CAP)
    c["ecap"] = np.broadcast_to(np.tile(ecap, NT).reshape(1, NT * NE), (128, NT * NE)).copy()
    c["tokidx"] = (np.arange(NT, dtype=np.int32)[None, :] * 128 + np.arange(128, dtype=np.int32)[:, None]).astype(np.int32)
    c["dump"] = np.broadcast_to((NSLOT + np.arange(128, dtype=np.float32))[:, None], (128, NT * NE)).copy()
    init = np.zeros((128, (NSLOT + 128) // 128, 2), np.int32)
    init[:, :, 0] = T
    c["slotinit"] = init
    slopes = np.array([2.0 ** (-8.0 * (i + 1) / 8) for i in range(8)], np.float64)
    kk = np.arange(128)[:, None]
    cc = np.arange(384)[None, :]
    dist = np.abs(cc - 128 - kk)
    ed = np.zeros((128, 8, 384), np.float32)
    for h in range(8):
        ed[:, h, :] = np.where(dist <= 128, np.exp(-slopes[h] * dist), 0.0)
    c["edec_d"] = ed
    cc = np.arange(256)[None, :]
    dist = np.abs(cc - 64 - kk)
    ec = np.zeros((128, 3, 8, 256), np.float32)
    for p, (_, dil) in enumerate(C_PATTERNS):
        for h in range(8):
            ec[:, p, h, :] = np.where(dist <= 64, np.exp(-slopes[h] * dist * dil), 0.0)
    c["edec_c"] = ec
    return c


CONSTS = None


def build_nc(layers=tuple(range(DEPTH)), debug=False, stop_after=None):
    nc = bass.Bass("TRN2", target_bir_lowering=False)
    dram = {}
    _uid = [0]

    def UQ(name):
        _uid[0] += 1
        return "%s_u%d" % (name, _uid[0])

    def din(name, shape, dt=F32):
        dram[name] = nc.dram_tensor(name, list(shape), dt, kind="ExternalInput").ap()
        return dram[name]

    x_in = din("x", [T, D])
    ev_w_in = din("ev_w_in", [2, D, 2048])
    ev_pool_w = din("ev_pool_w", [2, 4, 128, 128])
    ev_pool_scale = din("ev_pool_scale", [2, 512])
    ev_conv_w = din("ev_conv_w", [2, 3, 512])
    ev_w_out = din("ev_w_out", [2, 1024, 1024])
    od_w_in = din("od_w_in", [2, D, 2304])
    od_sink = din("od_sink", [2, 8])
    od_w_out = din("od_w_out", [2, 1024, 1024])
    router_w = din("router_w", [DEPTH, D, NE])
    router_b = din("router_b", [DEPTH, NE])
    exp_w_gu = din("exp_w_gu", [DEPTH, NE, D, 2048])
    exp_b_gu = din("exp_b_gu", [DEPTH, NE, 2048])
    exp_w_down = din("exp_w_down", [DEPTH, NE, 1024, D])
    exp_b_down = din("exp_b_down", [DEPTH, NE, D])
    ln_g = din("ln_g", [DEPTH, 2, D])
    ln_b = din("ln_b", [DEPTH, 2, D])
    cst = {}
    for k, v in CONSTS.items():
        cst[k] = din("c_" + k, v.shape, I32 if v.dtype == np.int32 else F32)

    y_out = nc.dram_tensor("y", [T, D], F32, kind="ExternalOutput").ap()
    okind = "ExternalOutput" if debug else "Internal"
    XA = nc.dram_tensor("XA", [T, D], F32, kind=okind).ap()
    X1 = nc.dram_tensor("X1", [T, D], F32, kind=okind).ap()
    XT = nc.dram_tensor("XT", [D, T], BF16, kind="Internal").ap()
    XS = nc.dram_tensor("XS", [T + 128, D], BF16, kind="Internal").ap()
    SLOT = nc.dram_tensor("SLOT", [NSLOT + 128, 2], I32, kind="Internal").ap()
    YS = nc.dram_tensor("YS", [NSLOT + 128, D], F32, kind="Internal").ap()
    dXA, dX1, dXT, dXS, dSLOT, dYS = (Buf(n) for n in ("XA", "X1", "XT", "XS", "SLOT", "YS"))
    dY = Buf("Y")

    with ExitStack() as es:
        sc = Sched(nc, es)
        sb = lambda name, shape, dt=F32: es.enter_context(nc.sbuf_tensor(UQ(name), list(shape), dt))

        ident = sb("ident", [128, 128])
        ident_bf = sb("ident_bf", [128, 128], BF16)
        ustrict_bf = sb("ustrict_bf", [128, 128], BF16)
        ones_bf = sb("ones_bf", [128, 128], BF16)
        ctmp = sb("ctmp", [128, 128])
        poolrc = sb("poolrc", [128, 64])
        tokidx = sb("tokidx", [128, NT], I32)
        G_all = sb("G_all", [128, NT, NE])
        L_all = sb("L_all", [128, NT, NE])
        M8_all = sb("M8_all", [128, NT, 8])
        M_all = sb("M_all", [128, NT * NE], BF16)
        SLK = sb("SLK", [128, TOPK, NT], U32)
        PAY = sb("PAY", [128, NT, TOPK, 2], I32)
        zrow = sb("zrow", [128, D], BF16)
        bC = Buf("consts")
        bG, bL, bM8, bM, bSLK, bPAY = (Buf(n) for n in ("G", "L", "M8", "M", "SLK", "PAY"))

        PS = [es.enter_context(nc.psum_tensor("ps%d" % i, [128, 1024], F32)) for i in range(4)]
        bPSh = [[Buf("ps%d_%d" % (i, hh)) for hh in range(2)] for i in range(4)]

        class _BP:
            def __getitem__(self, i):
                return _BPi(i)

        class _BPi(list):
            def __init__(self, i):
                super().__init__(bPSh[i])
        bPS = _BP()

        def ld(q, out_ap, in_ap, reads=(), writes=(), **kw):
            sc.dma(q, lambda h: h.dma_start(out=out_ap, in_=in_ap, **kw), reads=reads, writes=writes)

        ld("sp", ident[:], cst["ident"], writes=[bC])
        ld("sp", poolrc[:], cst["poolrc"], writes=[bC])
        ld("sp", tokidx[:], cst["tokidx"], writes=[bC])
        ld("pool", ident_bf[:], cst["ident"], writes=[bC])
        ld("pool", ustrict_bf[:], cst["ustrict"], writes=[bC])
        ld("pool", ones_bf[:], cst["ones"], writes=[bC])
        sc.op("dve", lambda h: h.memset(zrow[:], 0.0), writes=[bC])
        ld("sp", XS[T:T + 128, :], zrow[:], reads=[bC], writes=[dXS])
        with nc.sbuf_tensor(UQ("zf"), [128, D], F32) as zf:
            bzf = Buf("zf")
            sc.op("dve", lambda h: h.memset(zf[:], 0.0), writes=[bzf])
            ld("sp", YS[NSLOT:NSLOT + 128, :], zf[:], reads=[bzf], writes=[dYS])
            sc.barrier()

        def layer_norm_tile(z, bz, gt, bt_, bgb, out, bout, tmp):
            st, mv, rstd = tmp["st"], tmp["mv"], tmp["rstd"]
            bst = tmp["bst"]
            sc.op("dve", lambda h: h.bn_stats(out=st[:, 0, :], in_=z[:, 0:512]), reads=[bz], writes=[bst])
            sc.op("dve", lambda h: h.bn_stats(out=st[:, 1, :], in_=z[:, 512:1024]), reads=[bz], writes=[bst])
            sc.op("dve", lambda h: h.bn_aggr(out=mv[:], in_=st[:].rearrange("p a b -> p (a b)")), reads=[bst], writes=[bst])
            sc.op("dve", lambda h: h.tensor_scalar(out=rstd[:], in0=mv[:, 1:2], scalar1=LN_EPS, scalar2=None, op0=ALU.add), reads=[bst], writes=[bst])
            sc.op("act", lambda h: h.activation(out=rstd[:], in_=rstd[:], func=AF.Sqrt), reads=[bst], writes=[bst])
            sc.op("dve", lambda h: h.reciprocal(out=rstd[:], in_=rstd[:]), reads=[bst], writes=[bst])
            sc.op("dve", lambda h: h.scalar_tensor_tensor(out=z[:], in0=z[:], scalar=mv[:, 0:1], in1=gt[:], op0=ALU.subtract, op1=ALU.mult), reads=[bz, bst, bgb], writes=[bz])
            sc.op("dve", lambda h: h.scalar_tensor_tensor(out=out[:], in0=z[:], scalar=rstd[:, 0:1], in1=bt_[:], op0=ALU.mult, op1=ALU.add), reads=[bz, bst, bgb], writes=[bout])

        def transpose_to_XT(xt_tile, bxt, i, ps, bps, xtb, bxtb):
            def f(h):
                ins = None
                for k in range(8):
                    ins = h.transpose(out=ps[:, k * 128:(k + 1) * 128], in_=xt_tile[:, k * 128:(k + 1) * 128], identity=ident[:])
                return ins
            sc.op("pe", f, reads=[bxt, bC], writes=[bps])
            sc.op("act", lambda h: h.activation(out=xtb[:].rearrange("p k t -> p (k t)"), in_=ps[:], func=AF.Copy), reads=[bps], writes=[bxtb])
            ld("sp", XT.rearrange("(k p) t -> p k t", p=128)[:, :, i * 128:(i + 1) * 128], xtb[:], reads=[bxtb], writes=[])

        with ExitStack() as p0:
            xts = [p0.enter_context(nc.sbuf_tensor(UQ("p0x%d" % i), [128, D], F32)) for i in range(2)]
            bxts = [Buf("p0x%d" % i) for i in range(2)]
            xtbs = [p0.enter_context(nc.sbuf_tensor(UQ("p0b%d" % i), [128, 8, 128], BF16)) for i in range(2)]
            bxtbs = [Buf("p0b%d" % i) for i in range(2)]
            for i in range(NT):
                a = i % 2
                ld("sp", xts[a][:], x_in[i * 128:(i + 1) * 128, :], writes=[bxts[a]])
                transpose_to_XT(xts[a], bxts[a], i, PS[a], bPS[a], xtbs[a], bxtbs[a])
            sc.barrier()

        Xcur = x_in
        bXcur = Buf("xin")

        for L in layers:
            li = L // 2
            last = (L == layers[-1])
            with ExitStack() as p1:
                sbq = lambda name, shape, dt=F32: p1.enter_context(nc.sbuf_tensor(UQ(name), list(shape), dt))
                catT = sbq("catT", [128, 8, S], BF16); bcat = Buf("catT")
                for s in range(NSEQ):
                    t0 = s * S
                    with ExitStack() as pa:
                        sbp = lambda name, shape, dt=F32: pa.enter_context(nc.sbuf_tensor(UQ(name), list(shape), dt))
                        xT = sbp("xT", [128, 8, S], BF16); bxT = [Buf("xT%d" % k) for k in range(8)]
                        for k in range(8):
                            ld("sp", xT[:, k, :], XT[k * 128:(k + 1) * 128, t0:t0 + S], reads=[dXT], writes=[bxT[k]])
                        pcnt = [0]
                        if L % 2 == 0:
                            win = sbp("win", [128, 8, 2048], BF16); bwin = [Buf("win%d" % k) for k in range(8)]
                            for k in range(8):
                                ld("pool", win[:, k, :], ev_w_in[li, k * 128:(k + 1) * 128, :], writes=[bwin[k]], max_dma_last_dim=4096)
                            poolw = sbp("poolw", [128, 4, 128], BF16); bpw = Buf("poolw")
                            ld("pool", poolw[:], ev_pool_w[li].rearrange("g c d -> c g d"), writes=[bpw])
                            pscale = sbp("pscale", [128, 4]); convw = sbp("convw", [128, 3, 4])
                            ld("sp", pscale[:], ev_pool_scale[li].rearrange("(g p) -> p g", p=128), writes=[bpw], allow_slow_non_contiguous=True)
                            ld("sp", convw[:], ev_conv_w[li].rearrange("k (m p) -> p k m", p=128), writes=[bpw], allow_slow_non_contiguous=True)
                            WB = [sbp("wb%d" % i, [128, S + 2 * PAD]) for i in range(4)]
                            bWB = [Buf("wb%d" % i) for i in range(4)]
                            plb = sbp("plb", [128, S], BF16); bplb = Buf("plb")
                            etmp = sbp("etmp", [128, 16]); betmp = Buf("etmp")
                            for i in range(4):
                                sc.op("dve", lambda h, i=i: h.memset(WB[i][:], 0.0), writes=[bWB[i]])

                            def proj(fc, evac):
                                for tc in range(4):
                                    pi = pcnt[0] % 4
                                    pcnt[0] += 1
                                    ps = PS[pi]; bps = bPSh[pi][0]

                                    def f(h, tc=tc, ps=ps):
                                        ins = None
                                        for k in range(8):
                                            ins = h.matmul(ps[:, 0:512], lhsT=win[:, k, fc * 128:(fc + 1) * 128], rhs=xT[:, k, tc * 512:(tc + 1) * 512], start=(k == 0), stop=(k == 7))
                                        return ins
                                    sc.op("pe", f, reads=[bwin, bxT], writes=[bps])
                                    evac(tc, ps[:, 0:512], bps)

                            for g, w in enumerate(POOL_WINDOWS):
                                U, A, B = WB[0], WB[1], WB[2]
                                bU, bA, bB = bWB[0], bWB[1], bWB[2]
                                proj(g, lambda tc, pa_, bps: sc.op("act", lambda h: h.activation(out=U[:, PAD + tc * 512:PAD + (tc + 1) * 512], in_=pa_, func=AF.Copy), reads=[bps], writes=[bU]))
                                lo, n = PAD - 7, S + 14
                                sc.op("dve", lambda h: h.tensor_tensor(out=A[:, lo:lo + n], in0=U[:, lo - 1:lo - 1 + n], in1=U[:, lo:lo + n], op=ALU.add), reads=[bU], writes=[bA])
                                cur, bcur, oth, both = A, bA, B, bB
                                ext = 7
                                ww = 2
                                while ww < w:
                                    sh = ww // 2
                                    ext = ext - sh
                                    lo, n = PAD - ext, S + 2 * ext
                                    sc.op("dve", lambda h, cur=cur, oth=oth, lo=lo, n=n, sh=sh: h.tensor_tensor(out=oth[:, lo:lo + n], in0=cur[:, lo - sh:lo - sh + n], in1=cur[:, lo + sh:lo + sh + n], op=ALU.add), reads=[bcur], writes=[both])
                                    cur, bcur, oth, both = oth, both, cur, bcur
                                    ww *= 2
                                sc.op("dve", lambda h, cur=cur: h.scalar_tensor_tensor(out=plb[:], in0=cur[:, PAD:PAD + S], scalar=1.0 / w, in1=U[:, PAD:PAD + S], op0=ALU.mult, op1=ALU.subtract), reads=[bcur, bU], writes=[bplb])
                                sc.op("dve", lambda h, cur=cur: h.tensor_tensor(out=etmp[:, 0:8], in0=cur[:, PAD:PAD + 8], in1=poolrc[:, g * 16:g * 16 + 8], op=ALU.mult), reads=[bcur, bC], writes=[betmp])
                                sc.op("dve", lambda h, cur=cur: h.tensor_tensor(out=etmp[:, 8:16], in0=cur[:, PAD + S - 8:PAD + S], in1=poolrc[:, g * 16 + 8:g * 16 + 16], op=ALU.mult), reads=[bcur, bC], writes=[betmp])
                                sc.op("dve", lambda h: h.tensor_tensor(out=plb[:, 0:8], in0=etmp[:, 0:8], in1=U[:, PAD:PAD + 8], op=ALU.subtract), reads=[betmp, bU], writes=[bplb])
                                sc.op("dve", lambda h: h.tensor_tensor(out=plb[:, S - 8:S], in0=etmp[:, 8:16], in1=U[:, PAD + S - 8:PAD + S], op=ALU.subtract), reads=[betmp, bU], writes=[bplb])
                                for tc in range(4):
                                    pi = pcnt[0] % 4
                                    pcnt[0] += 1
                                    ps = PS[pi]; bps = bPSh[pi][0]
                                    sc.op("pe", lambda h, ps=ps, tc=tc: h.matmul(ps[:, 0:512], lhsT=poolw[:, g, :], rhs=plb[:, tc * 512:(tc + 1) * 512], start=True, stop=True), reads=[bpw, bplb], writes=[bps])
                                    sc.op("act", lambda h, ps=ps, tc=tc: h.activation(out=catT[:, g, tc * 512:(tc + 1) * 512], in_=ps[:, 0:512], func=AF.Copy, scale=pscale[:, g:g + 1]), reads=[bps, bpw], writes=[bcat])
                            for m in range(4):
                                Cb, U, A = WB[0], WB[3], WB[1]
                                bCb, bU, bA = bWB[0], bWB[3], bWB[1]
                                proj(8 + m, lambda tc, pa_, bps: sc.op("act", lambda h: h.activation(out=Cb[:, PAD + tc * 512:PAD + (tc + 1) * 512], in_=pa_, func=AF.Copy), reads=[bps], writes=[bCb]))
                                proj(12 + m, lambda tc, pa_, bps: sc.op("dve", lambda h: h.tensor_tensor(out=U[:, PAD + tc * 512:PAD + (tc + 1) * 512], in0=Cb[:, PAD + tc * 512:PAD + (tc + 1) * 512], in1=pa_, op=ALU.mult), reads=[bps, bCb], writes=[bU]))
                                sc.op("dve", lambda h: h.tensor_scalar(out=A[:, PAD:PAD + S], in0=U[:, PAD - 1:PAD - 1 + S], scalar1=convw[:, 0, m:m + 1], scalar2=None, op0=ALU.mult), reads=[bU, bpw], writes=[bA])
                                sc.op("dve", lambda h: h.scalar_tensor_tensor(out=A[:, PAD:PAD + S], in0=U[:, PAD:PAD + S], scalar=convw[:, 1, m:m + 1], in1=A[:, PAD:PAD + S], op0=ALU.mult, op1=ALU.add), reads=[bU, bpw, bA], writes=[bA])
                                sc.op("dve", lambda h: h.scalar_tensor_tensor(out=A[:, PAD:PAD + S], in0=U[:, PAD + 1:PAD + 1 + S], scalar=convw[:, 2, m:m + 1], in1=A[:, PAD:PAD + S], op0=ALU.mult, op1=ALU.add), reads=[bU, bpw, bA], writes=[bA])
                                proj(4 + m, lambda tc, pa_, bps: sc.op("dve", lambda h: h.tensor_tensor(out=catT[:, 4 + m, tc * 512:(tc + 1) * 512], in0=A[:, PAD + tc * 512:PAD + (tc + 1) * 512], in1=pa_, op=ALU.mult), reads=[bps, bA], writes=[bcat]))
                        else:
                            win = sbp("win", [128, 8, 2304], BF16); bwin = [Buf("win%d" % k) for k in range(8)]
                            for k in range(8):
                                ld("pool", win[:, k, :], od_w_in[li, k * 128:(k + 1) * 128, :], writes=[bwin[k]], max_dma_last_dim=4096)
                            qT = sbp("qT", [128, S], BF16); bqT = Buf("qT")
                            kTa = sbp("kTa", [128, S], BF16); kTb = sbp("kTb", [128, S], BF16); bkT = Buf("kT")
                            sc.op("dve", lambda h: h.memset(kTa[:], 0.0), writes=[bkT])
                            sc.op("dve", lambda h: h.memset(kTb[:], 0.0), writes=[bkT])
                            VV = sbp("VV", [128, 48, 128], BF16); bVV = Buf("VV")
                            PT = [sbp("PT%d" % i, [128, 16, 384], BF16) for i in range(2)]; bPT = [Buf("PT%d" % i) for i in range(2)]
                            Pr = [sbp("Pr%d" % i, [128, 384], BF16) for i in range(2)]; bPr = [Buf("Pr%d" % i) for i in range(2)]
                            acc_o = sbp("acc_o", [128, S]); bacc_o = Buf("acc_o")
                            acc_d = sbp("acc_d", [128, S]); bacc_d = Buf("acc_d")
                            etab = sbp("etab", [128, 3, 2, 384]); betab = Buf("etab")
                            esink = sbp("esink", [128, 8]); besink = Buf("esink")
                            ld("sp", esink[:], od_sink[li:li + 1, :].to_broadcast([128, 8]), writes=[besink])
                            sc.op("act", lambda h: h.activation(out=esink[:], in_=esink[:], func=AF.Exp), reads=[besink], writes=[besink])
                            ptc = [0]
                            sct = [0]

                            def projT(col0, dst, bdst, dup=False):
                                for tc in range(4):
                                    pi = pcnt[0] % 4
                                    pcnt[0] += 1
                                    ps = PS[pi // 2]; hh = pi % 2; bps = bPSh[pi // 2][hh]

                                    def f(h, tc=tc, ps=ps, hh=hh):
                                        ins = None
                                        for k in range(8):
                                            if not dup:
                                                ins = h.matmul(ps[:, hh * 512:(hh + 1) * 512], lhsT=win[:, k, col0:col0 + 128], rhs=xT[:, k, tc * 512:(tc + 1) * 512], start=(k == 0), stop=(k == 7))
                                            else:
                                                for half in range(2):
                                                    ins = h.matmul(ps[half * 64:(half + 1) * 64, hh * 512:(hh + 1) * 512], lhsT=win[:, k, col0:col0 + 64], rhs=xT[:, k, tc * 512:(tc + 1) * 512], start=(k == 0), stop=(k == 7))
                                        return ins
                                    sc.op("pe", f, reads=[bwin, bxT], writes=[bps])
                                    if dst is None:
                                        sc.op("act", lambda h, ps=ps, hh=hh, tc=tc: h.activation(out=kTa[0:64, tc * 512:(tc + 1) * 512], in_=ps[0:64, hh * 512:(hh + 1) * 512], func=AF.Copy), reads=[bps], writes=[bdst])
                                        sc.op("act", lambda h, ps=ps, hh=hh, tc=tc: h.activation(out=kTb[64:128, tc * 512:(tc + 1) * 512], in_=ps[64:128, hh * 512:(hh + 1) * 512], func=AF.Copy), reads=[bps], writes=[bdst])
                                    else:
                                        sc.op("act", lambda h, ps=ps, hh=hh, tc=tc: h.activation(out=dst[:, tc * 512:(tc + 1) * 512], in_=ps[:, hh * 512:(hh + 1) * 512], func=AF.Copy), reads=[bps], writes=[bdst])

                            def projV(col0, tiles):
                                for (vi, tstart, tstep) in tiles:
                                    pi = pcnt[0] % 4
                                    pcnt[0] += 1
                                    ps = PS[pi // 2]; hh = pi % 2; bps = bPSh[pi // 2][hh]

                                    def f(h, ps=ps, hh=hh, tstart=tstart, tstep=tstep):
                                        ins = None
                                        for k in range(8):
                                            ins = h.matmul(ps[:, hh * 512:hh * 512 + 128], lhsT=xT[:, k, tstart:tstart + 127 * tstep + 1:tstep], rhs=win[:, k, col0:col0 + 128], start=(k == 0), stop=(k == 7))
                                        return ins
                                    sc.op("pe", f, reads=[bwin, bxT], writes=[bps])
                                    sc.op("act", lambda h, ps=ps, hh=hh, vi=vi: h.activation(out=VV[:, vi, :], in_=ps[:, hh * 512:hh * 512 + 128], func=AF.Copy), reads=[bps], writes=[bVV])

                            def att_scores(job):
                                hb, vcol, tsel, pidx, (dil, vvb, W, rad), first, jn = job
                                if True:
                                    Ls = S // dil
                                    ntile = Ls // 128
                                    pt = PT[jn % 2]; bpt = bPT[jn % 2]
                                    tb = etab[:, tsel, hb // 64, :]
                                    for r in range(dil):
                                        for j in range(ntile):
                                            q0 = 128 * j - rad
                                            c_lo = max(0, -q0)
                                            c_hi = min(W, Ls - q0)
                                            nq = c_hi - c_lo
                                            si = sct[0] % 4
                                            sct[0] += 1
                                            ps = PS[si // 2]; hh = si % 2; bps = bPSh[si // 2][hh]
                                            kst = r + dil * (128 * j)
                                            qst = r + dil * (q0 + c_lo)
                                            sc.op("pe", lambda h, ps=ps, hh=hh, kst=kst, qst=qst, nq=nq: h.matmul(
                                                ps[:, hh * 512:hh * 512 + nq],
                                                lhsT=(kTa if hb == 0 else kTb)[:, kst:kst + 127 * dil + 1:dil],
                                                rhs=qT[:, qst:qst + (nq - 1) * dil + 1:dil], start=True, stop=True),
                                                reads=[bkT, bqT], writes=[bps])
                                            pr = Pr[si % 2]; bpr = bPr[si % 2]
                                            sc.op("act", lambda h, ps=ps, hh=hh, nq=nq, pr=pr: h.activation(out=pr[:, 0:nq], in_=ps[:, hh * 512:hh * 512 + nq], func=AF.Exp, scale=0.125), reads=[bps], writes=[bpr])
                                            ti = r * ntile + j
                                            sc.op("dve", lambda h, pr=pr, nq=nq, c_lo=c_lo, ti=ti: h.tensor_tensor(out=pt[:, ti, c_lo:c_lo + nq], in0=pr[:, 0:nq], in1=tb[:, c_lo:c_lo + nq], op=ALU.mult), reads=[bpr, betab], writes=[bpt])
                            def att_pv(job):
                                hb, vcol, tsel, pidx, (dil, vvb, W, rad), first, jn = job
                                if True:
                                    Ls = S // dil
                                    ntile = Ls // 128
                                    pt = PT[jn % 2]; bpt = bPT[jn % 2]
                                    QB = rad
                                    nqb = 512 // QB
                                    for ch in range(S // 512):
                                        hh = ch % 2
                                        pso, bpso = PS[2], bPSh[2][hh]
                                        psd, bpsd = PS[3], bPSh[3][hh]

                                        def fpv(h, which, ps, ch=ch, hh=hh):
                                            ins = None
                                            for b in range(nqb):
                                                gq = ch * 512 + b * QB
                                                r = gq // Ls
                                                l0 = gq % Ls
                                                parts = []
                                                for j in range(ntile):
                                                    k_lo = max(128 * j, l0 - rad)
                                                    k_hi = min(128 * j + 128, l0 + QB + rad)
                                                    if k_hi <= k_lo:
                                                        continue
                                                    parts.append((j, k_lo - 128 * j, k_hi - 128 * j))
                                                parts = [(j, 0, 128) for (j, _a, _b) in parts]
                                                for n_, (j, p_lo, p_hi) in enumerate(parts):
                                                    ti = r * ntile + j
                                                    col = l0 - (128 * j - rad)
                                                    if which == 0:
                                                        lhsT = VV[p_lo:p_hi, vvb + ti, vcol:vcol + 64]
                                                    else:
                                                        lhsT = ones_bf[p_lo:p_hi, 0:64]
                                                    ins = h.matmul(ps[hb:hb + 64, hh * 512 + b * QB:hh * 512 + (b + 1) * QB], lhsT=lhsT,
                                                                   rhs=pt[p_lo:p_hi, ti, col:col + QB], start=(n_ == 0), stop=(n_ == len(parts) - 1))
                                            return ins
                                        sc.op("pe", lambda h, pso=pso: fpv(h, 0, pso), reads=[bVV, bpt], writes=[bpso])
                                        sc.op("pe", lambda h, psd=psd: fpv(h, 1, psd), reads=[bC, bpt], writes=[bpsd])
                                        if dil == 1:
                                            dso = acc_o[hb:hb + 64, ch * 512:(ch + 1) * 512]
                                            dsd = acc_d[hb:hb + 64, ch * 512:(ch + 1) * 512]
                                            srco = pso[hb:hb + 64, hh * 512:(hh + 1) * 512]
                                            srcd = psd[hb:hb + 64, hh * 512:(hh + 1) * 512]
                                        else:
                                            nr = 512 // Ls if Ls < 512 else 1
                                            if Ls >= 512:
                                                r = (ch * 512) // Ls
                                                l0 = (ch * 512) % Ls
                                                st_ = r + dil * l0
                                                dso = acc_o[hb:hb + 64, st_:st_ + 511 * dil + 1:dil]
                                                dsd = acc_d[hb:hb + 64, st_:st_ + 511 * dil + 1:dil]
                                                srco = pso[hb:hb + 64, hh * 512:(hh + 1) * 512]
                                                srcd = psd[hb:hb + 64, hh * 512:(hh + 1) * 512]
                                            else:
                                                r0 = (ch * 512) // Ls
                                                dso = acc_o[hb:hb + 64, :].rearrange("p (l r) -> p r l", r=dil)[:, r0:r0 + nr, :]
                                                dsd = acc_d[hb:hb + 64, :].rearrange("p (l r) -> p r l", r=dil)[:, r0:r0 + nr, :]
                                                srco = pso[hb:hb + 64, hh * 512:(hh + 1) * 512].rearrange("p (r l) -> p r l", r=nr)
                                                srcd = psd[hb:hb + 64, hh * 512:(hh + 1) * 512].rearrange("p (r l) -> p r l", r=nr)
                                        if first and pidx == 0:
                                            sc.op("act", lambda h, dso=dso, srco=srco: h.activation(out=dso, in_=srco, func=AF.Copy), reads=[bpso], writes=[bacc_o])
                                            sc.op("act", lambda h, dsd=dsd, srcd=srcd: h.activation(out=dsd, in_=srcd, func=AF.Copy), reads=[bpsd], writes=[bacc_d])
                                        else:
                                            sc.op("dve", lambda h, dso=dso, srco=srco: h.tensor_tensor(out=dso, in0=dso, in1=srco, op=ALU.add), reads=[bpso, bacc_o], writes=[bacc_o])
                                            sc.op("dve", lambda h, dsd=dsd, srcd=srcd: h.tensor_tensor(out=dsd, in0=dsd, in1=srcd, op=ALU.add), reads=[bpsd, bacc_d], writes=[bacc_d])


                            def attend_all(jobs):
                                prev = None
                                for job in jobs:
                                    att_scores(job)
                                    if prev is not None:
                                        att_pv(prev)
                                    prev = job
                                att_pv(prev)

                            def finish(chunk, sink_heads=None):
                                if sink_heads is not None:
                                    for half, hd_ in enumerate(sink_heads):
                                        sc.op("dve", lambda h, half=half, hd_=hd_: h.tensor_scalar(out=acc_d[half * 64:(half + 1) * 64, :], in0=acc_d[half * 64:(half + 1) * 64, :], scalar1=esink[half * 64:(half + 1) * 64, hd_:hd_ + 1], scalar2=None, op0=ALU.add), reads=[bacc_d, besink], writes=[bacc_d])
                                sc.op("dve", lambda h: h.reciprocal(out=acc_d[:], in_=acc_d[:]), reads=[bacc_d], writes=[bacc_d])
                                sc.op("pool", lambda h: h.tensor_tensor(out=catT[:, chunk, :], in0=acc_o[:], in1=acc_d[:], op=ALU.mult), reads=[bacc_o, bacc_d], writes=[bcat])

                            for c in range(4):
                                ld("sp", etab[:, :, :, 0:256], cst["edec_c"][:, :, 2 * c:2 * c + 2, :], writes=[betab])
                                projT(0 + c * 128, qT, bqT)
                                projT(512 + c * 128, None, bkT)
                                tiles = []
                                vvb = {}
                                vi = 0
                                for (_, dil) in C_PATTERNS:
                                    vvb[dil] = vi
                                    Ls = S // dil
                                    for r in range(dil):
                                        for j in range(Ls // 128):
                                            tiles.append((vi, r + dil * 128 * j, dil))
                                            vi += 1
                                projV(1024 + c * 128, tiles)
                                pats = [(dil, vvb[dil], 256, 64) for (_, dil) in C_PATTERNS]
                                jobs = []
                                for half in range(2):
                                    for pidx, pat in enumerate(pats):
                                        jobs.append((half * 64, half * 64, pidx, pidx, pat, True, ptc[0]))
                                        ptc[0] += 1
                                attend_all(jobs)
                                finish(c)
                            for c in range(4):
                                g = c // 2
                                ld("sp", etab[:, 0, :, :], cst["edec_d"][:, 2 * c:2 * c + 2, :], writes=[betab])
                                projT(1536 + c * 128, qT, bqT)
                                projT(2048 + g * 64, None, bkT, dup=True)
                                projV(2176, [(j, 128 * j, 1) for j in range(16)])
                                jobs = []
                                for half in range(2):
                                    jobs.append((half * 64, g * 64, 0, 0, (1, 0, 384, 128), True, ptc[0]))
                                    ptc[0] += 1
                                attend_all(jobs)
                                finish(4 + c, sink_heads=(2 * c, 2 * c + 1))
                        sc.barrier()
                    with ExitStack() as pb:
                        sbp = lambda name, shape, dt=F32: pb.enter_context(nc.sbuf_tensor(UQ(name), list(shape), dt))
                        gtab = sbp("gtab", [128, D]); btab = sbp("btab", [128, D]); bgb = Buf("gb")
                        ld("sp", gtab[:], ln_g[L, 0:1, :].to_broadcast([128, D]), writes=[bgb])
                        ld("sp", btab[:], ln_b[L, 0:1, :].to_broadcast([128, D]), writes=[bgb])
                        rw = sbp("rw", [128, 8, NE]); brw = Buf("rw")
                        ld("sp", rw[:], router_w[L].rearrange("(k p) e -> p k e", p=128), writes=[brw])
                        rb = sbp("rb", [128, NE])
                        ld("sp", rb[:], router_b[L:L + 1, :].to_broadcast([128, NE]), writes=[brw])
                        wout = sbp("wout", [128, 8, D], BF16); bwout = [Buf("wout%d" % k) for k in range(8)]
                        w_out_src = (ev_w_out if L % 2 == 0 else od_w_out)[li]
                        for k in range(8):
                            ld("pool", wout[:, k, :], w_out_src[k * 128:(k + 1) * 128, :], writes=[bwout[k]], max_dma_last_dim=4096)
                        lnt = {"st": sbp("st", [128, 2, 6]), "mv": sbp("mv", [128, 2]), "rstd": sbp("rstd", [128, 1]), "bst": Buf("st")}
                        zt = [sbp("zt%d" % i, [128, D]) for i in range(2)]; bzt = [Buf("zt%d" % i) for i in range(2)]
                        xres, bxres, x1t, bx1t = zt, bzt, zt, bzt
                        x1b = [sbp("x1b%d" % i, [128, D], BF16) for i in range(2)]; bx1b = [Buf("x1b%d" % i) for i in range(2)]
                        x1T = sbp("x1T", [128, 8, 128]); bx1T = Buf("x1T")
                        rsm = {k: sbp("r_" + k, [128, n]) for k, n in (("nmax", 1), ("ex", NE), ("msk", NE), ("ssum", 1))}
                        brs = Buf("rsm")
                        for tt in range(S // 128):
                            i = s * (S // 128) + tt
                            a = i % 2
                            ps = PS[a]; bps = bPS[a]

                            def f(h, ps=ps, tt=tt):
                                ins = None
                                for n2 in range(2):
                                    for k in range(8):
                                        ins = h.matmul(ps[:, n2 * 512:(n2 + 1) * 512], lhsT=catT[:, k, tt * 128:(tt + 1) * 128], rhs=wout[:, k, n2 * 512:(n2 + 1) * 512], start=(k == 0), stop=(k == 7))
                                return ins
                            sc.op("pe", f, reads=[bcat, bwout], writes=[bps])
                            ld("sp", xres[a][:], Xcur[i * 128:(i + 1) * 128, :], reads=[bXcur], writes=[bxres[a]])
                            sc.op("dve", lambda h, a=a, ps=ps: h.scalar_tensor_tensor(out=zt[a][:], in0=xres[a][:], scalar=ALPHA, in1=ps[:], op0=ALU.mult, op1=ALU.add), reads=[bxres[a], bps], writes=[bzt[a]])
                            layer_norm_tile(zt[a], bzt[a], gtab, btab, bgb, x1t[a], bx1t[a], lnt)
                            ld("sp", X1[i * 128:(i + 1) * 128, :], x1t[a][:], reads=[bx1t[a]], writes=[])
                            sc.op("act", lambda h, a=a: h.activation(out=x1b[a][:], in_=x1t[a][:], func=AF.Copy), reads=[bx1t[a]], writes=[bx1b[a]])
                            ld("sp", XS[i * 128:(i + 1) * 128, :], x1b[a][:], reads=[bx1b[a]], writes=[])
                            ps2 = PS[2 + a]; bps2 = bPS[2 + a]

                            def f2(h, a=a, ps2=ps2):
                                ins = None
                                for k in range(8):
                                    ins = h.transpose(out=ps2[:, k * 128:(k + 1) * 128], in_=x1t[a][:, k * 128:(k + 1) * 128], identity=ident[:])
                                return ins
                            sc.op("pe", f2, reads=[bx1t[a], bC], writes=[bps2])
                            sc.op("act", lambda h, ps2=ps2: h.activation(out=x1T[:].rearrange("p k t -> p (k t)"), in_=ps2[:], func=AF.Copy), reads=[bps2], writes=[bx1T])

                            def f3(h, ps2=ps2):
                                ins = None
                                for k in range(8):
                                    ins = h.matmul(ps2[:, 0:NE], lhsT=x1T[:, k, :], rhs=rw[:, k, :], start=(k == 0), stop=(k == 7))
                                return ins
                            sc.op("pe", f3, reads=[bx1T, brw], writes=[bps2])
                            Li = L_all[:, i, :]
                            sc.op("dve", lambda h, ps2=ps2, Li=Li: h.tensor_tensor(out=Li, in0=ps2[:, 0:NE], in1=rb[:], op=ALU.add), reads=[bps2, brw], writes=[bL])
                            m8 = M8_all[:, i, :]
                            sc.op("dve", lambda h, Li=Li, m8=m8: h.max(out=m8, in_=Li), reads=[bL], writes=[bM8])
                            sc.op("dve", lambda h, Li=Li, m8=m8: h.tensor_scalar(out=rsm["msk"][:], in0=Li, scalar1=m8[:, 3:4], scalar2=None, op0=ALU.is_ge), reads=[bL, bM8], writes=[brs])
                            sc.op("dve", lambda h, m8=m8: h.tensor_scalar(out=rsm["nmax"][:], in0=m8[:, 0:1], scalar1=-1.0, scalar2=None, op0=ALU.mult), reads=[bM8], writes=[brs])
                            sc.op("act", lambda h, Li=Li: h.activation(out=rsm["ex"][:], in_=Li, func=AF.Exp, bias=rsm["nmax"][:, 0:1]), reads=[bL, brs], writes=[brs])
                            sc.op("dve", lambda h: h.scalar_tensor_tensor(out=rsm["ex"][:], in0=rsm["ex"][:], scalar=1.0, in1=rsm["msk"][:], op0=ALU.mult, op1=ALU.mult, accum_out=rsm["ssum"][:]), reads=[brs], writes=[brs])
                            sc.op("dve", lambda h: h.reciprocal(out=rsm["ssum"][:], in_=rsm["ssum"][:]), reads=[brs], writes=[brs])
                            sc.op("dve", lambda h, i=i: h.tensor_scalar(out=G_all[:, i, :], in0=rsm["ex"][:], scalar1=rsm["ssum"][:, 0:1], scalar2=None, op0=ALU.mult), reads=[brs], writes=[bG])
                            sc.op("dve", lambda h, i=i: h.tensor_copy(out=M_all[:, i * NE:(i + 1) * NE], in_=rsm["msk"][:]), reads=[brs], writes=[bM])
                        sc.barrier()
            if stop_after == (L, "mix"):
                break

            with ExitStack() as p1b:
                sbp = lambda name, shape, dt=F32: p1b.enter_context(nc.sbuf_tensor(UQ(name), list(shape), dt))
                pos = sbp("pos", [128, NT, NE]); bpos = Buf("pos")
                carry = sbp("carry", [128, NT, NE]); bcar = Buf("carry")
                tot = sbp("tot", [128, NT, NE]); btot = Buf("tot")
                oh = sbp("oh", [128, NT, NE]); boh = Buf("oh")
                prod = sbp("prod", [128, NT, NE]); bprod = Buf("prod")
                slkf = sbp("slkf", [128, TOPK, NT]); bslkf = Buf("slkf")
                gk = sbp("gk", [128, TOPK, NT]); bgk = Buf("gk")
                sinit = sbp("sinit", [128, (NSLOT + 128) // 128, 2], I32); bsin = Buf("sinit")
                ecap = sbp("ecap", [128, NT * NE]); dumpt = sbp("dumpt", [128, NT * NE])
                ld("sp", ecap[:], cst["ecap"], writes=[bC])
                ld("sp", dumpt[:], cst["dump"], writes=[bC])
                ld("sp", sinit[:], cst["slotinit"], writes=[bsin])
                ld("sp", SLOT.rearrange("(j p) c -> p j c", p=128), sinit[:], reads=[bsin], writes=[dSLOT])

                def fW(h):
                    ins = None
                    for n2 in range(2):
                        ins = h.matmul(PS[0][:, n2 * 512:(n2 + 1) * 512], lhsT=ustrict_bf[:], rhs=M_all[:, n2 * 512:(n2 + 1) * 512], start=True, stop=True)
                    return ins
                sc.op("pe", fW, reads=[bC, bM], writes=[bPS[0]])

                def fT(h):
                    ins = None
                    for n2 in range(2):
                        ins = h.matmul(PS[1][:, n2 * 512:(n2 + 1) * 512], lhsT=ones_bf[:], rhs=M_all[:, n2 * 512:(n2 + 1) * 512], start=True, stop=True)
                    return ins
                sc.op("pe", fT, reads=[bC, bM], writes=[bPS[1]])
                sc.op("act", lambda h: h.activation(out=tot[:].rearrange("p a b -> p (a b)"), in_=PS[1][:], func=AF.Copy), reads=[bPS[1]], writes=[btot])
                sc.op("dve", lambda h: h.memset(carry[:, 0, :], 0.0), writes=[bcar])
                for i in range(1, NT):
                    sc.op("dve", lambda h, i=i: h.tensor_tensor(out=carry[:, i, :], in0=carry[:, i - 1, :], in1=tot[:, i - 1, :], op=ALU.add), reads=[bcar, btot], writes=[bcar])
                fl = lambda t: t[:].rearrange("p a b -> p (a b)")
                sc.op("dve", lambda h: h.tensor_tensor(out=fl(pos), in0=fl(carry), in1=PS[0][:], op=ALU.add), reads=[bcar, bPS[0]], writes=[bpos])
                sc.op("dve", lambda h: h.tensor_scalar(out=fl(oh), in0=fl(pos), scalar1=float(CAP) - 0.5, scalar2=None, op0=ALU.is_lt), reads=[bpos], writes=[boh])
                sc.op("dve", lambda h: h.tensor_tensor(out=fl(oh), in0=fl(oh), in1=M_all[:], op=ALU.mult), reads=[boh, bM], writes=[boh])
                sc.op("dve", lambda h: h.tensor_tensor(out=fl(pos), in0=fl(pos), in1=ecap[:], op=ALU.add), reads=[bpos, bC], writes=[bpos])
                sc.op("dve", lambda h: h.tensor_tensor(out=fl(pos), in0=fl(pos), in1=dumpt[:], op=ALU.subtract), reads=[bpos, bC], writes=[bpos])
                sc.op("dve", lambda h: h.tensor_tensor(out=fl(pos), in0=fl(pos), in1=fl(oh), op=ALU.mult), reads=[bpos, boh], writes=[bpos])
                sc.op("dve", lambda h: h.tensor_tensor(out=fl(pos), in0=fl(pos), in1=dumpt[:], op=ALU.add), reads=[bpos, bC], writes=[bpos])
                for k in range(TOPK):
                    sc.op("dve", lambda h, k=k: h.tensor_tensor(out=oh[:], in0=L_all[:], in1=M8_all[:, :, k:k + 1].to_broadcast([128, NT, NE]), op=ALU.is_equal), reads=[bL, bM8], writes=[boh])
                    sc.op("dve", lambda h: h.tensor_tensor(out=fl(prod), in0=fl(oh), in1=fl(pos), op=ALU.mult), reads=[boh, bpos], writes=[bprod])
                    sc.op("dve", lambda h, k=k: h.tensor_reduce(out=slkf[:, k, :], in_=prod[:], axis=AX.X, op=ALU.add), reads=[bprod], writes=[bslkf])
                    sc.op("dve", lambda h: h.tensor_tensor(out=fl(prod), in0=fl(oh), in1=fl(G_all), op=ALU.mult), reads=[boh, bG], writes=[bprod])
                    sc.op("dve", lambda h, k=k: h.tensor_reduce(out=gk[:, k, :], in_=prod[:], axis=AX.X, op=ALU.add), reads=[bprod], writes=[bgk])
                sc.op("dve", lambda h: h.tensor_copy(out=SLK[:], in_=slkf[:]), reads=[bslkf], writes=[bSLK])
                for k in range(TOPK):
                    sc.op("dve", lambda h, k=k: h.tensor_copy(out=PAY[:, :, k, 0], in_=tokidx[:]), reads=[bC], writes=[bPAY])
                    sc.op("dve", lambda h, k=k: h.tensor_copy(out=PAY[:, :, k, 1], in_=gk[:, k, :].bitcast(I32)), reads=[bgk], writes=[bPAY])
                for i in range(NT):
                    for k in range(TOPK):
                        sc.dma("pool", lambda h, i=i, k=k: h.indirect_dma_start(
                            out=SLOT, out_offset=bass.IndirectOffsetOnAxis(ap=SLK[:, k, i:i + 1], axis=0),
                            in_=PAY[:, i, k, :], in_offset=None), reads=[bSLK, bPAY, dSLOT], writes=[])
                sc.barrier()
            if stop_after == (L, "route"):
                break

            with ExitStack() as p2:
                sbp = lambda name, shape, dt=F32: p2.enter_context(nc.sbuf_tensor(UQ(name), list(shape), dt))
                wgu = [sbp("wgu%d" % i, [128, 8, 2048], BF16) for i in range(2)]; bwgu = [[Buf("wgu%d_%d" % (i, k)) for k in range(8)] for i in range(2)]
                wdn = [sbp("wdn%d" % i, [128, 8, D], BF16) for i in range(2)]; bwdn = [[Buf("wdn%d_%d" % (i, k)) for k in range(8)] for i in range(2)]
                braw = sbp("braw", [NE, 2048]); bbraw = Buf("braw")
                bgu = sbp("bgu", [128, 16, NE]); bbgu = Buf("bgu")
                idx = [sbp("idx%d" % i, [128, NJ, 2], I32) for i in range(4)]; bidx = [Buf("idx%d" % i) for i in range(4)]
                xg = [sbp("xg%d" % i, [128, D], BF16) for i in range(NJ)]; bxg = [Buf("xg%d" % i) for i in range(NJ)]
                xgT = [sbp("xgT%d" % i, [128, 8, CAP], BF16) for i in range(2)]; bxgT = [Buf("xgT%d" % i) for i in range(2)]
                actT = sbp("actT", [128, 8, CAP], BF16); bactT = Buf("actT")
                NH = CAP // 2
                eg = [sbp("eg%d" % i, [128, NH]) for i in range(4)]; beg = [Buf("eg%d" % i) for i in range(4)]
                esg = [sbp("esg%d" % i, [128, NH]) for i in range(4)]; besg = [Buf("esg%d" % i) for i in range(4)]
                el = [sbp("el%d" % i, [128, NH]) for i in range(4)]; bel = [Buf("el%d" % i) for i in range(4)]
                ysb = [sbp("ysb%d" % i, [128, D]) for i in range(2)]; bysb = [Buf("ysb%d" % i) for i in range(2)]

                ld("sp", braw[:], exp_b_gu[L], writes=[bbraw])

                def fb(h):
                    ins = None
                    for m in range(8):
                        for two in range(2):
                            c0 = (two * 8 + m) * NE
                            src = braw[:, :].rearrange("e (c two) -> e two c", two=2)[:, two, m * 128:(m + 1) * 128]
                            ins = h.transpose(out=PS[0][:, c0:c0 + NE], in_=src, identity=ident[0:NE, 0:NE])
                    return ins
                sc.op("pe", fb, reads=[bbraw, bC], writes=[bPS[0]])
                sc.op("act", lambda h: h.activation(out=bgu[:].rearrange("p a b -> p (a b)"), in_=PS[0][:, 0:16 * NE], func=AF.Copy), reads=[bPS[0]], writes=[bbgu])

                def load_wgu(e):
                    a = e % 2
                    for k in range(8):
                        ld("pool", wgu[a][:, k, :], exp_w_gu[L, e, k * 128:(k + 1) * 128, :], writes=[bwgu[a][k]], max_dma_last_dim=4096)

                def load_wdn(e):
                    a = e % 2
                    for k in range(8):
                        ld("pool", wdn[a][:, k, :], exp_w_down[L, e, k * 128:(k + 1) * 128, :], writes=[bwdn[a][k]], max_dma_last_dim=4096)

                def idx_load(e):
                    a4 = e % 4
                    ld("sp", idx[a4][:], SLOT[e * CAP:(e + 1) * CAP, :].rearrange("(j p) c -> p j c", p=128), reads=[dSLOT], writes=[bidx[a4]])

                def gather_issue(e):
                    a3 = e % 4
                    for j in range(NJ):
                        sc.dma("pool", lambda h, a3=a3, j=j: h.indirect_dma_start(
                            out=xg[j][:], out_offset=None, in_=XS,
                            in_offset=bass.IndirectOffsetOnAxis(ap=idx[a3][:, j, 0:1].bitcast(U32), axis=0)),
                            reads=[bidx[a3], dXS], writes=[bxg[j]])

                tcount = [0]

                def transpose_x(e, j):
                    a = e % 2
                    pi = 2 + (tcount[0] % 2)
                    tcount[0] += 1
                    psb = PS[pi][:].bitcast(BF16)

                    def ft(h, psb=psb):
                        ins = None
                        for k in range(8):
                            ins = h.transpose(out=psb[:, k * 128:(k + 1) * 128], in_=xg[j][:, k * 128:(k + 1) * 128], identity=ident_bf[:])
                        return ins
                    sc.op("pe", ft, reads=[bxg[j], bC], writes=[bPS[pi]])
                    sc.op("act", lambda h, psb=psb: h.activation(out=xgT[a][:, :, j * 128:(j + 1) * 128], in_=psb[:, 0:1024].rearrange("p (k t) -> p k t", k=8), func=AF.Copy), reads=[bPS[pi]], writes=[bxgT[a]])

                idx_load(0)
                idx_load(1)
                load_wgu(0)
                gather_issue(0)
                load_wdn(0)
                for j in range(NJ):
                    transpose_x(0, j)
                ycount = [0]
                for e in range(NE):
                    a = e % 2
                    a3 = e % 4
                    if e + 2 < NE:
                        idx_load(e + 2)
                    if e + 1 < NE:
                        load_wgu(e + 1)
                        gather_issue(e + 1)
                        load_wdn(e + 1)
                    pending = []
                    pair = 0
                    for m in range(8):
                        for nh in range(2):
                            n0 = nh * NH
                            q = pair % 2
                            psg, bpsg = PS[0], bPSh[0][q]
                            psl, bpsl = PS[1], bPSh[1][q]

                            def fg(h, two, ps, m=m, n0=n0, q=q):
                                ins = None
                                wv = wgu[a][:].rearrange("p k (c two) -> p k two c", two=2)
                                for k in range(8):
                                    ins = h.matmul(ps[:, q * 512:q * 512 + NH], lhsT=wv[:, k, two, m * 128:(m + 1) * 128], rhs=xgT[a][:, k, n0:n0 + NH], start=(k == 0), stop=(k == 7))
                                return ins
                            ei = pair % 4
                            sc.op("pe", lambda h, fg=fg, psg=psg: fg(h, 0, psg), reads=[bwgu[a], bxgT[a]], writes=[bpsg])
                            sc.op("pe", lambda h, fg=fg, psl=psl: fg(h, 1, psl), reads=[bwgu[a], bxgT[a]], writes=[bpsl])
                            pg = psg[:, q * 512:q * 512 + NH]
                            pl = psl[:, q * 512:q * 512 + NH]
                            sc.op("dve", lambda h, pg=pg, ei=ei, m=m, e=e: h.tensor_scalar(out=eg[ei][:], in0=pg, scalar1=bgu[:, m, e:e + 1], scalar2=SW_LIMIT, op0=ALU.add, op1=ALU.min), reads=[bpsg, bbgu], writes=[beg[ei]])
                            sc.op("act", lambda h, pl=pl, ei=ei, m=m, e=e: h.activation(out=el[ei][:], in_=pl, func=AF.Identity, bias=bgu[:, 8 + m, e:e + 1]), reads=[bpsl, bbgu], writes=[bel[ei]])
                            sc.op("act", lambda h, ei=ei: h.activation(out=esg[ei][:], in_=eg[ei][:], func=AF.Sigmoid, scale=SW_ALPHA), reads=[beg[ei]], writes=[besg[ei]])
                            sc.op("pool", lambda h, ei=ei: h.tensor_tensor(out=eg[ei][:], in0=eg[ei][:], in1=esg[ei][:], op=ALU.mult), reads=[beg[ei], besg[ei]], writes=[beg[ei]])
                            for fn in pending:
                                fn()
                            pending = []

                            def fin(ei=ei, m=m, n0=n0):
                                sc.op("dve", lambda h: h.tensor_scalar(out=el[ei][:], in0=el[ei][:], scalar1=SW_LIMIT, scalar2=-SW_LIMIT, op0=ALU.min, op1=ALU.max), reads=[bel[ei]], writes=[bel[ei]])
                                sc.op("dve", lambda h: h.scalar_tensor_tensor(out=actT[:, m, n0:n0 + NH], in0=el[ei][:], scalar=1.0, in1=eg[ei][:], op0=ALU.add, op1=ALU.mult), reads=[beg[ei], bel[ei]], writes=[bactT])
                            pending.append(fin)
                            if e + 1 < NE and pair in (3, 5, 7, 9, 11, 13):
                                transpose_x(e + 1, (pair - 3) // 2)
                            pair += 1
                    for fn in pending:
                        fn()
                    for j in range(NJ):
                        pi = 2 + (ycount[0] % 2)
                        yb = ycount[0] % 2
                        ycount[0] += 1
                        ps, bps = PS[pi], bPS[pi]

                        def fd(h, ps=ps, j=j):
                            ins = None
                            for n2 in range(2):
                                for k in range(8):
                                    ins = h.matmul(ps[:, n2 * 512:(n2 + 1) * 512], lhsT=actT[:, k, j * 128:(j + 1) * 128], rhs=wdn[a][:, k, n2 * 512:(n2 + 1) * 512], start=(k == 0), stop=(k == 7))
                            return ins
                        sc.op("pe", fd, reads=[bactT, bwdn[a]], writes=[bps])
                        sc.op("act", lambda h, ps=ps, yb=yb, j=j: h.activation(out=ysb[yb][:], in_=ps[:], func=AF.Copy, scale=idx[a3][:, j, 1:2].bitcast(F32)), reads=[bps, bidx[a3]], writes=[bysb[yb]])
                        ld("sp", YS[e * CAP + j * 128:e * CAP + (j + 1) * 128, :], ysb[yb][:], reads=[bysb[yb]], writes=[])
                sc.barrier()
            if stop_after == (L, "moe"):
                break

            with ExitStack() as p3:
                sbp = lambda name, shape, dt=F32: p3.enter_context(nc.sbuf_tensor(UQ(name), list(shape), dt))
                gtab = sbp("gtab2", [128, D]); btab = sbp("btab2", [128, D]); bgb = Buf("gb2")
                ld("sp", gtab[:], ln_g[L, 1:2, :].to_broadcast([128, D]), writes=[bgb])
                ld("sp", btab[:], ln_b[L, 1:2, :].to_broadcast([128, D]), writes=[bgb])
                bd = sbp("bd", [NE, D]); bbd = Buf("bd")
                ld("sp", bd[:], exp_b_down[L], writes=[bbd])
                lnt = {"st": sbp("st2", [128, 2, 6]), "mv": sbp("mv2", [128, 2]), "rstd": sbp("rstd2", [128, 1]), "bst": Buf("st2")}
                yk = [[sbp("yk%d_%d" % (a, k), [128, D]) for k in range(TOPK)] for a in range(2)]
                byk = [[Buf("yk%d_%d" % (a, k)) for k in range(TOPK)] for a in range(2)]
                x1r = [sbp("x1r%d" % a, [128, D]) for a in range(2)]; bx1r = [Buf("x1r%d" % a) for a in range(2)]
                zt = [sbp("z2_%d" % a, [128, D]) for a in range(2)]; bzt = [Buf("z2_%d" % a) for a in range(2)]
                x2t = [sbp("x2t%d" % a, [128, D]) for a in range(2)]; bx2t = [Buf("x2t%d" % a) for a in range(2)]
                xtb = [sbp("xtb%d" % a, [128, 8, 128], BF16) for a in range(2)]; bxtb = [Buf("xtb%d" % a) for a in range(2)]
                GT = sbp("GT", [NE, 128]); bGT = Buf("GT")
                Xnext = y_out if last else XA
                dXn = dY if last else dXA
                for i in range(NT):
                    a = i % 2
                    for k in range(TOPK):
                        sc.dma("pool", lambda h, a=a, k=k, i=i: h.indirect_dma_start(
                            out=yk[a][k][:], out_offset=None, in_=YS,
                            in_offset=bass.IndirectOffsetOnAxis(ap=SLK[:, k, i:i + 1], axis=0)),
                            reads=[bSLK, dYS], writes=[byk[a][k]])
                    ld("sp", x1r[a][:], X1[i * 128:(i + 1) * 128, :], reads=[dX1], writes=[bx1r[a]])
                    ps, bps = PS[a], bPS[a]
                    sc.op("pe", lambda h, ps=ps, i=i: h.transpose(out=ps[0:NE, 0:128], in_=G_all[:, i, :], identity=ident[:]), reads=[bG, bC], writes=[bps])
                    sc.op("act", lambda h, ps=ps: h.activation(out=GT[:], in_=ps[0:NE, 0:128], func=AF.Copy), reads=[bps], writes=[bGT])

                    def fgb(h, ps=ps):
                        ins = None
                        for n2 in range(2):
                            ins = h.matmul(ps[:, n2 * 512:(n2 + 1) * 512], lhsT=GT[:], rhs=bd[:, n2 * 512:(n2 + 1) * 512], start=True, stop=True)
                        return ins
                    sc.op("pe", fgb, reads=[bGT, bbd], writes=[bps])
                    sc.op("dve", lambda h, a=a, ps=ps: h.scalar_tensor_tensor(out=zt[a][:], in0=x1r[a][:], scalar=ALPHA, in1=ps[:], op0=ALU.mult, op1=ALU.add), reads=[bx1r[a], bps], writes=[bzt[a]])
                    sc.op("pool", lambda h, a=a: h.tensor_tensor(out=yk[a][0][:], in0=yk[a][0][:], in1=yk[a][1][:], op=ALU.add), reads=[byk[a][0], byk[a][1]], writes=[byk[a][0]])
                    sc.op("dve", lambda h, a=a: h.tensor_tensor(out=zt[a][:], in0=zt[a][:], in1=yk[a][2][:], op=ALU.add), reads=[bzt[a], byk[a][2]], writes=[bzt[a]])
                    sc.op("dve", lambda h, a=a: h.tensor_tensor(out=zt[a][:], in0=zt[a][:], in1=yk[a][3][:], op=ALU.add), reads=[bzt[a], byk[a][3]], writes=[bzt[a]])
                    sc.op("dve", lambda h, a=a: h.tensor_tensor(out=zt[a][:], in0=zt[a][:], in1=yk[a][0][:], op=ALU.add), reads=[bzt[a], byk[a][0]], writes=[bzt[a]])
                    layer_norm_tile(zt[a], bzt[a], gtab, btab, bgb, x2t[a], bx2t[a], lnt)
                    ld("sp", Xnext[i * 128:(i + 1) * 128, :], x2t[a][:], reads=[bx2t[a]], writes=[])
                    if not last:
                        transpose_to_XT(x2t[a], bx2t[a], i, PS[2 + a], bPS[2 + a], xtb[a], bxtb[a])
                sc.barrier()
            Xcur = XA
            bXcur = dXA
        sc.barrier()
    return nc


def kernel(**inputs):
    global CONSTS
    if CONSTS is None:
        CONSTS = _consts()
    nc = build_nc()
    x = np.ascontiguousarray(inputs["x"], dtype=np.float32).reshape(NCORES, T, D)
    shared = {k: np.ascontiguousarray(v) for k, v in inputs.items() if k != "x"}
    for k, v in CONSTS.items():
        shared["c_" + k] = v
    in_maps = []
    for c in range(NCORES):
        m = dict(shared)
        m["x"] = x[c]
        in_maps.append(m)
    res = run_bass_kernel_spmd(nc, in_maps, core_ids=list(range(NCORES)))
    out = np.stack([np.asarray(r["y"]) for r in res.results], axis=0)
    return out.reshape(16, S, D).astype(np.float32)
```

```python
import numpy as np
from contextlib import ExitStack
import concourse.bass as bass
import concourse.mybir as mybir
from concourse.bass_utils import run_bass_kernel_spmd

F32 = mybir.dt.float32
BF16 = mybir.dt.bfloat16
I32 = mybir.dt.int32
U32 = mybir.dt.uint32
AF = mybir.ActivationFunctionType
ALU = mybir.AluOpType
AX = mybir.AxisListType

NCORES = 8
D = 1024
S = 2048
NSEQ = 2
T = NSEQ * S
NT = T // 128
DEPTH = 4
NE = 32
TOPK = 4
CAP = 768
NJ = CAP // 128
NSLOT = NE * CAP
ALPHA = float((2 * DEPTH) ** 0.25)
LN_EPS = 1e-5
PAD = 8
SW_LIMIT = 7.0
SW_ALPHA = 1.702
POOL_WINDOWS = (2, 4, 8, 16)
C_PATTERNS = ((128, 1), (512, 4), (2048, 16))


class Buf:
    __slots__ = ("name", "w", "r")

    def __init__(self, name):
        self.name = name
        self.w = None
        self.r = {}


class Sched:
    ENGS = ("pe", "act", "dve", "pool", "sp")

    def __init__(self, nc, es, n_lanes=40):
        self.nc = nc
        self.h = {"pe": nc.tensor, "act": nc.scalar, "dve": nc.vector, "pool": nc.gpsimd, "sp": nc.sync}
        self.sem = {e: es.enter_context(nc.semaphore("s_" + e)) for e in self.ENGS}
        self.cnt = {e: 0 for e in self.ENGS}
        self.known = {e: {} for e in self.ENGS}
        self.n_lanes = n_lanes
        self.lsem = [es.enter_context(nc.semaphore("l%d" % i)) for i in range(n_lanes)]
        self.lcnt = [0] * n_lanes
        self.next_lane = 0
        self.next_sw = 0
        self.n_hw = 24
        self.nwait = 0

    def _semof(self, key):
        return self.lsem[key[1]] if isinstance(key, tuple) else self.sem[key]

    def _need(self, e, ev, waits):
        if ev is None:
            return
        key, val = ev
        if key == e and e == "pe":
            return
        if self.known[e].get(key, 0) >= val:
            return
        if waits.get(key, 0) < val:
            waits[key] = val

    @staticmethod
    def _flat(bs):
        out = []
        for b in bs:
            if isinstance(b, (list, tuple)):
                out.extend(Sched._flat(b))
            else:
                out.append(b)
        return out

    def _deps(self, e, reads, writes):
        waits = {}
        for b in reads:
            self._need(e, b.w, waits)
        for b in writes:
            self._need(e, b.w, waits)
            for k, v in b.r.items():
                self._need(e, (k, v), waits)
        return waits

    def _emit_waits(self, e, waits):
        h = self.h[e]
        for k, v in waits.items():
            h.wait_ge(self._semof(k), v)
            self.known[e][k] = v
            self.nwait += 1

    def op(self, e, fn, reads=(), writes=()):
        reads = self._flat(reads); writes = self._flat(writes)
        waits = self._deps(e, reads, writes)
        self._emit_waits(e, waits)
        ins = fn(self.h[e])
        ins.then_inc(self.sem[e], 1)
        self.cnt[e] += 1
        v = self.cnt[e]
        for b in reads:
            b.r[e] = v
        for b in writes:
            b.w = (e, v)
            b.r = {}

    def dma(self, q, fn, reads=(), writes=()):
        if q == "pool":
            lane = self.n_hw + self.next_sw
            self.next_sw = (self.next_sw + 1) % (self.n_lanes - self.n_hw)
        else:
            lane = self.next_lane
            self.next_lane = (lane + 1) % self.n_hw
        key = ("L", lane)
        reads = self._flat(reads); writes = self._flat(writes)
        waits = self._deps(q, reads, writes)
        self._need(q, (key, self.lcnt[lane]), waits)
        self._emit_waits(q, waits)
        ins = fn(self.h[q])
        ins.then_inc(self.lsem[lane], 16)
        self.lcnt[lane] += 16
        v = self.lcnt[lane]
        for b in reads:
            b.r[key] = v
        for b in writes:
            b.w = (key, v)
            b.r = {}

    def barrier(self):
        for e in self.ENGS:
            waits = {}
            for o in self.ENGS:
                if o != e:
                    self._need(e, (o, self.cnt[o]), waits)
            for i in range(self.n_lanes):
                self._need(e, (("L", i), self.lcnt[i]), waits)
            self._emit_waits(e, waits)


def _consts():
    c = {}
    c["ident"] = np.eye(128, dtype=np.float32)
    c["ustrict"] = np.triu(np.ones((128, 128), np.float32), 1)
    c["ones"] = np.ones((128, 128), np.float32)
    rc = np.ones((4, 16), np.float32)
    for g, w in enumerate(POOL_WINDOWS):
        for t in range(8):
            lo = max(t - w // 2, 0)
            hi = min(t + w - w // 2, S)
            rc[g, t] = 1.0 / (hi - lo)
            tt = S - 8 + t
            lo = max(tt - w // 2, 0)
            hi = min(tt + w - w // 2, S)
            rc[g, 8 + t] = 1.0 / (hi - lo)
    c["poolrc"] = np.broadcast_to(rc.reshape(1, 64), (128, 64)).copy()
    ecap = (np.arange(NE, dtype=np.float32) * CAP)
    c["ecap"] = np.broadcast_to(np.tile(ecap, NT).reshape(1, NT * NE), (128, NT * NE)).copy()
    c["tokidx"] = (np.arange(NT, dtype=np.int32)[None, :] * 128 + np.arange(128, dtype=np.int32)[:, None]).astype(np.int32)
    c["dump"] = np.broadcast_to((NSLOT + np.arange(128, dtype=np.float32))[:, None], (128, NT * NE)).copy()
    init = np.zeros((128, (NSLOT + 128) // 128, 2), np.int32)
    init[:, :, 0] = T
    c["slotinit"] = init
    slopes = np.array([2.0 ** (-8.0 * (i + 1) / 8) for i in range(8)], np.float64)
    kk = np.arange(128)[:, None]
    cc = np.arange(384)[None, :]
    dist = np.abs(cc - 128 - kk)
    ed = np.zeros((128, 8, 384), np.float32)
    for h in range(8):
        ed[:, h, :] = np.where(dist <= 128, np.exp(-slopes[h] * dist), 0.0)
    c["edec_d"] = ed
    cc = np.arange(256)[None, :]
    dist = np.abs(cc - 64 - kk)
    ec = np.zeros((128, 3, 8, 256), np.float32)
    for p, (_, dil) in enumerate(C_PATTERNS):
        for h in range(8):
            ec[:, p, h, :] = np.where(dist <= 64, np.exp(-slopes[h] * dist * dil), 0.0)
    c["edec_c"] = ec
    return c


CONSTS = None


def build_nc(layers=tuple(range(DEPTH)), debug=False, stop_after=None):
    nc = bass.Bass("TRN2", target_bir_lowering=False)
    dram = {}
    _uid = [0]

    def UQ(name):
        _uid[0] += 1
        return "%s_u%d" % (name, _uid[0])

    def din(name, shape, dt=F32):
        dram[name] = nc.dram_tensor(name, list(shape), dt, kind="ExternalInput").ap()
        return dram[name]

    x_in = din("x", [T, D])
    ev_w_in = din("ev_w_in", [2, D, 2048])
    ev_pool_w = din("ev_pool_w", [2, 4, 128, 128])
    ev_pool_scale = din("ev_pool_scale", [2, 512])
    ev_conv_w = din("ev_conv_w", [2, 3, 512])
    ev_w_out = din("ev_w_out", [2, 1024, 1024])
    od_w_in = din("od_w_in", [2, D, 2304])
    od_sink = din("od_sink", [2, 8])
    od_w_out = din("od_w_out", [2, 1024, 1024])
    router_w = din("router_w", [DEPTH, D, NE])
    router_b = din("router_b", [DEPTH, NE])
    exp_w_gu = din("exp_w_gu", [DEPTH, NE, D, 2048])
    exp_b_gu = din("exp_b_gu", [DEPTH, NE, 2048])
    exp_w_down = din("exp_w_down", [DEPTH, NE, 1024, D])
    exp_b_down = din("exp_b_down", [DEPTH, NE, D])
    ln_g = din("ln_g", [DEPTH, 2, D])
    ln_b = din("ln_b", [DEPTH, 2, D])
    cst = {}
    for k, v in CONSTS.items():
        cst[k] = din("c_" + k, v.shape, I32 if v.dtype == np.int32 else F32)

    y_out = nc.dram_tensor("y", [T, D], F32, kind="ExternalOutput").ap()
    okind = "ExternalOutput" if debug else "Internal"
    XA = nc.dram_tensor("XA", [T, D], F32, kind=okind).ap()
    X1 = nc.dram_tensor("X1", [T, D], F32, kind=okind).ap()
    XT = nc.dram_tensor("XT", [D, T], BF16, kind="Internal").ap()
    XS = nc.dram_tensor("XS", [T + 128, D], BF16, kind="Internal").ap()
    SLOT = nc.dram_tensor("SLOT", [NSLOT + 128, 2], I32, kind="Internal").ap()
    YS = nc.dram_tensor("YS", [NSLOT + 128, D], F32, kind="Internal").ap()
    dXA, dX1, dXT, dXS, dSLOT, dYS = (Buf(n) for n in ("XA", "X1", "XT", "XS", "SLOT", "YS"))
    dY = Buf("Y")

    with ExitStack() as es:
        sc = Sched(nc, es)
        sb = lambda name, shape, dt=F32: es.enter_context(nc.sbuf_tensor(UQ(name), list(shape), dt))

        ident = sb("ident", [128, 128])
        ident_bf = sb("ident_bf", [128, 128], BF16)
        ustrict_bf = sb("ustrict_bf", [128, 128], BF16)
        ones_bf = sb("ones_bf", [128, 128], BF16)
        ctmp = sb("ctmp", [128, 128])
        poolrc = sb("poolrc", [128, 64])
        tokidx = sb("tokidx", [128, NT], I32)
        G_all = sb("G_all", [128, NT, NE])
        L_all = sb("L_all", [128, NT, NE])
        M8_all = sb("M8_all", [128, NT, 8])
        M_all = sb("M_all", [128, NT * NE], BF16)
        SLK = sb("SLK", [128, TOPK, NT], U32)
        PAY = sb("PAY", [128, NT, TOPK, 2], I32)
        zrow = sb("zrow", [128, D], BF16)
        bC = Buf("consts")
        bG, bL, bM8, bM, bSLK, bPAY = (Buf(n) for n in ("G", "L", "M8", "M", "SLK", "PAY"))

        PS = [es.enter_context(nc.psum_tensor("ps%d" % i, [128, 1024], F32)) for i in range(4)]
        bPSh = [[Buf("ps%d_%d" % (i, hh)) for hh in range(2)] for i in range(4)]

        class _BP:
            def __getitem__(self, i):
                return _BPi(i)

        class _BPi(list):
            def __init__(self, i):
                super().__init__(bPSh[i])
        bPS = _BP()

        def ld(q, out_ap, in_ap, reads=(), writes=(), **kw):
            sc.dma(q, lambda h: h.dma_start(out=out_ap, in_=in_ap, **kw), reads=reads, writes=writes)

        ld("sp", ident[:], cst["ident"], writes=[bC])
        ld("sp", poolrc[:], cst["poolrc"], writes=[bC])
        ld("sp", tokidx[:], cst["tokidx"], writes=[bC])
        ld("pool", ident_bf[:], cst["ident"], writes=[bC])
        ld("pool", ustrict_bf[:], cst["ustrict"], writes=[bC])
        ld("pool", ones_bf[:], cst["ones"], writes=[bC])
        sc.op("dve", lambda h: h.memset(zrow[:], 0.0), writes=[bC])
        ld("sp", XS[T:T + 128, :], zrow[:], reads=[bC], writes=[dXS])
        with nc.sbuf_tensor(UQ("zf"), [128, D], F32) as zf:
            bzf = Buf("zf")
            sc.op("dve", lambda h: h.memset(zf[:], 0.0), writes=[bzf])
            ld("sp", YS[NSLOT:NSLOT + 128, :], zf[:], reads=[bzf], writes=[dYS])
            sc.barrier()

        def layer_norm_tile(z, bz, gt, bt_, bgb, out, bout, tmp):
            st, mv, rstd = tmp["st"], tmp["mv"], tmp["rstd"]
            bst = tmp["bst"]
            sc.op("dve", lambda h: h.bn_stats(out=st[:, 0, :], in_=z[:, 0:512]), reads=[bz], writes=[bst])
            sc.op("dve", lambda h: h.bn_stats(out=st[:, 1, :], in_=z[:, 512:1024]), reads=[bz], writes=[bst])
            sc.op("dve", lambda h: h.bn_aggr(out=mv[:], in_=st[:].rearrange("p a b -> p (a b)")), reads=[bst], writes=[bst])
            sc.op("dve", lambda h: h.tensor_scalar(out=rstd[:], in0=mv[:, 1:2], scalar1=LN_EPS, scalar2=None, op0=ALU.add), reads=[bst], writes=[bst])
            sc.op("act", lambda h: h.activation(out=rstd[:], in_=rstd[:], func=AF.Sqrt), reads=[bst], writes=[bst])
            sc.op("dve", lambda h: h.reciprocal(out=rstd[:], in_=rstd[:]), reads=[bst], writes=[bst])
            sc.op("dve", lambda h: h.scalar_tensor_tensor(out=z[:], in0=z[:], scalar=mv[:, 0:1], in1=gt[:], op0=ALU.subtract, op1=ALU.mult), reads=[bz, bst, bgb], writes=[bz])
            sc.op("dve", lambda h: h.scalar_tensor_tensor(out=out[:], in0=z[:], scalar=rstd[:, 0:1], in1=bt_[:], op0=ALU.mult, op1=ALU.add), reads=[bz, bst, bgb], writes=[bout])

        def transpose_to_XT(xt_tile, bxt, i, ps, bps, xtb, bxtb):
            def f(h):
                ins = None
                for k in range(8):
                    ins = h.transpose(out=ps[:, k * 128:(k + 1) * 128], in_=xt_tile[:, k * 128:(k + 1) * 128], identity=ident[:])
                return ins
            sc.op("pe", f, reads=[bxt, bC], writes=[bps])
            sc.op("act", lambda h: h.activation(out=xtb[:].rearrange("p k t -> p (k t)"), in_=ps[:], func=AF.Copy), reads=[bps], writes=[bxtb])
            ld("sp", XT.rearrange("(k p) t -> p k t", p=128)[:, :, i * 128:(i + 1) * 128], xtb[:], reads=[bxtb], writes=[])

        with ExitStack() as p0:
            xts = [p0.enter_context(nc.sbuf_tensor(UQ("p0x%d" % i), [128, D], F32)) for i in range(2)]
            bxts = [Buf("p0x%d" % i) for i in range(2)]
            xtbs = [p0.enter_context(nc.sbuf_tensor(UQ("p0b%d" % i), [128, 8, 128], BF16)) for i in range(2)]
            bxtbs = [Buf("p0b%d" % i) for i in range(2)]
            for i in range(NT):
                a = i % 2
                ld("sp", xts[a][:], x_in[i * 128:(i + 1) * 128, :], writes=[bxts[a]])
                transpose_to_XT(xts[a], bxts[a], i, PS[a], bPS[a], xtbs[a], bxtbs[a])
            sc.barrier()

        Xcur = x_in
        bXcur = Buf("xin")

        for L in layers:
            li = L // 2
            last = (L == layers[-1])
            with ExitStack() as p1:
                sbq = lambda name, shape, dt=F32: p1.enter_context(nc.sbuf_tensor(UQ(name), list(shape), dt))
                catT = sbq("catT", [128, 8, S], BF16); bcat = Buf("catT")
                for s in range(NSEQ):
                    t0 = s * S
                    with ExitStack() as pa:
                        sbp = lambda name, shape, dt=F32: pa.enter_context(nc.sbuf_tensor(UQ(name), list(shape), dt))
                        xT = sbp("xT", [128, 8, S], BF16); bxT = [Buf("xT%d" % k) for k in range(8)]
                        for k in range(8):
                            ld("sp", xT[:, k, :], XT[k * 128:(k + 1) * 128, t0:t0 + S], reads=[dXT], writes=[bxT[k]])
                        pcnt = [0]
                        if L % 2 == 0:
                            win = sbp("win", [128, 8, 2048], BF16); bwin = [Buf("win%d" % k) for k in range(8)]
                            for k in range(8):
                                ld("pool", win[:, k, :], ev_w_in[li, k * 128:(k + 1) * 128, :], writes=[bwin[k]], max_dma_last_dim=4096)
                            poolw = sbp("poolw", [128, 4, 128], BF16); bpw = Buf("poolw")
                            ld("pool", poolw[:], ev_pool_w[li].rearrange("g c d -> c g d"), writes=[bpw])
                            pscale = sbp("pscale", [128, 4]); convw = sbp("convw", [128, 3, 4])
                            ld("sp", pscale[:], ev_pool_scale[li].rearrange("(g p) -> p g", p=128), writes=[bpw], allow_slow_non_contiguous=True)
                            ld("sp", convw[:], ev_conv_w[li].rearrange("k (m p) -> p k m", p=128), writes=[bpw], allow_slow_non_contiguous=True)
                            WB = [sbp("wb%d" % i, [128, S + 2 * PAD]) for i in range(4)]
                            bWB = [Buf("wb%d" % i) for i in range(4)]
                            plb = sbp("plb", [128, S], BF16); bplb = Buf("plb")
                            etmp = sbp("etmp", [128, 16]); betmp = Buf("etmp")
                            for i in range(4):
                                sc.op("dve", lambda h, i=i: h.memset(WB[i][:], 0.0), writes=[bWB[i]])

                            def proj(fc, evac):
                                for tc in range(4):
                                    pi = pcnt[0] % 4
                                    pcnt[0] += 1
                                    ps = PS[pi]; bps = bPSh[pi][0]

                                    def f(h, tc=tc, ps=ps):
                                        ins = None
                                        for k in range(8):
                                            ins = h.matmul(ps[:, 0:512], lhsT=win[:, k, fc * 128:(fc + 1) * 128], rhs=xT[:, k, tc * 512:(tc + 1) * 512], start=(k == 0), stop=(k == 7))
                                        return ins
                                    sc.op("pe", f, reads=[bwin, bxT], writes=[bps])
                                    evac(tc, ps[:, 0:512], bps)

                            for g, w in enumerate(POOL_WINDOWS):
                                U, A, B = WB[0], WB[1], WB[2]
                                bU, bA, bB = bWB[0], bWB[1], bWB[2]
                                proj(g, lambda tc, pa_, bps: sc.op("act", lambda h: h.activation(out=U[:, PAD + tc * 512:PAD + (tc + 1) * 512], in_=pa_, func=AF.Copy), reads=[bps], writes=[bU]))
                                lo, n = PAD - 7, S + 14
                                sc.op("dve", lambda h: h.tensor_tensor(out=A[:, lo:lo + n], in0=U[:, lo - 1:lo - 1 + n], in1=U[:, lo:lo + n], op=ALU.add), reads=[bU], writes=[bA])
                                cur, bcur, oth, both = A, bA, B, bB
                                ext = 7
                                ww = 2
                                while ww < w:
                                    sh = ww // 2
                                    ext = ext - sh
                                    lo, n = PAD - ext, S + 2 * ext
                                    sc.op("dve", lambda h, cur=cur, oth=oth, lo=lo, n=n, sh=sh: h.tensor_tensor(out=oth[:, lo:lo + n], in0=cur[:, lo - sh:lo - sh + n], in1=cur[:, lo + sh:lo + sh + n], op=ALU.add), reads=[bcur], writes=[both])
                                    cur, bcur, oth, both = oth, both, cur, bcur
                                    ww *= 2
                                sc.op("dve", lambda h, cur=cur: h.scalar_tensor_tensor(out=plb[:], in0=cur[:, PAD:PAD + S], scalar=1.0 / w, in1=U[:, PAD:PAD + S], op0=ALU.mult, op1=ALU.subtract), reads=[bcur, bU], writes=[bplb])
                                sc.op("dve", lambda h, cur=cur: h.tensor_tensor(out=etmp[:, 0:8], in0=cur[:, PAD:PAD + 8], in1=poolrc[:, g * 16:g * 16 + 8], op=ALU.mult), reads=[bcur, bC], writes=[betmp])
                                sc.op("dve", lambda h, cur=cur: h.tensor_tensor(out=etmp[:, 8:16], in0=cur[:, PAD + S - 8:PAD + S], in1=poolrc[:, g * 16 + 8:g * 16 + 16], op=ALU.mult), reads=[bcur, bC], writes=[betmp])
                                sc.op("dve", lambda h: h.tensor_tensor(out=plb[:, 0:8], in0=etmp[:, 0:8], in1=U[:, PAD:PAD + 8], op=ALU.subtract), reads=[betmp, bU], writes=[bplb])
                                sc.op("dve", lambda h: h.tensor_tensor(out=plb[:, S - 8:S], in0=etmp[:, 8:16], in1=U[:, PAD + S - 8:PAD + S], op=ALU.subtract), reads=[betmp, bU], writes=[bplb])
                                for tc in range(4):
                                    pi = pcnt[0] % 4
                                    pcnt[0] += 1
                                    ps = PS[pi]; bps = bPSh[pi][0]
                                    sc.op("pe", lambda h, ps=ps, tc=tc: h.matmul(ps[:, 0:512], lhsT=poolw[:, g, :], rhs=plb[:, tc * 512:(tc + 1) * 512], start=True, stop=True), reads=[bpw, bplb], writes=[bps])
                                    sc.op("act", lambda h, ps=ps, tc=tc: h.activation(out=catT[:, g, tc * 512:(tc + 1) * 512], in_=ps[:, 0:512], func=AF.Copy, scale=pscale[:, g:g + 1]), reads=[bps, bpw], writes=[bcat])
                            for m in range(4):
                                Cb, U, A = WB[0], WB[3], WB[1]
                                bCb, bU, bA = bWB[0], bWB[3], bWB[1]
                                proj(8 + m, lambda tc, pa_, bps: sc.op("act", lambda h: h.activation(out=Cb[:, PAD + tc * 512:PAD + (tc + 1) * 512], in_=pa_, func=AF.Copy), reads=[bps], writes=[bCb]))
                                proj(12 + m, lambda tc, pa_, bps: sc.op("dve", lambda h: h.tensor_tensor(out=U[:, PAD + tc * 512:PAD + (tc + 1) * 512], in0=Cb[:, PAD + tc * 512:PAD + (tc + 1) * 512], in1=pa_, op=ALU.mult), reads=[bps, bCb], writes=[bU]))
                                sc.op("dve", lambda h: h.tensor_scalar(out=A[:, PAD:PAD + S], in0=U[:, PAD - 1:PAD - 1 + S], scalar1=convw[:, 0, m:m + 1], scalar2=None, op0=ALU.mult), reads=[bU, bpw], writes=[bA])
                                sc.op("dve", lambda h: h.scalar_tensor_tensor(out=A[:, PAD:PAD + S], in0=U[:, PAD:PAD + S], scalar=convw[:, 1, m:m + 1], in1=A[:, PAD:PAD + S], op0=ALU.mult, op1=ALU.add), reads=[bU, bpw, bA], writes=[bA])
                                sc.op("dve", lambda h: h.scalar_tensor_tensor(out=A[:, PAD:PAD + S], in0=U[:, PAD + 1:PAD + 1 + S], scalar=convw[:, 2, m:m + 1], in1=A[:, PAD:PAD + S], op0=ALU.mult, op1=ALU.add), reads=[bU, bpw, bA], writes=[bA])
                                proj(4 + m, lambda tc, pa_, bps: sc.op("dve", lambda h: h.tensor_tensor(out=catT[:, 4 + m, tc * 512:(tc + 1) * 512], in0=A[:, PAD + tc * 512:PAD + (tc + 1) * 512], in1=pa_, op=ALU.mult), reads=[bps, bA], writes=[bcat]))
                        else:
                            win = sbp("win", [128, 8, 2304], BF16); bwin = [Buf("win%d" % k) for k in range(8)]
                            for k in range(8):
                                ld("pool", win[:, k, :], od_w_in[li, k * 128:(k + 1) * 128, :], writes=[bwin[k]], max_dma_last_dim=4096)
                            qT = sbp("qT", [128, S], BF16); bqT = Buf("qT")
                            kTa = sbp("kTa", [128, S], BF16); kTb = sbp("kTb", [128, S], BF16); bkT = Buf("kT")
                            sc.op("dve", lambda h: h.memset(kTa[:], 0.0), writes=[bkT])
                            sc.op("dve", lambda h: h.memset(kTb[:], 0.0), writes=[bkT])
                            VV = sbp("VV", [128, 48, 128], BF16); bVV = Buf("VV")
                            PT = [sbp("PT%d" % i, [128, 16, 384], BF16) for i in range(2)]; bPT = [Buf("PT%d" % i) for i in range(2)]
                            Pr = [sbp("Pr%d" % i, [128, 384], BF16) for i in range(2)]; bPr = [Buf("Pr%d" % i) for i in range(2)]
                            acc_o = sbp("acc_o", [128, S]); bacc_o = Buf("acc_o")
                            acc_d = sbp("acc_d", [128, S]); bacc_d = Buf("acc_d")
                            etab = sbp("etab", [128, 3, 2, 384]); betab = Buf("etab")
                            esink = sbp("esink", [128, 8]); besink = Buf("esink")
                            ld("sp", esink[:], od_sink[li:li + 1, :].to_broadcast([128, 8]), writes=[besink])
                            sc.op("act", lambda h: h.activation(out=esink[:], in_=esink[:], func=AF.Exp), reads=[besink], writes=[besink])
                            ptc = [0]
                            sct = [0]

                            def projT(col0, dst, bdst, dup=False):
                                for tc in range(4):
                                    pi = pcnt[0] % 4
                                    pcnt[0] += 1
                                    ps = PS[pi // 2]; hh = pi % 2; bps = bPSh[pi // 2][hh]

                                    def f(h, tc=tc, ps=ps, hh=hh):
                                        ins = None
                                        for k in range(8):
                                            if not dup:
                                                ins = h.matmul(ps[:, hh * 512:(hh + 1) * 512], lhsT=win[:, k, col0:col0 + 128], rhs=xT[:, k, tc * 512:(tc + 1) * 512], start=(k == 0), stop=(k == 7))
                                            else:
                                                for half in range(2):
                                                    ins = h.matmul(ps[half * 64:(half + 1) * 64, hh * 512:(hh + 1) * 512], lhsT=win[:, k, col0:col0 + 64], rhs=xT[:, k, tc * 512:(tc + 1) * 512], start=(k == 0), stop=(k == 7))
                                        return ins
                                    sc.op("pe", f, reads=[bwin, bxT], writes=[bps])
                                    if dst is None:
                                        sc.op("act", lambda h, ps=ps, hh=hh, tc=tc: h.activation(out=kTa[0:64, tc * 512:(tc + 1) * 512], in_=ps[0:64, hh * 512:(hh + 1) * 512], func=AF.Copy), reads=[bps], writes=[bdst])
                                        sc.op("act", lambda h, ps=ps, hh=hh, tc=tc: h.activation(out=kTb[64:128, tc * 512:(tc + 1) * 512], in_=ps[64:128, hh * 512:(hh + 1) * 512], func=AF.Copy), reads=[bps], writes=[bdst])
                                    else:
                                        sc.op("act", lambda h, ps=ps, hh=hh, tc=tc: h.activation(out=dst[:, tc * 512:(tc + 1) * 512], in_=ps[:, hh * 512:(hh + 1) * 512], func=AF.Copy), reads=[bps], writes=[bdst])

                            def projV(col0, tiles):
                                for (vi, tstart, tstep) in tiles:
                                    pi = pcnt[0] % 4
                                    pcnt[0] += 1
                                    ps = PS[pi // 2]; hh = pi % 2; bps = bPSh[pi // 2][hh]

                                    def f(h, ps=ps, hh=hh, tstart=tstart, tstep=tstep):
                                        ins = None
                                        for k in range(8):
                                            ins = h.matmul(ps[:, hh * 512:hh * 512 + 128], lhsT=xT[:, k, tstart:tstart + 127 * tstep + 1:tstep], rhs=win[:, k, col0:col0 + 128], start=(k == 0), stop=(k == 7))
                                        return ins
                                    sc.op("pe", f, reads=[bwin, bxT], writes=[bps])
                                    sc.op("act", lambda h, ps=ps, hh=hh, vi=vi: h.activation(out=VV[:, vi, :], in_=ps[:, hh * 512:hh * 512 + 128], func=AF.Copy), reads=[bps], writes=[bVV])

                            def att_scores(job):
                                hb, vcol, tsel, pidx, (dil, vvb, W, rad), first, jn = job
                                if True:
                                    Ls = S // dil
                                    ntile = Ls // 128
                                    pt = PT[jn % 2]; bpt = bPT[jn % 2]
                                    tb = etab[:, tsel, hb // 64, :]
                                    for r in range(dil):
                                        for j in range(ntile):
                                            q0 = 128 * j - rad
                                            c_lo = max(0, -q0)
                                            c_hi = min(W, Ls - q0)
                                            nq = c_hi - c_lo
                                            si = sct[0] % 4
                                            sct[0] += 1
                                            ps = PS[si // 2]; hh = si % 2; bps = bPSh[si // 2][hh]
                                            kst = r + dil * (128 * j)
                                            qst = r + dil * (q0 + c_lo)
                                            sc.op("pe", lambda h, ps=ps, hh=hh, kst=kst, qst=qst, nq=nq: h.matmul(
                                                ps[:, hh * 512:hh * 512 + nq],
                                                lhsT=(kTa if hb == 0 else kTb)[:, kst:kst + 127 * dil + 1:dil],
                                                rhs=qT[:, qst:qst + (nq - 1) * dil + 1:dil], start=True, stop=True),
                                                reads=[bkT, bqT], writes=[bps])
                                            pr = Pr[si % 2]; bpr = bPr[si % 2]
                                            sc.op("act", lambda h, ps=ps, hh=hh, nq=nq, pr=pr: h.activation(out=pr[:, 0:nq], in_=ps[:, hh * 512:hh * 512 + nq], func=AF.Exp, scale=0.125), reads=[bps], writes=[bpr])
                                            ti = r * ntile + j
                                            sc.op("dve", lambda h, pr=pr, nq=nq, c_lo=c_lo, ti=ti: h.tensor_tensor(out=pt[:, ti, c_lo:c_lo + nq], in0=pr[:, 0:nq], in1=tb[:, c_lo:c_lo + nq], op=ALU.mult), reads=[bpr, betab], writes=[bpt])
                            def att_pv(job):
                                hb, vcol, tsel, pidx, (dil, vvb, W, rad), first, jn = job
                                if True:
                                    Ls = S // dil
                                    ntile = Ls // 128
                                    pt = PT[jn % 2]; bpt = bPT[jn % 2]
                                    QB = rad
                                    nqb = 512 // QB
                                    for ch in range(S // 512):
                                        hh = ch % 2
                                        pso, bpso = PS[2], bPSh[2][hh]
                                        psd, bpsd = PS[3], bPSh[3][hh]

                                        def fpv(h, which, ps, ch=ch, hh=hh):
                                            ins = None
                                            for b in range(nqb):
                                                gq = ch * 512 + b * QB
                                                r = gq // Ls
                                                l0 = gq % Ls
                                                parts = []
                                                for j in range(ntile):
                                                    k_lo = max(128 * j, l0 - rad)
                                                    k_hi = min(128 * j + 128, l0 + QB + rad)
                                                    if k_hi <= k_lo:
                                                        continue
                                                    parts.append((j, k_lo - 128 * j, k_hi - 128 * j))
                                                parts = [(j, 0, 128) for (j, _a, _b) in parts]
                                                for n_, (j, p_lo, p_hi) in enumerate(parts):
                                                    ti = r * ntile + j
                                                    col = l0 - (128 * j - rad)
                                                    if which == 0:
                                                        lhsT = VV[p_lo:p_hi, vvb + ti, vcol:vcol + 64]
                                                    else:
                                                        lhsT = ones_bf[p_lo:p_hi, 0:64]
                                                    ins = h.matmul(ps[hb:hb + 64, hh * 512 + b * QB:hh * 512 + (b + 1) * QB], lhsT=lhsT,
                                                                   rhs=pt[p_lo:p_hi, ti, col:col + QB], start=(n_ == 0), stop=(n_ == len(parts) - 1))
                                            return ins
                                        sc.op("pe", lambda h, pso=pso: fpv(h, 0, pso), reads=[bVV, bpt], writes=[bpso])
                                        sc.op("pe", lambda h, psd=psd: fpv(h, 1, psd), reads=[bC, bpt], writes=[bpsd])
                                        if dil == 1:
                                            dso = acc_o[hb:hb + 64, ch * 512:(ch + 1) * 512]
                                            dsd = acc_d[hb:hb + 64, ch * 512:(ch + 1) * 512]
                                            srco = pso[hb:hb + 64, hh * 512:(hh + 1) * 512]
                                            srcd = psd[hb:hb + 64, hh * 512:(hh + 1) * 512]
                                        else:
                                            nr = 512 // Ls if Ls < 512 else 1
                                            if Ls >= 512:
                                                r = (ch * 512) // Ls
                                                l0 = (ch * 512) % Ls
                                                st_ = r + dil * l0
                                                dso = acc_o[hb:hb + 64, st_:st_ + 511 * dil + 1:dil]
                                                dsd = acc_d[hb:hb + 64, st_:st_ + 511 * dil + 1:dil]
                                                srco = pso[hb:hb + 64, hh * 512:(hh + 1) * 512]
                                                srcd = psd[hb:hb + 64, hh * 512:(hh + 1) * 512]
                                            else:
                                                r0 = (ch * 512) // Ls
                                                dso = acc_o[hb:hb + 64, :].rearrange("p (l r) -> p r l", r=dil)[:, r0:r0 + nr, :]
                                                dsd = acc_d[hb:hb + 64, :].rearrange("p (l r) -> p r l", r=dil)[:, r0:r0 + nr, :]
                                                srco = pso[hb:hb + 64, hh * 512:(hh + 1) * 512].rearrange("p (r l) -> p r l", r=nr)
                                                srcd = psd[hb:hb + 64, hh * 512:(hh + 1) * 512].rearrange("p (r l) -> p r l", r=nr)
                                        if first and pidx == 0:
                                            sc.op("act", lambda h, dso=dso, srco=srco: h.activation(out=dso, in_=srco, func=AF.Copy), reads=[bpso], writes=[bacc_o])
                                            sc.op("act", lambda h, dsd=dsd, srcd=srcd: h.activation(out=dsd, in_=srcd, func=AF.Copy), reads=[bpsd], writes=[bacc_d])
                                        else:
                                            sc.op("dve", lambda h, dso=dso, srco=srco: h.tensor_tensor(out=dso, in0=dso, in1=srco, op=ALU.add), reads=[bpso, bacc_o], writes=[bacc_o])
                                            sc.op("dve", lambda h, dsd=dsd, srcd=srcd: h.tensor_tensor(out=dsd, in0=dsd, in1=srcd, op=ALU.add), reads=[bpsd, bacc_d], writes=[bacc_d])


                            def attend_all(jobs):
                                prev = None
                                for job in jobs:
                                    att_scores(job)
                                    if prev is not None:
                                        att_pv(prev)
                                    prev = job
                                att_pv(prev)

                            def finish(chunk, sink_heads=None):
                                if sink_heads is not None:
                                    for half, hd_ in enumerate(sink_heads):
                                        sc.op("dve", lambda h, half=half, hd_=hd_: h.tensor_scalar(out=acc_d[half * 64:(half + 1) * 64, :], in0=acc_d[half * 64:(half + 1) * 64, :], scalar1=esink[half * 64:(half + 1) * 64, hd_:hd_ + 1], scalar2=None, op0=ALU.add), reads=[bacc_d, besink], writes=[bacc_d])
                                sc.op("dve", lambda h: h.reciprocal(out=acc_d[:], in_=acc_d[:]), reads=[bacc_d], writes=[bacc_d])
                                sc.op("pool", lambda h: h.tensor_tensor(out=catT[:, chunk, :], in0=acc_o[:], in1=acc_d[:], op=ALU.mult), reads=[bacc_o, bacc_d], writes=[bcat])

                            for c in range(4):
                                ld("sp", etab[:, :, :, 0:256], cst["edec_c"][:, :, 2 * c:2 * c + 2, :], writes=[betab])
                                projT(0 + c * 128, qT, bqT)
                                projT(512 + c * 128, None, bkT)
                                tiles = []
                                vvb = {}
                                vi = 0
                                for (_, dil) in C_PATTERNS:
                                    vvb[dil] = vi
                                    Ls = S // dil
                                    for r in range(dil):
                                        for j in range(Ls // 128):
                                            tiles.append((vi, r + dil * 128 * j, dil))
                                            vi += 1
                                projV(1024 + c * 128, tiles)
                                pats = [(dil, vvb[dil], 256, 64) for (_, dil) in C_PATTERNS]
                                jobs = []
                                for half in range(2):
                                    for pidx, pat in enumerate(pats):
                                        jobs.append((half * 64, half * 64, pidx, pidx, pat, True, ptc[0]))
                                        ptc[0] += 1
                                attend_all(jobs)
                                finish(c)
                            for c in range(4):
                                g = c // 2
                                ld("sp", etab[:, 0, :, :], cst["edec_d"][:, 2 * c:2 * c + 2, :], writes=[betab])
                                projT(1536 + c * 128, qT, bqT)
                                projT(2048 + g * 64, None, bkT, dup=True)
                                projV(2176, [(j, 128 * j, 1) for j in range(16)])
                                jobs = []
                                for half in range(2):
                                    jobs.append((half * 64, g * 64, 0, 0, (1, 0, 384, 128), True, ptc[0]))
                                    ptc[0] += 1
                                attend_all(jobs)
                                finish(4 + c, sink_heads=(2 * c, 2 * c + 1))
                        sc.barrier()
                    with ExitStack() as pb:
                        sbp = lambda name, shape, dt=F32: pb.enter_context(nc.sbuf_tensor(UQ(name), list(shape), dt))
                        gtab = sbp("gtab", [128, D]); btab = sbp("btab", [128, D]); bgb = Buf("gb")
                        ld("sp", gtab[:], ln_g[L, 0:1, :].to_broadcast([128, D]), writes=[bgb])
                        ld("sp", btab[:], ln_b[L, 0:1, :].to_broadcast([128, D]), writes=[bgb])
                        rw = sbp("rw", [128, 8, NE]); brw = Buf("rw")
                        ld("sp", rw[:], router_w[L].rearrange("(k p) e -> p k e", p=128), writes=[brw])
                        rb = sbp("rb", [128, NE])
                        ld("sp", rb[:], router_b[L:L + 1, :].to_broadcast([128, NE]), writes=[brw])
                        wout = sbp("wout", [128, 8, D], BF16); bwout = [Buf("wout%d" % k) for k in range(8)]
                        w_out_src = (ev_w_out if L % 2 == 0 else od_w_out)[li]
                        for k in range(8):
                            ld("pool", wout[:, k, :], w_out_src[k * 128:(k + 1) * 128, :], writes=[bwout[k]], max_dma_last_dim=4096)
                        lnt = {"st": sbp("st", [128, 2, 6]), "mv": sbp("mv", [128, 2]), "rstd": sbp("rstd", [128, 1]), "bst": Buf("st")}
                        zt = [sbp("zt%d" % i, [128, D]) for i in range(2)]; bzt = [Buf("zt%d" % i) for i in range(2)]
                        xres, bxres, x1t, bx1t = zt, bzt, zt, bzt
                        x1b = [sbp("x1b%d" % i, [128, D], BF16) for i in range(2)]; bx1b = [Buf("x1b%d" % i) for i in range(2)]
                        x1T = sbp("x1T", [128, 8, 128]); bx1T = Buf("x1T")
                        rsm = {k: sbp("r_" + k, [128, n]) for k, n in (("nmax", 1), ("ex", NE), ("msk", NE), ("ssum", 1))}
                        brs = Buf("rsm")
                        pendB = []
                        for tt in range(S // 128):
                            i = s * (S // 128) + tt
                            a = i % 2
                            ps = PS[a]; bps = bPS[a]

                            def f(h, ps=ps, tt=tt):
                                ins = None
                                for n2 in range(2):
                                    for k in range(8):
                                        ins = h.matmul(ps[:, n2 * 512:(n2 + 1) * 512], lhsT=catT[:, k, tt * 128:(tt + 1) * 128], rhs=wout[:, k, n2 * 512:(n2 + 1) * 512], start=(k == 0), stop=(k == 7))
                                return ins
                            sc.op("pe", f, reads=[bcat, bwout], writes=[bps])
                            ld("sp", xres[a][:], Xcur[i * 128:(i + 1) * 128, :], reads=[bXcur], writes=[bxres[a]])
                            sc.op("dve", lambda h, a=a, ps=ps: h.scalar_tensor_tensor(out=zt[a][:], in0=xres[a][:], scalar=ALPHA, in1=ps[:], op0=ALU.mult, op1=ALU.add), reads=[bxres[a], bps], writes=[bzt[a]])
                            layer_norm_tile(zt[a], bzt[a], gtab, btab, bgb, x1t[a], bx1t[a], lnt)
                            ld("sp", X1[i * 128:(i + 1) * 128, :], x1t[a][:], reads=[bx1t[a]], writes=[])
                            sc.op("act", lambda h, a=a: h.activation(out=x1b[a][:], in_=x1t[a][:], func=AF.Copy), reads=[bx1t[a]], writes=[bx1b[a]])
                            ld("sp", XS[i * 128:(i + 1) * 128, :], x1b[a][:], reads=[bx1b[a]], writes=[])
                            ps2 = PS[2 + a]; bps2 = bPS[2 + a]
                            if pendB:
                                pendB.pop(0)()

                            def part2(a=a, i=i, ps2=ps2, bps2=bps2):
                              def f2(h, a=a, ps2=ps2):
                                  ins = None
                                  for k in range(8):
                                      ins = h.transpose(out=ps2[:, k * 128:(k + 1) * 128], in_=x1t[a][:, k * 128:(k + 1) * 128], identity=ident[:])
                                  return ins
                              sc.op("pe", f2, reads=[bx1t[a], bC], writes=[bps2])
                              sc.op("act", lambda h, ps2=ps2: h.activation(out=x1T[:].rearrange("p k t -> p (k t)"), in_=ps2[:], func=AF.Copy), reads=[bps2], writes=[bx1T])

                              def f3(h, ps2=ps2):
                                  ins = None
                                  for k in range(8):
                                      ins = h.matmul(ps2[:, 0:NE], lhsT=x1T[:, k, :], rhs=rw[:, k, :], start=(k == 0), stop=(k == 7))
                                  return ins
                              sc.op("pe", f3, reads=[bx1T, brw], writes=[bps2])
                              Li = L_all[:, i, :]
                              sc.op("dve", lambda h, ps2=ps2, Li=Li: h.tensor_tensor(out=Li, in0=ps2[:, 0:NE], in1=rb[:], op=ALU.add), reads=[bps2, brw], writes=[bL])
                              m8 = M8_all[:, i, :]
                              sc.op("dve", lambda h, Li=Li, m8=m8: h.max(out=m8, in_=Li), reads=[bL], writes=[bM8])
                              sc.op("dve", lambda h, Li=Li, m8=m8: h.tensor_scalar(out=rsm["msk"][:], in0=Li, scalar1=m8[:, 3:4], scalar2=None, op0=ALU.is_ge), reads=[bL, bM8], writes=[brs])
                              sc.op("dve", lambda h, m8=m8: h.tensor_scalar(out=rsm["nmax"][:], in0=m8[:, 0:1], scalar1=-1.0, scalar2=None, op0=ALU.mult), reads=[bM8], writes=[brs])
                              sc.op("act", lambda h, Li=Li: h.activation(out=rsm["ex"][:], in_=Li, func=AF.Exp, bias=rsm["nmax"][:, 0:1]), reads=[bL, brs], writes=[brs])
                              sc.op("dve", lambda h: h.scalar_tensor_tensor(out=rsm["ex"][:], in0=rsm["ex"][:], scalar=1.0, in1=rsm["msk"][:], op0=ALU.mult, op1=ALU.mult, accum_out=rsm["ssum"][:]), reads=[brs], writes=[brs])
                              sc.op("dve", lambda h: h.reciprocal(out=rsm["ssum"][:], in_=rsm["ssum"][:]), reads=[brs], writes=[brs])
                              sc.op("dve", lambda h, i=i: h.tensor_scalar(out=G_all[:, i, :], in0=rsm["ex"][:], scalar1=rsm["ssum"][:, 0:1], scalar2=None, op0=ALU.mult), reads=[brs], writes=[bG])
                              sc.op("dve", lambda h, i=i: h.tensor_copy(out=M_all[:, i * NE:(i + 1) * NE], in_=rsm["msk"][:]), reads=[brs], writes=[bM])
                            pendB.append(part2)
                        while pendB:
                            pendB.pop(0)()
                        sc.barrier()
            if stop_after == (L, "mix"):
                break

            with ExitStack() as p1b:
                sbp = lambda name, shape, dt=F32: p1b.enter_context(nc.sbuf_tensor(UQ(name), list(shape), dt))
                pos = sbp("pos", [128, NT, NE]); bpos = Buf("pos")
                carry = sbp("carry", [128, NT, NE]); bcar = Buf("carry")
                tot = sbp("tot", [128, NT, NE]); btot = Buf("tot")
                oh = sbp("oh", [128, NT, NE]); boh = Buf("oh")
                prod = sbp("prod", [128, NT, NE]); bprod = Buf("prod")
                slkf = sbp("slkf", [128, TOPK, NT]); bslkf = Buf("slkf")
                gk = sbp("gk", [128, TOPK, NT]); bgk = Buf("gk")
                sinit = sbp("sinit", [128, (NSLOT + 128) // 128, 2], I32); bsin = Buf("sinit")
                ecap = sbp("ecap", [128, NT * NE]); dumpt = sbp("dumpt", [128, NT * NE])
                ld("sp", ecap[:], cst["ecap"], writes=[bC])
                ld("sp", dumpt[:], cst["dump"], writes=[bC])
                ld("sp", sinit[:], cst["slotinit"], writes=[bsin])
                ld("sp", SLOT.rearrange("(j p) c -> p j c", p=128), sinit[:], reads=[bsin], writes=[dSLOT])

                def fW(h):
                    ins = None
                    for n2 in range(2):
                        ins = h.matmul(PS[0][:, n2 * 512:(n2 + 1) * 512], lhsT=ustrict_bf[:], rhs=M_all[:, n2 * 512:(n2 + 1) * 512], start=True, stop=True)
                    return ins
                sc.op("pe", fW, reads=[bC, bM], writes=[bPS[0]])

                def fT(h):
                    ins = None
                    for n2 in range(2):
                        ins = h.matmul(PS[1][:, n2 * 512:(n2 + 1) * 512], lhsT=ones_bf[:], rhs=M_all[:, n2 * 512:(n2 + 1) * 512], start=True, stop=True)
                    return ins
                sc.op("pe", fT, reads=[bC, bM], writes=[bPS[1]])
                sc.op("act", lambda h: h.activation(out=tot[:].rearrange("p a b -> p (a b)"), in_=PS[1][:], func=AF.Copy), reads=[bPS[1]], writes=[btot])
                sc.op("dve", lambda h: h.memset(carry[:, 0, :], 0.0), writes=[bcar])
                for i in range(1, NT):
                    sc.op("dve", lambda h, i=i: h.tensor_tensor(out=carry[:, i, :], in0=carry[:, i - 1, :], in1=tot[:, i - 1, :], op=ALU.add), reads=[bcar, btot], writes=[bcar])
                fl = lambda t: t[:].rearrange("p a b -> p (a b)")
                sc.op("dve", lambda h: h.tensor_tensor(out=fl(pos), in0=fl(carry), in1=PS[0][:], op=ALU.add), reads=[bcar, bPS[0]], writes=[bpos])
                sc.op("dve", lambda h: h.tensor_scalar(out=fl(oh), in0=fl(pos), scalar1=float(CAP) - 0.5, scalar2=None, op0=ALU.is_lt), reads=[bpos], writes=[boh])
                sc.op("dve", lambda h: h.tensor_tensor(out=fl(oh), in0=fl(oh), in1=M_all[:], op=ALU.mult), reads=[boh, bM], writes=[boh])
                sc.op("dve", lambda h: h.tensor_tensor(out=fl(pos), in0=fl(pos), in1=ecap[:], op=ALU.add), reads=[bpos, bC], writes=[bpos])
                sc.op("dve", lambda h: h.tensor_tensor(out=fl(pos), in0=fl(pos), in1=dumpt[:], op=ALU.subtract), reads=[bpos, bC], writes=[bpos])
                sc.op("dve", lambda h: h.tensor_tensor(out=fl(pos), in0=fl(pos), in1=fl(oh), op=ALU.mult), reads=[bpos, boh], writes=[bpos])
                sc.op("dve", lambda h: h.tensor_tensor(out=fl(pos), in0=fl(pos), in1=dumpt[:], op=ALU.add), reads=[bpos, bC], writes=[bpos])
                for k in range(TOPK):
                    sc.op("dve", lambda h, k=k: h.tensor_tensor(out=oh[:], in0=L_all[:], in1=M8_all[:, :, k:k + 1].to_broadcast([128, NT, NE]), op=ALU.is_equal), reads=[bL, bM8], writes=[boh])
                    sc.op("dve", lambda h: h.tensor_tensor(out=fl(prod), in0=fl(oh), in1=fl(pos), op=ALU.mult), reads=[boh, bpos], writes=[bprod])
                    sc.op("dve", lambda h, k=k: h.tensor_reduce(out=slkf[:, k, :], in_=prod[:], axis=AX.X, op=ALU.add), reads=[bprod], writes=[bslkf])
                    sc.op("dve", lambda h: h.tensor_tensor(out=fl(prod), in0=fl(oh), in1=fl(G_all), op=ALU.mult), reads=[boh, bG], writes=[bprod])
                    sc.op("dve", lambda h, k=k: h.tensor_reduce(out=gk[:, k, :], in_=prod[:], axis=AX.X, op=ALU.add), reads=[bprod], writes=[bgk])
                sc.op("dve", lambda h: h.tensor_copy(out=SLK[:], in_=slkf[:]), reads=[bslkf], writes=[bSLK])
                for k in range(TOPK):
                    sc.op("dve", lambda h, k=k: h.tensor_copy(out=PAY[:, :, k, 0], in_=tokidx[:]), reads=[bC], writes=[bPAY])
                    sc.op("dve", lambda h, k=k: h.tensor_copy(out=PAY[:, :, k, 1], in_=gk[:, k, :].bitcast(I32)), reads=[bgk], writes=[bPAY])
                for i in range(NT):
                    for k in range(TOPK):
                        sc.dma("pool", lambda h, i=i, k=k: h.indirect_dma_start(
                            out=SLOT, out_offset=bass.IndirectOffsetOnAxis(ap=SLK[:, k, i:i + 1], axis=0),
                            in_=PAY[:, i, k, :], in_offset=None), reads=[bSLK, bPAY, dSLOT], writes=[])
                sc.barrier()
            if stop_after == (L, "route"):
                break

            with ExitStack() as p2:
                sbp = lambda name, shape, dt=F32: p2.enter_context(nc.sbuf_tensor(UQ(name), list(shape), dt))
                wgu = [sbp("wgu%d" % i, [128, 8, 2048], BF16) for i in range(2)]; bwgu = [[Buf("wgu%d_%d" % (i, k)) for k in range(8)] for i in range(2)]
                wdn = [sbp("wdn%d" % i, [128, 8, D], BF16) for i in range(2)]; bwdn = [[Buf("wdn%d_%d" % (i, k)) for k in range(8)] for i in range(2)]
                braw = sbp("braw", [NE, 2048]); bbraw = Buf("braw")
                bgu = sbp("bgu", [128, 16, NE]); bbgu = Buf("bgu")
                idx = [sbp("idx%d" % i, [128, NJ, 2], I32) for i in range(4)]; bidx = [Buf("idx%d" % i) for i in range(4)]
                xg = [sbp("xg%d" % i, [128, D], BF16) for i in range(NJ)]; bxg = [Buf("xg%d" % i) for i in range(NJ)]
                xgT = [sbp("xgT%d" % i, [128, 8, CAP], BF16) for i in range(2)]; bxgT = [Buf("xgT%d" % i) for i in range(2)]
                actT = sbp("actT", [128, 8, CAP], BF16); bactT = Buf("actT")
                NH = CAP // 2
                eg = [sbp("eg%d" % i, [128, NH]) for i in range(4)]; beg = [Buf("eg%d" % i) for i in range(4)]
                esg = [sbp("esg%d" % i, [128, NH]) for i in range(4)]; besg = [Buf("esg%d" % i) for i in range(4)]
                el = [sbp("el%d" % i, [128, NH]) for i in range(4)]; bel = [Buf("el%d" % i) for i in range(4)]
                ysb = [sbp("ysb%d" % i, [128, D]) for i in range(2)]; bysb = [Buf("ysb%d" % i) for i in range(2)]

                ld("sp", braw[:], exp_b_gu[L], writes=[bbraw])

                def fb(h):
                    ins = None
                    for m in range(8):
                        for two in range(2):
                            c0 = (two * 8 + m) * NE
                            src = braw[:, :].rearrange("e (c two) -> e two c", two=2)[:, two, m * 128:(m + 1) * 128]
                            ins = h.transpose(out=PS[0][:, c0:c0 + NE], in_=src, identity=ident[0:NE, 0:NE])
                    return ins
                sc.op("pe", fb, reads=[bbraw, bC], writes=[bPS[0]])
                sc.op("act", lambda h: h.activation(out=bgu[:].rearrange("p a b -> p (a b)"), in_=PS[0][:, 0:16 * NE], func=AF.Copy), reads=[bPS[0]], writes=[bbgu])

                def load_wgu(e):
                    a = e % 2
                    for k in range(8):
                        ld("pool", wgu[a][:, k, :], exp_w_gu[L, e, k * 128:(k + 1) * 128, :], writes=[bwgu[a][k]], max_dma_last_dim=4096)

                def load_wdn(e):
                    a = e % 2
                    for k in range(8):
                        ld("pool", wdn[a][:, k, :], exp_w_down[L, e, k * 128:(k + 1) * 128, :], writes=[bwdn[a][k]], max_dma_last_dim=4096)

                def idx_load(e):
                    a4 = e % 4
                    ld("sp", idx[a4][:], SLOT[e * CAP:(e + 1) * CAP, :].rearrange("(j p) c -> p j c", p=128), reads=[dSLOT], writes=[bidx[a4]])

                def gather_issue(e):
                    a3 = e % 4
                    for j in range(NJ):
                        sc.dma("pool", lambda h, a3=a3, j=j: h.indirect_dma_start(
                            out=xg[j][:], out_offset=None, in_=XS,
                            in_offset=bass.IndirectOffsetOnAxis(ap=idx[a3][:, j, 0:1].bitcast(U32), axis=0)),
                            reads=[bidx[a3], dXS], writes=[bxg[j]])

                tcount = [0]

                def transpose_x(e, j):
                    a = e % 2
                    pi = 2 + (tcount[0] % 2)
                    tcount[0] += 1
                    psb = PS[pi][:].bitcast(BF16)

                    def ft(h, psb=psb):
                        ins = None
                        for k in range(8):
                            ins = h.transpose(out=psb[:, k * 128:(k + 1) * 128], in_=xg[j][:, k * 128:(k + 1) * 128], identity=ident_bf[:])
                        return ins
                    sc.op("pe", ft, reads=[bxg[j], bC], writes=[bPS[pi]])
                    sc.op("act", lambda h, psb=psb: h.activation(out=xgT[a][:, :, j * 128:(j + 1) * 128], in_=psb[:, 0:1024].rearrange("p (k t) -> p k t", k=8), func=AF.Copy), reads=[bPS[pi]], writes=[bxgT[a]])

                idx_load(0)
                idx_load(1)
                load_wgu(0)
                gather_issue(0)
                load_wdn(0)
                for j in range(NJ):
                    transpose_x(0, j)
                ycount = [0]
                for e in range(NE):
                    a = e % 2
                    a3 = e % 4
                    if e + 2 < NE:
                        idx_load(e + 2)
                    if e + 1 < NE:
                        load_wgu(e + 1)
                        gather_issue(e + 1)
                        load_wdn(e + 1)
                    pending = []
                    pair = 0
                    for m in range(8):
                        for nh in range(2):
                            n0 = nh * NH
                            q = pair % 2
                            psg, bpsg = PS[0], bPSh[0][q]
                            psl, bpsl = PS[1], bPSh[1][q]

                            def fg(h, two, ps, m=m, n0=n0, q=q):
                                ins = None
                                wv = wgu[a][:].rearrange("p k (c two) -> p k two c", two=2)
                                for k in range(8):
                                    ins = h.matmul(ps[:, q * 512:q * 512 + NH], lhsT=wv[:, k, two, m * 128:(m + 1) * 128], rhs=xgT[a][:, k, n0:n0 + NH], start=(k == 0), stop=(k == 7))
                                return ins
                            ei = pair % 4
                            sc.op("pe", lambda h, fg=fg, psg=psg: fg(h, 0, psg), reads=[bwgu[a], bxgT[a]], writes=[bpsg])
                            sc.op("pe", lambda h, fg=fg, psl=psl: fg(h, 1, psl), reads=[bwgu[a], bxgT[a]], writes=[bpsl])
                            pg = psg[:, q * 512:q * 512 + NH]
                            pl = psl[:, q * 512:q * 512 + NH]
                            sc.op("dve", lambda h, pg=pg, ei=ei, m=m, e=e: h.tensor_scalar(out=eg[ei][:], in0=pg, scalar1=bgu[:, m, e:e + 1], scalar2=SW_LIMIT, op0=ALU.add, op1=ALU.min), reads=[bpsg, bbgu], writes=[beg[ei]])
                            sc.op("act", lambda h, pl=pl, ei=ei, m=m, e=e: h.activation(out=el[ei][:], in_=pl, func=AF.Identity, bias=bgu[:, 8 + m, e:e + 1]), reads=[bpsl, bbgu], writes=[bel[ei]])
                            sc.op("act", lambda h, ei=ei: h.activation(out=esg[ei][:], in_=eg[ei][:], func=AF.Sigmoid, scale=SW_ALPHA), reads=[beg[ei]], writes=[besg[ei]])
                            sc.op("pool", lambda h, ei=ei: h.tensor_tensor(out=eg[ei][:], in0=eg[ei][:], in1=esg[ei][:], op=ALU.mult), reads=[beg[ei], besg[ei]], writes=[beg[ei]])
                            for fn in pending:
                                fn()
                            pending = []

                            def fin(ei=ei, m=m, n0=n0):
                                sc.op("dve", lambda h: h.tensor_scalar(out=el[ei][:], in0=el[ei][:], scalar1=SW_LIMIT, scalar2=-SW_LIMIT, op0=ALU.min, op1=ALU.max), reads=[bel[ei]], writes=[bel[ei]])
                                sc.op("dve", lambda h: h.scalar_tensor_tensor(out=actT[:, m, n0:n0 + NH], in0=el[ei][:], scalar=1.0, in1=eg[ei][:], op0=ALU.add, op1=ALU.mult), reads=[beg[ei], bel[ei]], writes=[bactT])
                            pending.append(fin)
                            if e + 1 < NE and pair in (3, 5, 7, 9, 11, 13):
                                transpose_x(e + 1, (pair - 3) // 2)
                            pair += 1
                    for fn in pending:
                        fn()
                    for j in range(NJ):
                        pi = 2 + (ycount[0] % 2)
                        yb = ycount[0] % 2
                        ycount[0] += 1
                        ps, bps = PS[pi], bPS[pi]

                        def fd(h, ps=ps, j=j):
                            ins = None
                            for n2 in range(2):
                                for k in range(8):
                                    ins = h.matmul(ps[:, n2 * 512:(n2 + 1) * 512], lhsT=actT[:, k, j * 128:(j + 1) * 128], rhs=wdn[a][:, k, n2 * 512:(n2 + 1) * 512], start=(k == 0), stop=(k == 7))
                            return ins
                        sc.op("pe", fd, reads=[bactT, bwdn[a]], writes=[bps])
                        sc.op("act", lambda h, ps=ps, yb=yb, j=j: h.activation(out=ysb[yb][:], in_=ps[:], func=AF.Copy, scale=idx[a3][:, j, 1:2].bitcast(F32)), reads=[bps, bidx[a3]], writes=[bysb[yb]])
                        ld("sp", YS[e * CAP + j * 128:e * CAP + (j + 1) * 128, :], ysb[yb][:], reads=[bysb[yb]], writes=[])
                sc.barrier()
            if stop_after == (L, "moe"):
                break

            with ExitStack() as p3:
                sbp = lambda name, shape, dt=F32: p3.enter_context(nc.sbuf_tensor(UQ(name), list(shape), dt))
                gtab = sbp("gtab2", [128, D]); btab = sbp("btab2", [128, D]); bgb = Buf("gb2")
                ld("sp", gtab[:], ln_g[L, 1:2, :].to_broadcast([128, D]), writes=[bgb])
                ld("sp", btab[:], ln_b[L, 1:2, :].to_broadcast([128, D]), writes=[bgb])
                bd = sbp("bd", [NE, D]); bbd = Buf("bd")
                ld("sp", bd[:], exp_b_down[L], writes=[bbd])
                lnt = {"st": sbp("st2", [128, 2, 6]), "mv": sbp("mv2", [128, 2]), "rstd": sbp("rstd2", [128, 1]), "bst": Buf("st2")}
                yk = [[sbp("yk%d_%d" % (a, k), [128, D]) for k in range(TOPK)] for a in range(2)]
                byk = [[Buf("yk%d_%d" % (a, k)) for k in range(TOPK)] for a in range(2)]
                x1r = [sbp("x1r%d" % a, [128, D]) for a in range(2)]; bx1r = [Buf("x1r%d" % a) for a in range(2)]
                zt = [sbp("z2_%d" % a, [128, D]) for a in range(2)]; bzt = [Buf("z2_%d" % a) for a in range(2)]
                x2t = [sbp("x2t%d" % a, [128, D]) for a in range(2)]; bx2t = [Buf("x2t%d" % a) for a in range(2)]
                xtb = [sbp("xtb%d" % a, [128, 8, 128], BF16) for a in range(2)]; bxtb = [Buf("xtb%d" % a) for a in range(2)]
                GT = sbp("GT", [NE, 128]); bGT = Buf("GT")
                Xnext = y_out if last else XA
                dXn = dY if last else dXA
                pend3 = []
                for i in range(NT):
                    a = i % 2
                    for k in range(TOPK):
                        sc.dma("pool", lambda h, a=a, k=k, i=i: h.indirect_dma_start(
                            out=yk[a][k][:], out_offset=None, in_=YS,
                            in_offset=bass.IndirectOffsetOnAxis(ap=SLK[:, k, i:i + 1], axis=0)),
                            reads=[bSLK, dYS], writes=[byk[a][k]])
                    ld("sp", x1r[a][:], X1[i * 128:(i + 1) * 128, :], reads=[dX1], writes=[bx1r[a]])
                    ps, bps = PS[a], bPS[a]
                    sc.op("pe", lambda h, ps=ps, i=i: h.transpose(out=ps[0:NE, 0:128], in_=G_all[:, i, :], identity=ident[:]), reads=[bG, bC], writes=[bps])
                    sc.op("act", lambda h, ps=ps: h.activation(out=GT[:], in_=ps[0:NE, 0:128], func=AF.Copy), reads=[bps], writes=[bGT])

                    def fgb(h, ps=ps):
                        ins = None
                        for n2 in range(2):
                            ins = h.matmul(ps[:, n2 * 512:(n2 + 1) * 512], lhsT=GT[:], rhs=bd[:, n2 * 512:(n2 + 1) * 512], start=True, stop=True)
                        return ins
                    sc.op("pe", fgb, reads=[bGT, bbd], writes=[bps])
                    sc.op("dve", lambda h, a=a, ps=ps: h.scalar_tensor_tensor(out=zt[a][:], in0=x1r[a][:], scalar=ALPHA, in1=ps[:], op0=ALU.mult, op1=ALU.add), reads=[bx1r[a], bps], writes=[bzt[a]])
                    sc.op("pool", lambda h, a=a: h.tensor_tensor(out=yk[a][0][:], in0=yk[a][0][:], in1=yk[a][1][:], op=ALU.add), reads=[byk[a][0], byk[a][1]], writes=[byk[a][0]])
                    sc.op("dve", lambda h, a=a: h.tensor_tensor(out=zt[a][:], in0=zt[a][:], in1=yk[a][2][:], op=ALU.add), reads=[bzt[a], byk[a][2]], writes=[bzt[a]])
                    sc.op("dve", lambda h, a=a: h.tensor_tensor(out=zt[a][:], in0=zt[a][:], in1=yk[a][3][:], op=ALU.add), reads=[bzt[a], byk[a][3]], writes=[bzt[a]])
                    sc.op("dve", lambda h, a=a: h.tensor_tensor(out=zt[a][:], in0=zt[a][:], in1=yk[a][0][:], op=ALU.add), reads=[bzt[a], byk[a][0]], writes=[bzt[a]])
                    layer_norm_tile(zt[a], bzt[a], gtab, btab, bgb, x2t[a], bx2t[a], lnt)
                    ld("sp", Xnext[i * 128:(i + 1) * 128, :], x2t[a][:], reads=[bx2t[a]], writes=[])
                    if pend3:
                        pend3.pop(0)()
                    if not last:
                        pend3.append(lambda a=a, i=i: transpose_to_XT(x2t[a], bx2t[a], i, PS[2 + a], bPS[2 + a], xtb[a], bxtb[a]))
                while pend3:
                    pend3.pop(0)()
                sc.barrier()
            Xcur = XA
            bXcur = dXA
        sc.barrier()
    return nc


def kernel(**inputs):
    global CONSTS
    if CONSTS is None:
        CONSTS = _consts()
    nc = build_nc()
    x = np.ascontiguousarray(inputs["x"], dtype=np.float32).reshape(NCORES, T, D)
    shared = {k: np.ascontiguousarray(v) for k, v in inputs.items() if k != "x"}
    for k, v in CONSTS.items():
        shared["c_" + k] = v
    in_maps = []
    for c in range(NCORES):
        m = dict(shared)
        m["x"] = x[c]
        in_maps.append(m)
    res = run_bass_kernel_spmd(nc, in_maps, core_ids=list(range(NCORES)))
    out = np.stack([np.asarray(r["y"]) for r in res.results], axis=0)
    return out.reshape(16, S, D).astype(np.float32)
```
